# Optimizing a Trainium2 kernel written in Bass

```python
import math
import jax
import jax.numpy as jnp
from jax import lax
import numpy as np

D_MODEL = 1024
BATCH = 8
SEQ = 4096
DEPTH = 1

CTX_LEN = 256
GRID_W = 64
NORM_EPS = 1e-6

RW_HEAD = 64
RW_HEADS = D_MODEL // RW_HEAD
RW_DIM = RW_HEADS * RW_HEAD
RW_DECAY_LORA = 64
RW_AAA_LORA = 64
RW_GATE_LORA = 128
RW_GN_EPS = 64e-5
RW_COLS = 3 * RW_DIM + 2 * RW_DECAY_LORA + 2 * RW_AAA_LORA + RW_GATE_LORA
RW_SPLITS = (RW_DIM, 2 * RW_DIM, 3 * RW_DIM, 3 * RW_DIM + 2 * RW_DECAY_LORA, 3 * RW_DIM + 2 * RW_DECAY_LORA + 2 * RW_AAA_LORA)

SSM_DIM = 2 * D_MODEL
SSM_HEAD = 64
SSM_HEADS = SSM_DIM // SSM_HEAD
SSM_GROUPS = 4
SSM_HPG = SSM_HEADS // SSM_GROUPS
SSM_STATE = 128
SSM_CONV = 5
SSM_CHUNK = 128
SSM_BC = SSM_GROUPS * SSM_STATE
SSM_CONV_DIM = SSM_DIM + 2 * SSM_BC
SSM_COLS = SSM_DIM + SSM_CONV_DIM + 2 * SSM_HEADS

GATE_COLS = 2 * D_MODEL
N_IN = RW_COLS + SSM_COLS + GATE_COLS
IN_SPLITS = (RW_COLS, RW_COLS + SSM_COLS)

N_EXPERTS = 16
EXPERT_FF = 1024
EC_CAPACITY = 2

F32 = jnp.float32

kernel_name = 'hybrid_rwkv7_mamba2_ec_moe_dit_block'


def rms_norm(x, g):
    xf = x.astype(F32)
    y = xf * lax.rsqrt(jnp.mean(xf * xf, axis=-1, keepdims=True) + NORM_EPS)
    return (y * g.astype(F32)).astype(x.dtype)


def adaln_mod(cond, w, b):
    m = jnp.einsum('...d,de->...e', jax.nn.silu(cond), w) + b
    return jnp.split(m[..., None, :], 6, axis=-1)


def centred_shift(u, mu):
    zero = jnp.zeros_like(u[:, :1])
    prev = jnp.concatenate([zero, u[:, :-1]], axis=1)
    nxt = jnp.concatenate([u[:, 1:], zero], axis=1)
    return u + mu[0] * (prev - u) + mu[1] * (nxt - u)


def dw_conv_centred(u, w, b):
    pad = w.shape[1] // 2
    out = lax.conv_general_dilated(u, jnp.transpose(w)[:, None, :].astype(u.dtype), window_strides=(1,), padding=[(pad, pad)], dimension_numbers=('NWC', 'WIO', 'NWC'), feature_group_count=u.shape[-1])
    return out + b


def to_colmajor(t, rows):
    b, n, ch = t.shape
    return t.reshape(b, rows, GRID_W, ch).swapaxes(1, 2).reshape(b, n, ch)


def from_colmajor(t, rows):
    b, n, ch = t.shape
    return t.reshape(b, GRID_W, rows, ch).swapaxes(1, 2).reshape(b, n, ch)


def rwkv_prep(u, p):
    b_, n = u.shape[:2]
    r, k, v, xw, xa, xg = jnp.split(u, RW_SPLITS, axis=-1)
    xw = xw.reshape(b_, n, 2, RW_DECAY_LORA)
    xa = xa.reshape(b_, n, 2, RW_AAA_LORA)
    w_raw = (p['rw_w0'] + jnp.einsum('blde,def->bldf', jnp.tanh(xw), p['rw_w2'])).astype(F32)
    decay = jnp.exp(-jnp.exp(-jax.nn.softplus(-w_raw) - 0.5))
    a = jax.nn.sigmoid(p['rw_a0'] + jnp.einsum('blde,def->bldf', xa, p['rw_a2']))
    g = jnp.einsum('ble,ef->blf', jax.nn.sigmoid(xg), p['rw_g2'])
    kk = (k * p['rw_kk']).astype(F32).reshape(b_, n, RW_HEADS, RW_HEAD)
    kk = kk * lax.rsqrt(jnp.maximum(jnp.sum(kk * kk, axis=-1, keepdims=True), 1e-12))
    k_eff = k[:, :, None] * (1 + (a - 1) * p['rw_ka'])
    hd = lambda t: t.reshape(*t.shape[:-1], RW_HEADS, RW_HEAD)
    return dict(r=hd(r), w=hd(decay), k=hd(k_eff), v=hd(v), kk=kk, a=hd(a), g=g)


def rwkv_scan(q, d, s0, reverse, with_output):
    seq = (q['r'], q['w'][:, :, d], q['k'][:, :, d], q['v'], q['kk'], q['a'][:, :, d])
    seq = tuple(jnp.moveaxis(t.astype(F32), 1, 0) for t in seq)

    def step(s, inp):
        r_t, w_t, k_t, v_t, kk_t, a_t = inp
        sa = jnp.einsum('bhvk,bhk->bhv', s, -kk_t)
        s = s * w_t[:, :, None, :] + sa[..., None] * (kk_t * a_t)[:, :, None, :] + v_t[..., None] * k_t[:, :, None, :]
        return s, (jnp.einsum('bhvk,bhk->bhv', s, r_t) if with_output else None)

    s, ys = lax.scan(step, s0, seq, reverse=reverse)
    return s, (jnp.moveaxis(ys, 0, 1) if with_output else None)


def rwkv_readout(y, q, p):
    b_, n = y.shape[:2]
    mean = jnp.mean(y, axis=-1, keepdims=True)
    var = jnp.mean(jnp.square(y - mean), axis=-1, keepdims=True)
    yn = ((y - mean) * lax.rsqrt(var + RW_GN_EPS)).reshape(b_, n, RW_DIM) * p['rw_ln_w'].astype(F32) + p['rw_ln_b'].astype(F32)
    bonus = jnp.einsum('blhn,bldhn,hn->blh', q['r'].astype(F32), q['k'].astype(F32), p['rw_rk'].astype(F32))[..., None] * q['v'].astype(F32)
    return (yn + bonus.reshape(b_, n, RW_DIM)) * q['g'].astype(F32)


def rwkv_branch(uc, ul, p, ctx_out):
    qc = rwkv_prep(centred_shift(uc, p['rw_mu']), p)
    ql = rwkv_prep(centred_shift(ul, p['rw_mu']), p)
    s0 = jnp.zeros((ul.shape[0], RW_HEADS, RW_HEAD, RW_HEAD), F32)
    sc_f, yc_f = rwkv_scan(qc, 0, s0, False, ctx_out)
    sc_b, yc_b = rwkv_scan(qc, 1, s0, True, ctx_out)
    _, yl_f = rwkv_scan(ql, 0, sc_f, False, True)
    _, yl_b = rwkv_scan(ql, 1, sc_b, True, True)
    out_l = rwkv_readout(yl_f + yl_b, ql, p)
    out_c = rwkv_readout(yc_f + yc_b, qc, p) if ctx_out else None
    return out_c, out_l


def ssm_prep(u, p):
    b_, n = u.shape[:2]
    z, xbc, dt = jnp.split(u, [SSM_DIM, SSM_DIM + SSM_CONV_DIM], axis=-1)
    xbc = jax.nn.silu(dw_conv_centred(xbc, p['ssm_conv_w'], p['ssm_conv_b']))
    xs, bm, cm = jnp.split(xbc, [SSM_DIM, SSM_DIM + SSM_BC], axis=-1)
    dt = jax.nn.softplus((dt.reshape(b_, n, 2, SSM_HEADS) + p['ssm_dt_bias']).astype(F32))
    return dict(z=z, x=xs.reshape(b_, n, SSM_HEADS, SSM_HEAD), b=bm.reshape(b_, n, SSM_GROUPS, SSM_STATE), c=cm.reshape(b_, n, SSM_GROUPS, SSM_STATE), dt=dt)


def ssd_chunked(xs, dt, bm, cm, a_head, h0, with_output):
    b_, n = xs.shape[:2]
    nc = n // SSM_CHUNK

    def chunks(t):
        t = t.astype(F32).reshape(b_, nc, SSM_CHUNK, *t.shape[2:])
        return jnp.moveaxis(t, 1, 0)

    xg = chunks(xs.reshape(b_, n, SSM_GROUPS, SSM_HPG, SSM_HEAD))
    dtg = chunks(dt.reshape(b_, n, SSM_GROUPS, SSM_HPG))
    bg, cg = chunks(bm), chunks(cm)
    ag = a_head.reshape(SSM_GROUPS, SSM_HPG)
    lower = jnp.tril(jnp.ones((SSM_CHUNK, SSM_CHUNK), bool))[None, :, :, None, None]

    def step(h, inp):
        x_c, dt_c, b_c, c_c = inp
        cum = jnp.cumsum(dt_c * ag, axis=1)
        xdt = x_c * dt_c[..., None]
        h_new = h * jnp.exp(cum[:, -1])[..., None, None] + jnp.einsum('bjgn,bjge,bjgep->bgepn', b_c, jnp.exp(cum[:, -1:] - cum), xdt)
        if not with_output:
            return h_new, None
        seg = jnp.exp(jnp.where(lower, cum[:, :, None] - cum[:, None, :], -jnp.inf))
        cb = jnp.einsum('bign,bjgn->bijg', c_c, b_c)
        y_intra = jnp.einsum('bijg,bijge,bjgep->bigep', cb, seg, xdt)
        y_inter = jnp.einsum('bign,bgepn->bigep', c_c, h) * jnp.exp(cum)[..., None]
        return h_new, y_intra + y_inter

    h, ys = lax.scan(step, h0, (xg, dtg, bg, cg))
    if not with_output:
        return h, None
    return h, jnp.moveaxis(ys, 0, 1).reshape(b_, n, SSM_HEADS, SSM_HEAD)


def ssd_direction(q, d, a_log, h0, reverse, with_output):
    seq = (q['x'], q['dt'][:, :, d], q['b'], q['c'])
    if reverse:
        seq = tuple(jnp.flip(t, axis=1) for t in seq)
    h, y = ssd_chunked(*seq, -jnp.exp(a_log[d].astype(F32)), h0, with_output)
    if reverse and with_output:
        y = jnp.flip(y, axis=1)
    return h, y


def ssm_readout(y, q, p):
    b_, n = y.shape[:2]
    y = (y + p['ssm_d'].astype(F32)[:, None] * q['x'].astype(F32)).reshape(b_, n, SSM_DIM)
    return rms_norm(y * jax.nn.silu(q['z'].astype(F32)), p['ssm_norm'])


def ssm_branch(uc, ul, p, rows, ctx_out):
    qc = ssm_prep(uc, p)
    ql = ssm_prep(to_colmajor(ul, rows), p)
    h0 = jnp.zeros((ul.shape[0], SSM_GROUPS, SSM_HPG, SSM_HEAD, SSM_STATE), F32)
    hc_f, yc_f = ssd_direction(qc, 0, p['ssm_a_log'], h0, False, ctx_out)
    hc_b, yc_b = ssd_direction(qc, 1, p['ssm_a_log'], h0, True, ctx_out)
    _, yl_f = ssd_direction(ql, 0, p['ssm_a_log'], hc_f, False, True)
    _, yl_b = ssd_direction(ql, 1, p['ssm_a_log'], hc_b, True, True)
    out_l = from_colmajor(ssm_readout(yl_f + yl_b, ql, p), rows)
    out_c = ssm_readout(yc_f + yc_b, qc, p) if ctx_out else None
    return out_c, out_l


def merge(ya, yb, gates, p):
    ga, gb = jnp.split(jax.nn.sigmoid(gates.astype(F32)), 2, axis=-1)
    ya = jnp.einsum('blc,cd->bld', ya, p['proj_a'])
    yb = jnp.einsum('blc,cd->bld', yb, p['proj_b'])
    return jnp.einsum('bld,de->ble', ga * ya + gb * yb, p['w_out']).astype(gates.dtype)


def mixer(hc, hl, p, rows, ctx_out):
    pc = jnp.einsum('bld,de->ble', hc, p['w_in'])
    pl = jnp.einsum('bld,de->ble', hl, p['w_in'])
    rw_c, ss_c, gt_c = jnp.split(pc, IN_SPLITS, axis=-1)
    rw_l, ss_l, gt_l = jnp.split(pl, IN_SPLITS, axis=-1)
    ya_c, ya_l = rwkv_branch(rw_c, rw_l, p, ctx_out)
    yb_c, yb_l = ssm_branch(ss_c, ss_l, p, rows, ctx_out)
    out_l = merge(ya_l, yb_l, gt_l, p)
    out_c = merge(ya_c, yb_c, gt_c, p) if ctx_out else None
    return out_c, out_l


def ec_moe(h, router, w1, w3, w2):
    b_, n, _ = h.shape
    cap = EC_CAPACITY * n // N_EXPERTS
    aff = jax.nn.softmax(jnp.einsum('bld,de->ble', h, router).astype(F32), axis=-1)
    gate, idx = lax.top_k(jnp.swapaxes(aff, 1, 2), cap)
    bidx = jnp.arange(b_)[:, None, None]
    xe = h[bidx, idx]
    hid = jax.nn.silu(jnp.einsum('becd,edf->becf', xe, w1)) * jnp.einsum('becd,edf->becf', xe, w3)
    ye = jnp.einsum('becf,efd->becd', hid, w2) * gate[..., None].astype(h.dtype)
    return jnp.zeros_like(h).at[bidx, idx].add(ye)


def setup_inputs(seed: int = 0) -> dict:
    key = jax.random.key(seed)
    ks = list(jax.random.split(key, 40))
    cnt = [0]

    def nxt():
        cnt[0] += 1
        return ks[cnt[0] - 1]

    def nrm(shape, scale):
        return scale * jax.random.normal(nxt(), shape, jnp.float32)

    def unif(shape, lo, hi):
        return jax.random.uniform(nxt(), shape, jnp.float32, lo, hi)

    L = DEPTH
    x = nrm((BATCH, SEQ, D_MODEL), 1.0)
    c = nrm((BATCH, D_MODEL), 1.0)
    ctx = nrm((BATCH, CTX_LEN, D_MODEL), 1.0)
    c_ctx = nrm((D_MODEL,), 1.0)
    ada_w = nrm((L, D_MODEL, 6 * D_MODEL), 0.5 * D_MODEL ** -0.5)
    ada_b = nrm((L, 6 * D_MODEL), 0.02)
    norm1_pre = 1.0 + nrm((L, D_MODEL), 0.02)
    norm1_post = 1.0 + nrm((L, D_MODEL), 0.02)
    norm2_pre = 1.0 + nrm((L, D_MODEL), 0.02)
    norm2_post = 1.0 + nrm((L, D_MODEL), 0.02)
    w_in = nrm((L, D_MODEL, N_IN), D_MODEL ** -0.5)
    rw_mu = unif((L, 2, RW_COLS), 0.0, 0.5)
    rw_w0 = unif((L, 2, RW_DIM), -6.0, -0.5)
    rw_w2 = nrm((L, 2, RW_DECAY_LORA, RW_DIM), 0.5 * RW_DECAY_LORA ** -0.5)
    rw_a0 = nrm((L, 2, RW_DIM), 0.1)
    rw_a2 = nrm((L, 2, RW_AAA_LORA, RW_DIM), 0.5 * RW_AAA_LORA ** -0.5)
    rw_g2 = nrm((L, RW_GATE_LORA, RW_DIM), RW_GATE_LORA ** -0.5)
    rw_kk = 0.85 + nrm((L, RW_DIM), 0.05)
    rw_ka = 1.0 + nrm((L, RW_DIM), 0.05)
    rw_rk = nrm((L, RW_HEADS, RW_HEAD), 0.1)
    rw_ln_w = 1.0 + nrm((L, RW_DIM), 0.02)
    rw_ln_b = nrm((L, RW_DIM), 0.02)
    ssm_conv_w = nrm((L, SSM_CONV_DIM, SSM_CONV), SSM_CONV ** -0.5)
    ssm_conv_b = nrm((L, SSM_CONV_DIM), 0.02)
    dt0 = jnp.exp(unif((L, 2, SSM_HEADS), math.log(1e-3), math.log(1e-1)))
    ssm_dt_bias = dt0 + jnp.log(-jnp.expm1(-dt0))
    ssm_a_log = jnp.log(unif((L, 2, SSM_HEADS), 1.0, 16.0))
    ssm_d = 1.0 + nrm((L, SSM_HEADS), 0.02)
    ssm_norm = 1.0 + nrm((L, SSM_DIM), 0.02)
    proj_a = nrm((L, RW_DIM, D_MODEL), RW_DIM ** -0.5)
    proj_b = nrm((L, SSM_DIM, D_MODEL), SSM_DIM ** -0.5)
    w_out = nrm((L, D_MODEL, D_MODEL), D_MODEL ** -0.5)
    router = nrm((L, D_MODEL, N_EXPERTS), D_MODEL ** -0.5)
    exp_w1 = nrm((L, N_EXPERTS, D_MODEL, EXPERT_FF), D_MODEL ** -0.5)
    exp_w3 = nrm((L, N_EXPERTS, D_MODEL, EXPERT_FF), D_MODEL ** -0.5)
    exp_w2 = nrm((L, N_EXPERTS, EXPERT_FF, D_MODEL), EXPERT_FF ** -0.5)
    return {'x': x, 'c': c, 'ctx': ctx, 'c_ctx': c_ctx, 'ada_w': ada_w, 'ada_b': ada_b,
            'norm1_pre': norm1_pre, 'norm1_post': norm1_post, 'norm2_pre': norm2_pre, 'norm2_post': norm2_post,
            'w_in': w_in, 'rw_mu': rw_mu, 'rw_w0': rw_w0, 'rw_w2': rw_w2, 'rw_a0': rw_a0, 'rw_a2': rw_a2,
            'rw_g2': rw_g2, 'rw_kk': rw_kk, 'rw_ka': rw_ka, 'rw_rk': rw_rk, 'rw_ln_w': rw_ln_w, 'rw_ln_b': rw_ln_b,
            'ssm_conv_w': ssm_conv_w, 'ssm_conv_b': ssm_conv_b, 'ssm_dt_bias': ssm_dt_bias, 'ssm_a_log': ssm_a_log,
            'ssm_d': ssm_d, 'ssm_norm': ssm_norm, 'proj_a': proj_a, 'proj_b': proj_b, 'w_out': w_out,
            'router': router, 'exp_w1': exp_w1, 'exp_w3': exp_w3, 'exp_w2': exp_w2}


def reference(x, c, ctx, c_ctx, ada_w, ada_b, norm1_pre, norm1_post, norm2_pre, norm2_post,
              w_in, rw_mu, rw_w0, rw_w2, rw_a0, rw_a2, rw_g2, rw_kk, rw_ka, rw_rk, rw_ln_w, rw_ln_b,
              ssm_conv_w, ssm_conv_b, ssm_dt_bias, ssm_a_log, ssm_d, ssm_norm, proj_a, proj_b, w_out,
              router, exp_w1, exp_w3, exp_w2):
    rows = x.shape[1] // GRID_W
    for l in range(DEPTH):
        ctx_out = l < DEPTH - 1
        p = dict(w_in=w_in[l], rw_mu=rw_mu[l], rw_w0=rw_w0[l], rw_w2=rw_w2[l], rw_a0=rw_a0[l], rw_a2=rw_a2[l],
                 rw_g2=rw_g2[l], rw_kk=rw_kk[l], rw_ka=rw_ka[l], rw_rk=rw_rk[l], rw_ln_w=rw_ln_w[l], rw_ln_b=rw_ln_b[l],
                 ssm_conv_w=ssm_conv_w[l], ssm_conv_b=ssm_conv_b[l], ssm_dt_bias=ssm_dt_bias[l], ssm_a_log=ssm_a_log[l],
                 ssm_d=ssm_d[l], ssm_norm=ssm_norm[l], proj_a=proj_a[l], proj_b=proj_b[l], w_out=w_out[l])
        sh1, sc1, g1, sh2, sc2, g2 = adaln_mod(c, ada_w[l], ada_b[l])
        csh1, csc1, cg1, csh2, csc2, cg2 = adaln_mod(c_ctx, ada_w[l], ada_b[l])
        hl = rms_norm(x, norm1_pre[l]) * (1 + sc1) + sh1
        hc = rms_norm(ctx, norm1_pre[l]) * (1 + csc1) + csh1
        mc, ml = mixer(hc, hl, p, rows, ctx_out)
        x = x + g1 * rms_norm(ml, norm1_post[l])
        h2 = rms_norm(x, norm2_pre[l]) * (1 + sc2) + sh2
        x = x + g2 * rms_norm(ec_moe(h2, router[l], exp_w1[l], exp_w3[l], exp_w2[l]), norm2_post[l])
        if ctx_out:
            ctx = ctx + cg1 * rms_norm(mc, norm1_post[l])
            hc2 = rms_norm(ctx, norm2_pre[l]) * (1 + csc2) + csh2
            ctx = ctx + cg2 * rms_norm(ec_moe(hc2, router[l], exp_w1[l], exp_w3[l], exp_w2[l]), norm2_post[l])
    return x
```

```python
import contextlib
import math
import numpy as np
import ml_dtypes
import concourse.bass as bass
import concourse.mybir as mybir
from concourse.bass_utils import run_bass_kernel_spmd

F32 = mybir.dt.float32
BF16 = mybir.dt.bfloat16
AF = mybir.ActivationFunctionType
ALU = mybir.AluOpType
AX = mybir.AxisListType

D = 1024
SEQ = 4096
CTX = 256
NTOK = SEQ + CTX
PADW = NTOK + 4
C0 = 1
L0 = CTX + 3
N_IN = 10688
EPS = 1e-6

SEM_CAP = 24000
NDMA_SEM = 12


class Sched:
    def __init__(self, nc, stack):
        self.nc = nc
        self.stack = stack
        self.engs = {"pe": nc.tensor, "dve": nc.vector, "act": nc.scalar, "pool": nc.gpsimd, "sp": nc.sync}
        self.sem = {}
        self.cnt = {}
        self.nsem = 0
        for e in self.engs:
            self._new_sem(e)
        self.dq = {"sp": "sp", "pool": "pool", "act": "act"}
        self.dsem = {q: [self._mk_sem("d%s%d" % (q, i)) for i in range(NDMA_SEM)] for q in self.dq}
        self.dcnt = {q: 0 for q in self.dq}
        self.seen = {e: {} for e in self.engs}
        self.lastw = {}
        self.reads = {}
        self.nins = 0

    def _mk_sem(self, name):
        self.nsem += 1
        return self.stack.enter_context(self.nc.semaphore("s%d_%s" % (self.nsem, name)))

    def _new_sem(self, e):
        self.sem[e] = self._mk_sem(e)
        self.cnt[e] = 0

    def _wait(self, e, ev):
        sem, val, src = ev
        sid = id(sem)
        if self.seen[e].get(sid, 0) >= val:
            return
        self.seen[e][sid] = val
        self.engs[e].wait_ge(sem, val)
        self.nins += 1

    def _deps(self, e, reads, writes, me):
        evs = []
        for k in reads:
            w = self.lastw.get(k)
            if w is not None and not (w[2] == me and me == "pe"):
                evs.append(w)
        for k in writes:
            w = self.lastw.get(k)
            if w is not None and w[2] != me:
                evs.append(w)
            for r in self.reads.get(k, ()):
                if r[2] != me:
                    evs.append(r)
        for ev in evs:
            self._wait(e, ev)

    def _record(self, ev, reads, writes):
        for k in reads:
            lst = self.reads.setdefault(k, [])
            lst.append(ev)
            if len(lst) > 12:
                d = {}
                for r in lst:
                    d[(r[2], id(r[0]))] = r
                self.reads[k] = list(d.values())
        for k in writes:
            self.lastw[k] = ev
            self.reads[k] = []

    def op(self, e, fn, reads=(), writes=()):
        self._deps(e, reads, writes, e)
        if self.cnt[e] >= SEM_CAP:
            self._new_sem(e)
        ins = fn(self.engs[e])
        self.cnt[e] += 1
        ins.then_inc(self.sem[e], 1)
        self.nins += 1
        ev = (self.sem[e], self.cnt[e], e)
        self._record(ev, reads, writes)
        return ev

    def dma(self, q, out, in_, reads=(), writes=(), **kw):
        e = self.dq[q]
        self._deps(e, reads, writes, None)
        i = self.dcnt[q]
        slot = i % NDMA_SEM
        rnd = i // NDMA_SEM
        sem = self.dsem[q][slot]
        if rnd > 0:
            self._wait(e, (sem, 16 * rnd, "dma" + q))
        self.engs[e].dma_start(out=out, in_=in_, **kw).then_inc(sem, 16)
        self.dcnt[q] += 1
        self.nins += 1
        ev = (sem, 16 * (rnd + 1), "dma" + q)
        self._record(ev, reads, writes)
        return ev

    def _all_events(self):
        evs = []
        for e in self.engs:
            if self.cnt[e] > 0:
                evs.append((self.sem[e], self.cnt[e], e))
        for q in self.dq:
            n = self.dcnt[q]
            for slot in range(NDMA_SEM):
                k = (n - slot + NDMA_SEM - 1) // NDMA_SEM if n > slot else 0
                if k > 0:
                    evs.append((self.dsem[q][slot], 16 * k, "dma" + q))
        return evs

    def barrier(self):
        evs = self._all_events()
        for e in self.engs:
            for ev in evs:
                if ev[2] != e:
                    self._wait(e, ev)
        self.lastw.clear()
        self.reads.clear()

    def finish(self, e="sp"):
        for ev in self._all_events():
            if ev[2] != e:
                self._wait(e, ev)


INPUT_NAMES = []


def consts():
    i = np.arange(128)
    row, col = i[:, None], i[None, :]
    masks = np.stack([col > row, col >= row, col < row, col <= row]).astype(np.float32)
    blk = (row // 64 == col // 64).astype(np.float32)
    selm = np.zeros((16, 16, 128), np.float32)
    for e_ in range(16):
        selm[e_, e_, :] = 1.0
    return {"ident": np.eye(128, dtype=np.float32), "masks": masks, "blk64": blk, "selm": selm}


def build(dbg=None):
    nc = bass.Bass("TRN2", target_bir_lowering=False)
    stack = contextlib.ExitStack()
    with stack:
        _emit(nc, stack, dbg)
    return nc


def _emit(nc, stack, dbg):
    S = Sched(nc, stack)

    del INPUT_NAMES[:]

    def din(name, shape, dt=F32):
        INPUT_NAMES.append(name)
        return nc.dram_tensor(name, list(shape), dt, kind="ExternalInput").ap()

    def dout(name, shape, dt=F32):
        return nc.dram_tensor(name, list(shape), dt, kind="ExternalOutput").ap()

    def sb(name, shape, dt=F32, st=None):
        return (st or stack).enter_context(nc.sbuf_tensor(name, list(shape), dt))

    def ps(name, shape, dt=F32, st=None):
        return (st or stack).enter_context(nc.psum_tensor(name, list(shape), dt))

    x_d = din("x", [SEQ, D])
    ctx_d = din("ctx", [CTX, D])
    c_d = din("c", [D])
    cctx_d = din("c_ctx", [D])
    adaw_d = din("ada_w", [D, 6 * D])
    adab_d = din("ada_b", [6 * D])
    n1pre_d = din("norm1_pre", [D])
    win_d = din("w_in", [D, N_IN])
    ident_d = din("ident", [128, 128])
    out_d = dout("out", [SEQ, D]) if dbg is None else None

    ident32 = sb("ident32", [128, 128])
    identb = sb("identb", [128, 128], BF16)
    S.dma("sp", ident32[:], ident_d, writes=["ident32"])
    S.op("dve", lambda e: e.tensor_copy(identb[:], ident32[:]), reads=["ident32"], writes=["identb"])

    blk_d = din("blk64", [128, 128]); masks_d = din("masks", [4, 128, 128])
    blk32 = sb("blk32", [128, 128])
    S.dma("sp", blk32[:], blk_d, writes=["blk32"])
    msk = sb("msk", [128, 4, 128])
    S.dma("sp", msk[:], masks_d.rearrange("m p c -> p m c"), writes=["msk"])
    onec = sb("onec", [128, 1])
    S.op("dve", lambda e: e.memset(onec[:], 1.0), writes=["onec"])
    epsc = sb("epsc", [128, 1])
    S.op("dve", lambda e: e.memset(epsc[:], EPS), writes=["epsc"])
    S.barrier()

    PS = [ps("ps%d" % i, [128, 512]) for i in range(6)]
    PSB = [ps("psb%d" % i, [128, 1024], BF16) for i in range(2)]

    psn = [0]

    def nextps_():
        i = psn[0] % 6
        psn[0] += 1
        return PS[i], "PS%d" % i

    modL = sb("modL", [128, 6 * D])
    modC = sb("modC", [128, 2 * D])
    with contextlib.ExitStack() as st:
        cs = sb("cs", [128, 2, 8], st=st)
        S.dma("sp", cs[:, 0, :], c_d.rearrange("(k p) -> p k", p=128), writes=["cs"], allow_slow_non_contiguous=True)
        S.dma("sp", cs[:, 1, :], cctx_d.rearrange("(k p) -> p k", p=128), writes=["cs"], allow_slow_non_contiguous=True)
        css = sb("css", [128, 2, 8], st=st)
        S.op("act", lambda e: e.activation(css[:], cs[:], AF.Silu), reads=["cs"], writes=["css"])
        cbc = sb("cbc", [128, 2, 8, 128], st=st)
        for w in range(2):
            S.op("dve", lambda e, w=w: e.tensor_copy(cbc[:, w], css[:, w, :].unsqueeze(2).to_broadcast([128, 8, 128])),
                 reads=["css"], writes=["cbc"])
        adab = sb("adab", [128, 6 * D], st=st)
        S.dma("sp", adab[:], adab_d.partition_broadcast(128), writes=["adab"])
        wbuf = [sb("adawbuf%d" % i, [128, 8, 512], st=st) for i in range(2)]
        jobs = [(0, j) for j in range(12)] + [(1, j) for j in range(4)]
        for n, (w, j) in enumerate(jobs):
            wb = wbuf[n % 2]
            S.dma("sp", wb[:], adaw_d[:, j * 512:(j + 1) * 512].rearrange("(k p) c -> p k c", p=128),
                  writes=["adaw%d" % (n % 2)])
            pt = PS[n % 2]
            for k in range(8):
                S.op("pe", lambda e, k=k, wb=wb, pt=pt, w=w: e.matmul(pt[:], cbc[:, w, k, :], wb[:, k, :], start=(k == 0), stop=(k == 7)),
                     reads=["cbc", "adaw%d" % (n % 2)], writes=["PS%d" % (n % 2)])
            dst = (modL if w == 0 else modC)[:, j * 512:(j + 1) * 512]
            S.op("dve", lambda e, dst=dst, pt=pt, j=j: e.tensor_tensor(dst, pt[:], adab[:, j * 512:(j + 1) * 512], ALU.add),
                 reads=["PS%d" % (n % 2), "adab"], writes=["mod"])
        n1pre = sb("n1pre", [128, D], st=st)
        S.dma("sp", n1pre[:], n1pre_d.partition_broadcast(128), writes=["n1pre"])
        for m in (modL, modC):
            S.op("dve", lambda e, m=m: e.scalar_tensor_tensor(m[:, D:2 * D], m[:, D:2 * D], 1.0, n1pre[:], ALU.add, ALU.mult),
                 reads=["mod", "n1pre"], writes=["mod"])
        S.barrier()

    stH = contextlib.ExitStack()
    stack.callback(stH.close)
    hT = sb("hT", [128, 8, PADW], BF16, st=stH)
    S.op("pool", lambda e: e.memset(hT[:, :, 0:1], 0.0), writes=["hT"])
    S.op("pool", lambda e: e.memset(hT[:, :, CTX + 1:CTX + 3], 0.0), writes=["hT"])
    S.op("pool", lambda e: e.memset(hT[:, :, PADW - 1:PADW], 0.0), writes=["hT"])
    with contextlib.ExitStack() as st:
        xt = [sb("xt%d" % i, [128, D], st=st) for i in range(2)]
        junk = sb("junk", [128, D], st=st)
        hb = [sb("hb%d" % i, [128, D], BF16, st=st) for i in range(2)]
        t32 = sb("t32", [128, D], st=st)
        ss = [sb("ss%d" % i, [128, 1], st=st) for i in range(2)]
        for tt in range(NTOK // 128):
            b = tt % 2
            src = ctx_d[tt * 128:(tt + 1) * 128, :] if tt < 2 else x_d[(tt - 2) * 128:(tt - 1) * 128, :]
            m = modC if tt < 2 else modL
            S.dma("sp", xt[b][:], src, writes=["xt%d" % b])
            S.op("act", lambda e, b=b: e.activation(junk[:], xt[b][:], AF.Square, accum_out=ss[b][:]),
                 reads=["xt%d" % b], writes=["junk", "ss%d" % b])
            S.op("act", lambda e, b=b: e.activation(ss[b][:], ss[b][:], AF.Sqrt, bias=epsc[:], scale=1.0 / D),
                 reads=["ss%d" % b], writes=["ss%d" % b])
            S.op("dve", lambda e, b=b: e.reciprocal(ss[b][:], ss[b][:]),
                 reads=["ss%d" % b], writes=["ss%d" % b])
            S.op("dve", lambda e, b=b, m=m: e.scalar_tensor_tensor(t32[:], xt[b][:], ss[b][:], m[:, D:2 * D], ALU.mult, ALU.mult),
                 reads=["xt%d" % b, "ss%d" % b, "mod"], writes=["t32"])
            S.op("pool", lambda e, b=b, m=m: e.tensor_tensor(hb[b][:], t32[:], m[:, 0:D], ALU.add),
                 reads=["t32", "mod"], writes=["hb%d" % b])
            pt = PSB[b]
            for k in range(8):
                S.op("pe", lambda e, k=k, b=b, pt=pt: e.transpose(pt[:, k * 128:(k + 1) * 128], hb[b][:, k * 128:(k + 1) * 128], identb[:]),
                     reads=["hb%d" % b, "identb"], writes=["PSB%d" % b])
            pp = (C0 + tt * 128) if tt < 2 else (L0 + (tt - 2) * 128)
            S.op("act", lambda e, pp=pp, pt=pt: e.copy(hT[:, :, pp:pp + 128], pt[:].rearrange("p (k t) -> p k t", k=8)),
                 reads=["PSB%d" % b], writes=["hT"])
        S.barrier()

    if dbg == "B":
        o = dout("dbg", [128, 8, PADW], BF16)
        S.dma("sp", o, hT[:], reads=["hT"])
        o2 = dout("dbg2", [128, 6 * D])
        S.dma("sp", o2, modL[:], reads=["mod"])
        S.finish()
        return

    rwmu_d = din("rw_mu", [2, 3456]); rww0_d = din("rw_w0", [2, 1024]); rww2_d = din("rw_w2", [2, 64, 1024])
    rwa0_d = din("rw_a0", [2, 1024]); rwa2_d = din("rw_a2", [2, 64, 1024]); rwg2_d = din("rw_g2", [128, 1024])
    rwkk_d = din("rw_kk", [1024]); rwka_d = din("rw_ka", [1024]); rwrk_d = din("rw_rk", [1024])
    rwlnw_d = din("rw_ln_w", [1024]); rwlnb_d = din("rw_ln_b", [1024])
    A2_s = nc.dram_tensor("rw_a2s", [1024, 2, NTOK], BF16).ap()
    D2_s = nc.dram_tensor("rw_d2s", [2, 1024, 2, NTOK], BF16).ap()
    LWT_s = nc.dram_tensor("rw_lwt", [2, NTOK, 1024], F32).ap()
    VT_s = nc.dram_tensor("rw_vt", [NTOK, 1024], BF16).ap()
    G_s = nc.dram_tensor("rw_gs", [1024, SEQ], BF16).ap()
    BV_s = nc.dram_tensor("rw_bvs", [1024, SEQ], BF16).ap()
    SKIP_RW = dbg in ("S1", "S2", "S3")
    TG = [(C0, 256, 0)] + [(L0 + 256 * g, 256, CTX + 256 * g) for g in range(16)]
    NEG_E = -math.exp(-0.5)

    with contextlib.ExitStack() as st:
        def cols(name, src):
            t = sb(name, [128, 8], st=st)
            S.dma("sp", t[:], src.rearrange("(h p) -> p h", p=128), writes=[name], allow_slow_non_contiguous=True)
            return t
        kkw = cols("kkw", rwkk_d); kaw = cols("kaw", rwka_d); rkw = cols("rkw", rwrk_d)
        w0c = [cols("w0c%d" % d, rww0_d[d]) for d in range(2)]
        a0c = [cols("a0c%d" % d, rwa0_d[d]) for d in range(2)]
        omka = sb("omka", [128, 8], st=st)
        S.op("dve", lambda e: e.tensor_scalar(omka[:], kaw[:], -1.0, 1.0, ALU.mult, ALU.add), reads=["kaw"], writes=["omka"])
        mu_all = sb("mu_all", [128, 27, 2], st=st)
        for m_ in range(2):
            S.dma("sp", mu_all[:, :, m_], rwmu_d[m_].rearrange("(t p) -> p t", p=128), writes=["mu_all"], allow_slow_non_contiguous=True)
        c0_all = sb("c0_all", [128, 27], st=st)
        S.op("dve", lambda e: e.tensor_tensor(c0_all[:], mu_all[:, :, 0], mu_all[:, :, 1], ALU.add), reads=["mu_all"], writes=["c0_all"])
        S.op("dve", lambda e: e.tensor_scalar(c0_all[:], c0_all[:], -1.0, 1.0, ALU.mult, ALU.add), reads=["c0_all"], writes=["c0_all"])
        lw32 = sb("lw32", [128, 1024], st=st)
        w2b = sb("w2b", [128, 1024], BF16, st=st); a2b = sb("a2b", [128, 1024], BF16, st=st); g2b = sb("g2b", [128, 1024], BF16, st=st)
        for dst, src in ((w2b, rww2_d.rearrange("d e c -> (d e) c")), (a2b, rwa2_d.rearrange("d e c -> (d e) c")), (g2b, rwg2_d)):
            S.dma("sp", lw32[:], src, writes=["lw32"])
            S.op("dve", lambda e, dst=dst: e.tensor_copy(dst[:], lw32[:]), reads=["lw32"], writes=["lorab"])
        th = sb("th", [128, NTOK], BF16, st=st); xab = sb("xab", [128, NTOK], BF16, st=st); sg = sb("sg", [128, NTOK], BF16, st=st)
        w32 = [sb("w32_%d" % i, [128, 8, 128], st=st) for i in range(2)]
        wbt = [sb("wbt_%d" % i, [128, 8, 128], BF16, st=st) for i in range(4)]
        nw = [0]

        def load_w(ct):
            i = nw[0]; nw[0] += 1
            a, b = w32[i % 2], wbt[i % 4]
            S.dma("sp", a[:], win_d[:, ct * 128:(ct + 1) * 128].rearrange("(k p) c -> p k c", p=128), writes=["w32_%d" % (i % 2)])
            S.op("pool", lambda e: e.tensor_copy(b[:], a[:]), reads=["w32_%d" % (i % 2)], writes=["wbt_%d" % (i % 4)])
            return b, "wbt_%d" % (i % 4)

        def proj(wb, wkey, pt, pkey, p0, n):
            for k in range(8):
                S.op("pe", lambda e, k=k: e.matmul(pt[:, 0:n + 2], wb[:, k, :], hT[:, k, p0 - 1:p0 + n + 1], start=(k == 0), stop=(k == 7)),
                     reads=[wkey, "hT"], writes=[pkey])

        def shift(pt, pkey, dst, dkey, ct, n):
            S.op("act", lambda e: e.activation(dst, pt[:, 1:n + 1], AF.Identity, scale=c0_all[:, ct:ct + 1]),
                 reads=[pkey, "c0_all"], writes=[dkey])
            S.op("dve", lambda e: e.scalar_tensor_tensor(dst, pt[:, 0:n], mu_all[:, ct, 0:1], dst, ALU.mult, ALU.add),
                 reads=[pkey, "mu_all", dkey], writes=[dkey])
            S.op("dve", lambda e: e.scalar_tensor_tensor(dst, pt[:, 2:n + 2], mu_all[:, ct, 1:2], dst, ALU.mult, ALU.add),
                 reads=[pkey, "mu_all", dkey], writes=[dkey])

        u32 = [sb("u32_%d" % i, [128, 256], st=st) for i in range(2)]
        for ct, dst, fn in (() if SKIP_RW else ((24, th, AF.Tanh), (25, xab, AF.Identity), (26, sg, AF.Sigmoid))):
            wb, wkey = load_w(ct)
            for gi, (p0, n, t0) in enumerate(TG):
                pt, pkey = PS[gi % 2], "PS%d" % (gi % 2)
                proj(wb, wkey, pt, pkey, p0, n)
                u = u32[gi % 2]; ukey = "u32_%d" % (gi % 2)
                shift(pt, pkey, u[:], ukey, ct, n)
                S.op("act", lambda e, dst=dst, fn=fn, u=u, t0=t0, n=n: e.activation(dst[:, t0:t0 + n], u[:], fn),
                     reads=[ukey], writes=["lorares"])

        def t32(name, shape=(128, 256), dt=F32):
            return [sb("%s_%d" % (name, i), list(shape), dt, st=st) for i in range(2)]
        ru_, ku_, vu_, kq_, sq_, rs_, kk_ = [t32(nm) for nm in ("ru", "ku", "vu", "kq", "sq", "rs", "kk")]
        lw_, a_, tm_, ks_ = [t32(nm) for nm in ("lw", "aa", "tm", "ks")]
        A2t_ = t32("A2t", (128, 2, 256), BF16); D2t_ = [t32("D2t%d" % d, (128, 2, 256), BF16) for d in range(2)]
        lwT_ = [t32("lwT%d" % d, (128, 2, 128)) for d in range(2)]
        vb_ = t32("vb", (128, 256), BF16); vT_ = t32("vT", (128, 2, 128), BF16)
        gb_ = t32("gb", (128, 256), BF16); bv_ = t32("bv", (128, 256), BF16)
        for hp in range(0 if SKIP_RW else 8):
            wr, wrk = load_w(hp); wk, wkk = load_w(8 + hp); wv, wvk = load_w(16 + hp)
            for gi, (p0, n, t0) in enumerate(TG):
                b = gi % 2
                sfx = "_%d" % b
                ru, ku, vu, kq, sq, rs, kk = (x[b] for x in (ru_, ku_, vu_, kq_, sq_, rs_, kk_))
                lw, aa, tm, ks = (x[b] for x in (lw_, a_, tm_, ks_))
                A2t = A2t_[b]; vb = vb_[b]; vT = vT_[b]; gb = gb_[b]; bv = bv_[b]
                for (wb, wkey, pi, dst, dk, ct) in ((wr, wrk, 0, ru, "ru", hp), (wk, wkk, 1, ku, "ku", 8 + hp), (wv, wvk, 2, vu, "vu", 16 + hp)):
                    proj(wb, wkey, PS[pi], "PS%d" % pi, p0, n)
                    shift(PS[pi], "PS%d" % pi, dst[:], dk + sfx, ct, n)
                S.op("dve", lambda e: e.tensor_scalar(kq[:], ku[:], kkw[:, hp:hp + 1], None, ALU.mult), reads=["ku" + sfx, "kkw"], writes=["kq" + sfx])
                S.op("act", lambda e: e.activation(sq[:], kq[:], AF.Square), reads=["kq" + sfx], writes=["sq" + sfx])
                S.op("pe", lambda e: e.matmul(PS[3][:, 0:n], blk32[:], sq[:], start=True, stop=True), reads=["blk32", "sq" + sfx], writes=["PS3"])
                S.op("dve", lambda e: e.tensor_scalar(rs[:], PS[3][:, 0:n], 1e-12, None, ALU.max), reads=["PS3"], writes=["rs" + sfx])
                S.op("act", lambda e: e.activation(rs[:], rs[:], AF.Sqrt), reads=["rs" + sfx], writes=["rs" + sfx])
                S.op("dve", lambda e: e.reciprocal(rs[:], rs[:]), reads=["rs" + sfx], writes=["rs" + sfx])
                S.op("dve", lambda e: e.tensor_tensor(kk[:], kq[:], rs[:], ALU.mult), reads=["kq" + sfx, "rs" + sfx], writes=["kk" + sfx])
                S.op("dve", lambda e: e.tensor_scalar(A2t[:, 0, :], kk[:], -1.0, None, ALU.mult), reads=["kk" + sfx], writes=["A2t" + sfx])
                S.op("act", lambda e: e.copy(A2t[:, 1, :], ru[:]), reads=["ru" + sfx], writes=["A2t" + sfx])
                S.dma("pool", A2_s[hp * 128:(hp + 1) * 128, :, t0:t0 + n], A2t[:], reads=["A2t" + sfx])
                for d in range(2):
                    D2t = D2t_[d][b]; lwT = lwT_[d][b]
                    dk = "d%d%s" % (d, sfx)
                    S.op("pe", lambda e, d=d: e.matmul(PS[4][:, 0:n], w2b[d * 64:(d + 1) * 64, hp * 128:(hp + 1) * 128], th[d * 64:(d + 1) * 64, t0:t0 + n], start=True, stop=True),
                         reads=["lorab", "lorares"], writes=["PS4"])
                    S.op("act", lambda e, d=d: e.activation(lw[:], PS[4][:, 0:n], AF.Sigmoid, bias=w0c[d][:, hp:hp + 1]), reads=["PS4", "w0c%d" % d], writes=["lw" + sfx])
                    S.op("dve", lambda e: e.tensor_scalar(lw[:], lw[:], NEG_E, None, ALU.mult), reads=["lw" + sfx], writes=["lw" + sfx])
                    for j in range(2):
                        S.op("pe", lambda e, j=j: e.transpose(PS[5][:, j * 128:(j + 1) * 128], lw[:, j * 128:(j + 1) * 128], ident32[:]),
                             reads=["lw" + sfx, "ident32"], writes=["PS5"])
                    S.op("act", lambda e, lwT=lwT: e.copy(lwT[:], PS[5][:, 0:256].rearrange("p (j c) -> p j c", j=2)), reads=["PS5"], writes=["lwT" + dk])
                    S.dma("pool", LWT_s[d, t0:t0 + n, hp * 128:(hp + 1) * 128].rearrange("(j p) c -> p j c", p=128), lwT[:], reads=["lwT" + dk])
                    S.op("pe", lambda e, d=d: e.matmul(PS[4][:, 0:n], a2b[d * 64:(d + 1) * 64, hp * 128:(hp + 1) * 128], xab[d * 64:(d + 1) * 64, t0:t0 + n], start=True, stop=True),
                         reads=["lorab", "lorares"], writes=["PS4"])
                    S.op("act", lambda e, d=d: e.activation(aa[:], PS[4][:, 0:n], AF.Sigmoid, bias=a0c[d][:, hp:hp + 1]), reads=["PS4", "a0c%d" % d], writes=["aa" + sfx])
                    S.op("dve", lambda e, D2t=D2t: e.tensor_tensor(D2t[:, 0, :], kk[:], aa[:], ALU.mult), reads=["kk" + sfx, "aa" + sfx], writes=["D2t" + dk])
                    S.op("dve", lambda e: e.tensor_scalar(tm[:], aa[:], kaw[:, hp:hp + 1], omka[:, hp:hp + 1], ALU.mult, ALU.add), reads=["aa" + sfx, "kaw", "omka"], writes=["tm" + sfx])
                    S.op("dve", lambda e: e.tensor_tensor(tm[:], tm[:], ku[:], ALU.mult), reads=["tm" + sfx, "ku" + sfx], writes=["tm" + sfx])
                    S.op("act", lambda e, D2t=D2t: e.copy(D2t[:, 1, :], tm[:]), reads=["tm" + sfx], writes=["D2t" + dk])
                    if d == 0:
                        S.op("act", lambda e: e.copy(ks[:], tm[:]), reads=["tm" + sfx], writes=["ks" + sfx])
                    else:
                        S.op("dve", lambda e: e.tensor_tensor(ks[:], ks[:], tm[:], ALU.add), reads=["tm" + sfx, "ks" + sfx], writes=["ks" + sfx])
                    S.dma("pool", D2_s[d, hp * 128:(hp + 1) * 128, :, t0:t0 + n], D2t[:], reads=["D2t" + dk])
                S.op("act", lambda e: e.copy(vb[:], vu[:]), reads=["vu" + sfx], writes=["vb" + sfx])
                for j in range(2):
                    S.op("pe", lambda e, j=j: e.transpose(PSB[0][:, j * 128:(j + 1) * 128], vb[:, j * 128:(j + 1) * 128], identb[:]),
                         reads=["vb" + sfx, "identb"], writes=["PSB0"])
                S.op("act", lambda e: e.copy(vT[:], PSB[0][:, 0:256].rearrange("p (j c) -> p j c", j=2)), reads=["PSB0"], writes=["vT" + sfx])
                S.dma("pool", VT_s[t0:t0 + n, hp * 128:(hp + 1) * 128].rearrange("(j p) c -> p j c", p=128), vT[:], reads=["vT" + sfx])
                if t0 >= CTX:
                    l0 = t0 - CTX
                    S.op("pe", lambda e: e.matmul(PS[4][:, 0:n], g2b[:, hp * 128:(hp + 1) * 128], sg[:, t0:t0 + n], start=True, stop=True),
                         reads=["lorab", "lorares"], writes=["PS4"])
                    S.op("act", lambda e: e.copy(gb[:], PS[4][:, 0:n]), reads=["PS4"], writes=["gb" + sfx])
                    S.dma("pool", G_s[hp * 128:(hp + 1) * 128, l0:l0 + n], gb[:], reads=["gb" + sfx])
                    S.op("dve", lambda e: e.scalar_tensor_tensor(ks[:], ks[:], rkw[:, hp:hp + 1], ru[:], ALU.mult, ALU.mult), reads=["ks" + sfx, "rkw", "ru" + sfx], writes=["ks" + sfx])
                    S.op("pe", lambda e: e.matmul(PS[3][:, 0:n], blk32[:], ks[:], start=True, stop=True), reads=["blk32", "ks" + sfx], writes=["PS3"])
                    S.op("dve", lambda e: e.tensor_tensor(bv[:], PS[3][:, 0:n], vu[:], ALU.mult), reads=["PS3", "vu" + sfx], writes=["bv" + sfx])
                    S.dma("pool", BV_s[hp * 128:(hp + 1) * 128, l0:l0 + n], bv[:], reads=["bv" + sfx])
        S.barrier()

    if dbg == "R1":
        o = dout("dbg", [4, 128, 4, NTOK], BF16)
        S.dma("sp", o[0, :, 0:2, :], A2_s[0:128], reads=[])
        S.dma("sp", o[1, :, 0:2, :], D2_s[0, 0:128], reads=[])
        S.dma("sp", o[2, :, 0:2, :], D2_s[1, 0:128], reads=[])
        S.dma("sp", o[3, :, 0, 0:SEQ], G_s[0:128], reads=[])
        S.dma("sp", o[3, :, 1, 0:SEQ], BV_s[0:128], reads=[])
        o2 = dout("dbg2", [2, NTOK, 128])
        S.dma("sp", o2[0], LWT_s[0, :, 0:128], reads=[])
        S.dma("sp", o2[1], LWT_s[1, :, 0:128], reads=[])
        o3 = dout("dbg3", [NTOK, 128], BF16)
        S.dma("sp", o3, VT_s[:, 0:128], reads=[])
        S.finish()
        return

    convw_d = din("ssm_conv_w", [3072, 5]); convb_d = din("ssm_conv_b", [3072])
    dtb_d = din("ssm_dt_bias", [64]); alog_d = din("ssm_a_log", [64])
    XT_s = nc.dram_tensor("ss_xt", [NTOK, 2048], BF16).ap()
    BT_s = nc.dram_tensor("ss_bt", [NTOK, 512], BF16).ap()
    BF_s = nc.dram_tensor("ss_bf", [4, 128, NTOK], BF16).ap()
    CF_s = nc.dram_tensor("ss_cf", [4, 128, NTOK], BF16).ap()
    ZS_s = nc.dram_tensor("ss_zs", [SEQ, 2048], BF16).ap()
    DT_s = nc.dram_tensor("ss_dt", [NTOK, 64], F32).ap()
    DA_s = nc.dram_tensor("ss_da", [NTOK, 64], F32).ap()
    GA_s = nc.dram_tensor("mg_ga", [SEQ, 1024], BF16).ap()
    GB_s = nc.dram_tensor("mg_gb", [SEQ, 1024], BF16).ap()
    hT_cm = hT[:, :, L0:L0 + SEQ].rearrange("p k (r c) -> p k c r", c=64)
    XBC0 = 3456 + 2048
    DT0 = XBC0 + 3072
    GATE0 = 3456 + 5184
    with contextlib.ExitStack() as st:
        cw = sb("cw", [128, 24, 5], st=st); cb = sb("cb", [128, 24], st=st)
        S.dma("sp", cw[:], convw_d.rearrange("(t p) j -> p t j", p=128), writes=["cw"], allow_slow_non_contiguous=True)
        S.dma("sp", cb[:], convb_d.rearrange("(t p) -> p t", p=128), writes=["cb"], allow_slow_non_contiguous=True)
        w32 = [sb("s1w32_%d" % i, [128, 8, 128], st=st) for i in range(2)]
        wbt = [sb("s1wbt_%d" % i, [128, 8, 128], BF16, st=st) for i in range(2)]
        raw_ = [sb("s1raw_%d" % i, [128, 260], st=st) for i in range(2)]
        acc_ = [sb("s1acc_%d" % i, [128, 256], st=st) for i in range(2)]
        ob_ = [sb("s1ob_%d" % i, [128, 256], BF16, st=st) for i in range(2)]
        oT_ = [sb("s1oT_%d" % i, [128, 2, 128], BF16, st=st) for i in range(2)]
        it = 0
        for ct in range(24):
            a, wb = w32[ct % 2], wbt[ct % 2]
            wkey = "s1wbt_%d" % (ct % 2)
            S.dma("sp", a[:], win_d[:, XBC0 + ct * 128:XBC0 + (ct + 1) * 128].rearrange("(k p) c -> p k c", p=128), writes=["s1w32_%d" % (ct % 2)])
            S.op("pool", lambda e: e.tensor_copy(wb[:], a[:]), reads=["s1w32_%d" % (ct % 2)], writes=[wkey])
            for gi in range(17):
                b = it % 2; it += 1
                sfx = "_%d" % b
                raw, acc, ob, oT = raw_[b], acc_[b], ob_[b], oT_[b]
                pt, pk = PS[b], "PS%d" % b
                s0 = 0 if gi == 0 else CTX + 256 * (gi - 1)
                if gi == 0:
                    for k in range(8):
                        S.op("pe", lambda e, k=k: e.matmul(pt[:, 2:258], wb[:, k, :], hT[:, k, C0:C0 + 256], start=(k == 0), stop=(k == 7)), reads=[wkey, "hT"], writes=[pk])
                else:
                    g = gi - 1
                    for k in range(8):
                        S.op("pe", lambda e, k=k, g=g: e.matmul(pt[:, 2:258].rearrange("p (c r) -> p c r", c=4), wb[:, k, :], hT_cm[:, k, 4 * g:4 * g + 4, :], start=(k == 0), stop=(k == 7)), reads=[wkey, "hT"], writes=[pk])
                    if g > 0:
                        for k in range(8):
                            S.op("pe", lambda e, k=k, g=g: e.matmul(pt[:, 0:2], wb[:, k, :], hT_cm[:, k, 4 * g - 1, 62:64], start=(k == 0), stop=(k == 7)), reads=[wkey, "hT"], writes=[pk])
                    if g < 15:
                        for k in range(8):
                            S.op("pe", lambda e, k=k, g=g: e.matmul(pt[:, 258:260], wb[:, k, :], hT_cm[:, k, 4 * g + 4, 0:2], start=(k == 0), stop=(k == 7)), reads=[wkey, "hT"], writes=[pk])
                S.op("act", lambda e: e.copy(raw[:], pt[:, 0:260]), reads=[pk], writes=["s1raw" + sfx])
                if gi <= 1:
                    S.op("pool", lambda e: e.memset(raw[:, 0:2], 0.0), writes=["s1raw" + sfx])
                if gi == 0 or gi == 16:
                    S.op("pool", lambda e: e.memset(raw[:, 258:260], 0.0), writes=["s1raw" + sfx])
                S.op("dve", lambda e: e.tensor_scalar(acc[:], raw[:, 0:256], cw[:, ct, 0:1], cb[:, ct:ct + 1], ALU.mult, ALU.add), reads=["s1raw" + sfx, "cw", "cb"], writes=["s1acc" + sfx])
                for j in range(1, 5):
                    S.op("dve", lambda e, j=j: e.scalar_tensor_tensor(acc[:], raw[:, j:j + 256], cw[:, ct, j:j + 1], acc[:], ALU.mult, ALU.add), reads=["s1raw" + sfx, "cw", "s1acc" + sfx], writes=["s1acc" + sfx])
                S.op("act", lambda e: e.activation(ob[:], acc[:], AF.Silu), reads=["s1acc" + sfx], writes=["s1ob" + sfx])
                if ct >= 16:
                    dst = (BF_s if ct < 20 else CF_s)[(ct - 16) % 4, :, s0:s0 + 256]
                    S.dma("pool", dst, ob[:], reads=["s1ob" + sfx])
                if ct < 20:
                    for j in range(2):
                        S.op("pe", lambda e, j=j: e.transpose(PSB[0][:, j * 128:(j + 1) * 128], ob[:, j * 128:(j + 1) * 128], identb[:]), reads=["s1ob" + sfx, "identb"], writes=["PSB0"])
                    S.op("act", lambda e: e.copy(oT[:], PSB[0][:, 0:256].rearrange("p (j c) -> p j c", j=2)), reads=["PSB0"], writes=["s1oT" + sfx])
                    if ct < 16:
                        dst = XT_s[s0:s0 + 256, ct * 128:(ct + 1) * 128]
                    else:
                        dst = BT_s[s0:s0 + 256, (ct - 16) * 128:(ct - 15) * 128]
                    S.dma("pool", dst.rearrange("(j p) c -> p j c", p=128), oT[:], reads=["s1oT" + sfx])
        S.barrier()

    def tiles_for(order, with_ctx):
        out = []
        if with_ctx:
            for j in range(2):
                out.append(((lambda j: (lambda k: hT[:, k, C0 + j * 128:C0 + (j + 1) * 128]))(j), 128, j * 128))
        base = CTX if with_ctx else 0
        if order == "ssm":
            for c in range(64):
                out.append(((lambda c: (lambda k: hT_cm[:, k, c, :]))(c), 64, base + c * 64))
        else:
            for tt in range(32):
                out.append(((lambda tt: (lambda k: hT[:, k, L0 + tt * 128:L0 + (tt + 1) * 128]))(tt), 128, base + tt * 128))
        return out

    for name, col0, ncol, fn, dst_s, order in (("z", 3456, 2048, AF.Silu, ZS_s, "ssm"), ("gb", GATE0 + 1024, 1024, AF.Sigmoid, GB_s, "ssm"), ("ga", GATE0, 1024, AF.Sigmoid, GA_s, "nat")):
        with contextlib.ExitStack() as st:
            wz = sb("wz" + name, [128, 8, ncol], BF16, st=st)
            stg = [sb("wzs%s%d" % (name, i), [128, 8, 512], st=st) for i in range(2)]
            for cbk in range(ncol // 512):
                S.dma("sp", stg[cbk % 2][:], win_d[:, col0 + cbk * 512:col0 + (cbk + 1) * 512].rearrange("(k p) c -> p k c", p=128), writes=["wzs%d" % (cbk % 2)])
                S.op("pool", lambda e, cbk=cbk: e.tensor_copy(wz[:, :, cbk * 512:(cbk + 1) * 512], stg[cbk % 2][:]), reads=["wzs%d" % (cbk % 2)], writes=["wz"])
            zt_ = [sb("zt%s%d" % (name, i), [128, ncol], BF16, st=st) for i in range(2)]
            for ti, (lf, nr, r0) in enumerate(tiles_for(order, False)):
                zt = zt_[ti % 2]
                for cbk in range(ncol // 512):
                    pt, pk = nextps_()
                    for k in range(8):
                        S.op("pe", lambda e, k=k, cbk=cbk, pt=pt: e.matmul(pt[0:nr, :], lf(k), wz[:, k, cbk * 512:(cbk + 1) * 512], start=(k == 0), stop=(k == 7)), reads=["wz", "hT"], writes=[pk])
                    S.op("act", lambda e, cbk=cbk, pt=pt: e.activation(zt[0:nr, cbk * 512:(cbk + 1) * 512], pt[0:nr, :], fn), reads=[pk], writes=["zt%d" % (ti % 2)])
                S.dma("pool", dst_s[r0:r0 + nr, :], zt[0:nr, :], reads=["zt%d" % (ti % 2)])
            S.barrier()
    with contextlib.ExitStack() as st:
        wd32 = sb("wd32", [128, 8, 64], st=st); wdb = sb("wdb", [128, 8, 64], BF16, st=st)
        S.dma("sp", wd32[:], win_d[:, DT0:DT0 + 64].rearrange("(k p) c -> p k c", p=128), writes=["wd32"])
        S.op("dve", lambda e: e.tensor_copy(wdb[:], wd32[:]), reads=["wd32"], writes=["wdb"])
        dtb = sb("dtb", [128, 64], st=st); aneg = sb("aneg", [128, 64], st=st)
        S.dma("sp", dtb[:], dtb_d.partition_broadcast(128), writes=["dtb"])
        S.dma("sp", aneg[:], alog_d.partition_broadcast(128), writes=["aneg"])
        S.op("act", lambda e: e.activation(aneg[:], aneg[:], AF.Exp), reads=["aneg"], writes=["aneg"])
        S.op("dve", lambda e: e.tensor_scalar(aneg[:], aneg[:], -1.0, None, ALU.mult), reads=["aneg"], writes=["aneg"])
        dx_ = [sb("dx%d" % i, [128, 64], st=st) for i in range(2)]
        da_ = [sb("dax%d" % i, [128, 64], st=st) for i in range(2)]
        for ti, (lf, nr, r0) in enumerate(tiles_for("ssm", True)):
            b = ti % 2
            dx, da = dx_[b], da_[b]
            pt, pk = nextps_()
            for k in range(8):
                S.op("pe", lambda e, k=k: e.matmul(pt[0:nr, 0:64], lf(k), wdb[:, k, :], start=(k == 0), stop=(k == 7)), reads=["wdb", "hT"], writes=[pk])
            S.op("dve", lambda e: e.scalar_tensor_tensor(dx[0:nr, :], pt[0:nr, 0:64], 30.0, dtb[0:nr, :], ALU.min, ALU.add), reads=[pk, "dtb"], writes=["dx%d" % b])
            S.op("act", lambda e: e.activation(dx[0:nr, :], dx[0:nr, :], AF.Exp), reads=["dx%d" % b], writes=["dx%d" % b])
            S.op("act", lambda e: e.activation(dx[0:nr, :], dx[0:nr, :], AF.Ln, bias=onec[0:nr, :]), reads=["dx%d" % b, "onec"], writes=["dx%d" % b])
            S.op("dve", lambda e: e.tensor_tensor(da[0:nr, :], dx[0:nr, :], aneg[0:nr, :], ALU.mult), reads=["dx%d" % b, "aneg"], writes=["dax%d" % b])
            S.dma("pool", DT_s[r0:r0 + nr, :], dx[0:nr, :], reads=["dx%d" % b])
            S.dma("pool", DA_s[r0:r0 + nr, :], da[0:nr, :], reads=["dax%d" % b])
        S.barrier()
    if dbg == "S1":
        o = dout("dbgX", [512, 2048], BF16); S.dma("sp", o[0:256], XT_s[0:256], reads=[]); S.dma("sp", o[256:512], XT_s[NTOK - 256:NTOK], reads=[])
        o = dout("dbgB", [NTOK, 512], BF16); S.dma("sp", o, BT_s, reads=[])
        o = dout("dbgBF", [128, NTOK], BF16); S.dma("sp", o, BF_s[1], reads=[])
        o = dout("dbgC", [128, NTOK], BF16); S.dma("sp", o, CF_s[2], reads=[])
        o = dout("dbgZ", [256, 2048], BF16); S.dma("sp", o, ZS_s[1024:1280], reads=[])
        o = dout("dbgGA", [256, 1024], BF16); S.dma("sp", o, GA_s[1024:1280], reads=[])
        o = dout("dbgGB", [256, 1024], BF16); S.dma("sp", o, GB_s[1024:1280], reads=[])
        o = dout("dbgDT", [NTOK, 64]); S.dma("sp", o, DT_s, reads=[])
        o = dout("dbgDA", [NTOK, 64]); S.dma("sp", o, DA_s, reads=[])
        S.finish()
        return

    S.barrier()
    stH.close()

    YS = nc.dram_tensor("rw_ys", [2, 16, 64, SEQ], F32).ap()
    nextps = nextps_

    r2cnt = [0]

    def r2_round(streams, nchunks=34):
        r2cnt[0] += 1
        rid = r2cnt[0]
        with contextlib.ExitStack() as st:
            B = []
            for si, (d, hg) in enumerate(streams):
                def T_(nm, shape, dt=BF16):
                    return sb("r2%s_%d_%d" % (nm, si, rid), list(shape), dt, st=st)
                bb = dict(
                    lwT=T_("lwT", [128, 256], F32), A2c=T_("A2c", [64, 4, 2, 128]), D2c=T_("D2c", [64, 4, 2, 128]), Vc=T_("Vc", [128, 4, 64]),
                    E1=T_("E1", [64, 4, 128], F32), E2=T_("E2", [64, 4, 128], F32), E3=T_("E3", [64, 4, 128], F32),
                    alt=T_("alt", [64, 4, 128]), rt=T_("rt", [64, 4, 128]), bet=T_("bet", [64, 4, 128]), kat=T_("kat", [64, 4, 128]),
                    W0=T_("W0", [128, 4, 128], F32), W1=T_("W1", [128, 4, 128], F32), N0=T_("N0", [128, 4, 128], F32), N1=T_("N1", [128, 4, 128], F32),
                    XA=T_("XA", [128, 4, 64]),
                    Wak=T_("Wak", [128, 4, 128]), Mrb=T_("Mrb", [128, 4, 128]), Mrk=T_("Mrk", [128, 4, 128]),
                    TT=T_("TT", [128, 2, 4, 64]), X=T_("X", [128, 4, 128], F32), AtF=T_("AtF", [64, 4, 128]), U=T_("U", [128, 4, 64]),
                    Z32=T_("Z32", [64, 4, 64], F32), Zb=T_("Zb", [64, 4, 64]), tz=T_("tz", [64, 4, 64], F32), Yt=T_("Yt", [64, 4, 128], F32),
                )
                S.op("pool", lambda e, bb=bb: e.memset(bb["Z32"][:], 0.0), writes=["Z32_%d" % si])
                S.op("pool", lambda e, bb=bb: e.memset(bb["Zb"][:], 0.0), writes=["Zb_%d" % si])
                B.append(bb)

            def k_(nm, si):
                return "%s_%d" % (nm, si)

            for step in range(nchunks):
                info = []
                for si, (d, hg) in enumerate(streams):
                    if d == 0:
                        c = step
                    else:
                        c = (1 - step) if step < 2 else (35 - step)
                    info.append((si, d, hg, c, c * 128, B[si]))
                for si, d, hg, c, t0, bb in info:
                    ch0 = hg * 256
                    S.dma("sp", bb["lwT"][:], LWT_s[d, t0:t0 + 128, ch0:ch0 + 256], writes=[k_("lwT", si)])
                    for a_ in range(2):
                        S.dma("sp", bb["A2c"][:, :, a_, :], A2_s[ch0:ch0 + 256, a_, t0:t0 + 128].rearrange("(h k) t -> k h t", k=64), writes=[k_("A2c", si)])
                        S.dma("sp", bb["D2c"][:, :, a_, :], D2_s[d, ch0:ch0 + 256, a_, t0:t0 + 128].rearrange("(h k) t -> k h t", k=64), writes=[k_("D2c", si)])
                    S.dma("sp", bb["Vc"][:], VT_s[t0:t0 + 128, ch0:ch0 + 256].rearrange("t (h v) -> t h v", h=4), writes=[k_("Vc", si)])
                for si, d, hg, c, t0, bb in info:
                    mi, me = (1, 0) if d == 0 else (3, 2)
                    pi_, pik = nextps(); pe_, pek = nextps()
                    for h in range(4):
                        S.op("pe", lambda e, h=h, bb=bb, pi_=pi_, mi=mi: e.matmul(pi_[0:64, h * 128:(h + 1) * 128], bb["lwT"][:, h * 64:(h + 1) * 64], msk[:, mi, :], start=True, stop=True),
                             reads=[k_("lwT", si), "msk"], writes=[pik])
                    for h in range(4):
                        S.op("pe", lambda e, h=h, bb=bb, pe_=pe_, me=me: e.matmul(pe_[0:64, h * 128:(h + 1) * 128], bb["lwT"][:, h * 64:(h + 1) * 64], msk[:, me, :], start=True, stop=True),
                             reads=[k_("lwT", si), "msk"], writes=[pek])
                    v3 = lambda t: t[0:64, :].rearrange("p (h t) -> p h t", h=4)
                    S.op("act", lambda e, bb=bb, pe_=pe_: e.activation(bb["E1"][:], v3(pe_), AF.Exp), reads=[pek], writes=[k_("E1", si)])
                    S.op("act", lambda e, bb=bb, pi_=pi_: e.activation(bb["E2"][:], v3(pi_), AF.Exp), reads=[pik], writes=[k_("E2", si)])
                    S.op("act", lambda e, bb=bb, pi_=pi_: e.activation(bb["E3"][:], v3(pi_), AF.Exp, scale=-1.0), reads=[pik], writes=[k_("E3", si)])
                for si, d, hg, c, t0, bb in info:
                    for dst, src, a_, E in (("alt", "A2c", 0, "E1"), ("rt", "A2c", 1, "E2"), ("bet", "D2c", 0, "E3"), ("kat", "D2c", 1, "E3")):
                        S.op("dve", lambda e, bb=bb, dst=dst, src=src, a_=a_, E=E: e.tensor_tensor(bb[dst][:], bb[src][:, :, a_, :], bb[E][:], ALU.mult),
                             reads=[k_(src, si), k_(E, si)], writes=[k_(dst, si)])
                for si, d, hg, c, t0, bb in info:
                    Wm, Nm, Mm = (0, 2, 1) if d == 0 else (2, 0, 3)
                    for dst, L, R_, mm in (("W0", "bet", "alt", Wm), ("N0", "alt", "bet", Nm), ("Wak", "kat", "alt", Wm), ("Mrb", "bet", "rt", Mm), ("Mrk", "kat", "rt", Mm)):
                        pa, pk = nextps()
                        for h in range(4):
                            S.op("pe", lambda e, h=h, bb=bb, pa=pa, L=L, R_=R_: e.matmul(pa[:, h * 128:(h + 1) * 128], bb[L][:, h, :], bb[R_][:, h, :], start=True, stop=True),
                                 reads=[k_(L, si), k_(R_, si)], writes=[pk])
                        S.op("dve", lambda e, bb=bb, pa=pa, dst=dst, mm=mm: e.tensor_tensor(bb[dst][:], pa[:].rearrange("p (h t) -> p h t", h=4), msk[:, mm, :].unsqueeze(1).to_broadcast([128, 4, 128]), ALU.mult),
                             reads=[pk, "msk"], writes=[k_(dst, si)])
                    for j, src in enumerate(("alt", "bet", "kat")):
                        for h in range(4):
                            S.op("pe", lambda e, h=h, j=j, bb=bb, src=src: e.transpose(PSB[0][:, (j * 4 + h) * 64:(j * 4 + h + 1) * 64], bb[src][:, h, :], identb[0:64, 0:64]),
                                 reads=[k_(src, si), "identb"], writes=["PSB0"])
                    S.op("act", lambda e, bb=bb: e.copy(bb["X"][:, :, 0:64], PSB[0][:, 0:256].rearrange("p (h k) -> p h k", h=4)), reads=["PSB0"], writes=[k_("X", si)])
                    S.op("act", lambda e, bb=bb: e.copy(bb["TT"][:], PSB[0][:, 256:768].rearrange("p (j h k) -> p j h k", j=2, h=4)), reads=["PSB0"], writes=[k_("TT", si)])
                    pa, pk = nextps()
                    for h in range(4):
                        S.op("pe", lambda e, h=h, bb=bb, pa=pa: e.matmul(pa[:, h * 64:(h + 1) * 64], bb["Wak"][:, h, :], bb["Vc"][:, h, :], start=True, stop=True),
                             reads=[k_("Wak", si), k_("Vc", si)], writes=[pk])
                    S.op("act", lambda e, bb=bb, pa=pa: e.copy(bb["X"][:, :, 64:128], pa[:, 0:256].rearrange("p (h v) -> p h v", h=4)), reads=[pk], writes=[k_("X", si)])
                for lvl in range(7):
                    for si, d, hg, c, t0, bb in info:
                        Wc, Nc = ("W0", "N0") if lvl % 2 == 0 else ("W1", "N1")
                        Wn, Nn = ("W1", "N1") if lvl % 2 == 0 else ("W0", "N0")
                        px, pxk = nextps()
                        for h in range(4):
                            S.op("pe", lambda e, h=h, bb=bb, px=px, Wc=Wc: e.matmul(px[:, h * 128:(h + 1) * 128], bb[Wc][:, h, :], bb["X"][:, h, :], start=True, stop=True),
                                 reads=[k_(Wc, si), k_("X", si)], writes=[pxk])
                        if lvl < 6:
                            pw, pwk = nextps(); pn, pnk = nextps()
                            for h in range(4):
                                S.op("pe", lambda e, h=h, bb=bb, pw=pw, Wc=Wc, Nc=Nc: e.matmul(pw[:, h * 128:(h + 1) * 128], bb[Nc][:, h, :], bb[Wc][:, h, :], start=True, stop=True),
                                     reads=[k_(Wc, si), k_(Nc, si)], writes=[pwk])
                            for h in range(4):
                                S.op("pe", lambda e, h=h, bb=bb, pn=pn, Wc=Wc, Nc=Nc: e.matmul(pn[:, h * 128:(h + 1) * 128], bb[Wc][:, h, :], bb[Nc][:, h, :], start=True, stop=True),
                                     reads=[k_(Wc, si), k_(Nc, si)], writes=[pnk])
                        S.op("dve", lambda e, bb=bb, px=px: e.tensor_tensor(bb["X"][:], px[:].rearrange("p (h t) -> p h t", h=4), bb["X"][:], ALU.add),
                             reads=[pxk, k_("X", si)], writes=[k_("X", si)])
                        if lvl < 6:
                            S.op("act", lambda e, bb=bb, pw=pw, Wn=Wn: e.copy(bb[Wn][:], pw[:].rearrange("p (h t) -> p h t", h=4)), reads=[pwk], writes=[k_(Wn, si)])
                            S.op("dve", lambda e, bb=bb, pn=pn, Nn=Nn: e.tensor_copy(bb[Nn][:], pn[:].rearrange("p (h t) -> p h t", h=4)), reads=[pnk], writes=[k_(Nn, si)])
                for si, d, hg, c, t0, bb in info:
                    S.op("act", lambda e, bb=bb: e.copy(bb["XA"][:], bb["X"][:, :, 0:64]), reads=[k_("X", si)], writes=[k_("XA", si)])
                    for h in range(4):
                        S.op("pe", lambda e, h=h, bb=bb: e.transpose(PSB[1][0:64, h * 128:(h + 1) * 128], bb["XA"][:, h, :], identb[:]),
                             reads=[k_("XA", si), "identb"], writes=["PSB1"])
                    S.op("act", lambda e, bb=bb: e.copy(bb["AtF"][:], PSB[1][0:64, 0:512].rearrange("p (h t) -> p h t", h=4)), reads=["PSB1"], writes=[k_("AtF", si)])
                for si, d, hg, c, t0, bb in info:
                    pa, pk = nextps()
                    for h in range(4):
                        S.op("pe", lambda e, h=h, bb=bb, pa=pa: e.matmul(pa[:, h * 64:(h + 1) * 64], bb["AtF"][:, h, :], bb["Zb"][:, h, :], start=True, stop=True),
                             reads=[k_("AtF", si), k_("Zb", si)], writes=[pk])
                    S.op("dve", lambda e, bb=bb, pa=pa: e.tensor_tensor(bb["U"][:], pa[:, 0:256].rearrange("p (h v) -> p h v", h=4), bb["X"][:, :, 64:128], ALU.add),
                         reads=[pk, k_("X", si)], writes=[k_("U", si)])
                for si, d, hg, c, t0, bb in info:
                    if c >= 2:
                        py, pyk = nextps()
                        for h in range(4):
                            o_ = py[0:64, h * 128:(h + 1) * 128]
                            S.op("pe", lambda e, h=h, bb=bb, o_=o_: e.matmul(o_, bb["Zb"][:, h, :], bb["rt"][:, h, :], start=True, stop=False), reads=[k_("Zb", si), k_("rt", si)], writes=[pyk])
                            S.op("pe", lambda e, h=h, bb=bb, o_=o_: e.matmul(o_, bb["U"][:, h, :], bb["Mrb"][:, h, :], start=False, stop=False), reads=[k_("U", si), k_("Mrb", si)], writes=[pyk])
                            S.op("pe", lambda e, h=h, bb=bb, o_=o_: e.matmul(o_, bb["Vc"][:, h, :], bb["Mrk"][:, h, :], start=False, stop=True), reads=[k_("Vc", si), k_("Mrk", si)], writes=[pyk])
                        S.op("act", lambda e, bb=bb, py=py: e.copy(bb["Yt"][:], py[0:64, :].rearrange("p (h t) -> p h t", h=4)), reads=[pyk], writes=[k_("Yt", si)])
                        l0 = t0 - CTX
                        S.dma("pool", YS[d, hg * 4:hg * 4 + 4, :, l0:l0 + 128].rearrange("h v t -> v h t"), bb["Yt"][:], reads=[k_("Yt", si)])
                    pz, pzk = nextps()
                    for h in range(4):
                        o_ = pz[0:64, h * 64:(h + 1) * 64]
                        S.op("pe", lambda e, h=h, bb=bb, o_=o_: e.matmul(o_, bb["TT"][:, 0, h, :], bb["U"][:, h, :], start=True, stop=False), reads=[k_("TT", si), k_("U", si)], writes=[pzk])
                        S.op("pe", lambda e, h=h, bb=bb, o_=o_: e.matmul(o_, bb["TT"][:, 1, h, :], bb["Vc"][:, h, :], start=False, stop=True), reads=[k_("TT", si), k_("Vc", si)], writes=[pzk])
                    last = 127 if d == 0 else 0
                    S.op("dve", lambda e, bb=bb, pz=pz: e.tensor_tensor(bb["tz"][:], pz[0:64, 0:256].rearrange("p (h v) -> p h v", h=4), bb["Z32"][:], ALU.add),
                         reads=[pzk, k_("Z32", si)], writes=[k_("tz", si)])
                    S.op("dve", lambda e, bb=bb, last=last: e.tensor_tensor(bb["Z32"][:], bb["tz"][:], bb["E2"][:, :, last:last + 1].to_broadcast([64, 4, 64]), ALU.mult),
                         reads=[k_("tz", si), k_("E2", si)], writes=[k_("Z32", si)])
                    S.op("act", lambda e, bb=bb: e.copy(bb["Zb"][:], bb["Z32"][:]), reads=[k_("Z32", si)], writes=[k_("Zb", si)])
            S.barrier()
            if dbg == "R2a":
                bb = B[0]
                o = dout("dbgA", [64, 4, 4, 128], BF16)
                for j, nm in enumerate(("alt", "rt", "bet", "kat")):
                    S.dma("sp", o[:, j], bb[nm][:], reads=[])
                o = dout("dbgB", [128, 4, 128])
                S.dma("sp", o, bb["X"][:], reads=[])
                o = dout("dbgC", [128, 4, 64], BF16)
                S.dma("sp", o, bb["U"][:], reads=[])
                o = dout("dbgD", [64, 4, 64])
                S.dma("sp", o, bb["Z32"][:], reads=[])
                o = dout("dbgE", [64, 3, 4, 128])
                for j, nm in enumerate(("E1", "E2", "E3")):
                    S.dma("sp", o[:, j], bb[nm][:], reads=[])
                S.barrier()

    if dbg == "R2a":
        r2_round([(0, 0), (1, 0)], 1)
        S.finish()
        return
    R2N = 34 if dbg != "R2" else 6
    if dbg == "R2":
        r2_round([(0, 0), (1, 0)], R2N)
        o = dout("dbg", [2, 4, 64, 512])
        S.dma("sp", o[0], YS[0, 0:4, :, 0:512], reads=[])
        S.dma("sp", o[1], YS[1, 0:4, :, SEQ - 512:SEQ], reads=[])
        S.finish()
        return
    if dbg == "R3":
        r2_round([(0, 0), (1, 0)])
    elif not SKIP_RW:
        for rnd in range(2):
            r2_round([(0, 2 * rnd), (1, 2 * rnd), (0, 2 * rnd + 1), (1, 2 * rnd + 1)])

    YD_s = nc.dram_tensor("ss_yd", [2, SEQ, 2048], F32).ap()

    def s2_scan(nsteps=34):
        with contextlib.ExitStack() as st:
            ones128 = sb("ones128", [128, 128], st=st)
            S.op("dve", lambda e: e.memset(ones128[:], 1.0), writes=["ones128"])
            negm = sb("negm", [128, 2, 128], st=st)
            S.op("dve", lambda e: e.tensor_scalar(negm[:, 0, :], msk[:, 2, :], -1e30, None, ALU.mult), reads=["msk"], writes=["negm"])
            S.op("dve", lambda e: e.tensor_scalar(negm[:, 1, :], msk[:, 0, :], -1e30, None, ALU.mult), reads=["msk"], writes=["negm"])
            B = []
            for d in range(2):
                def T_(nm, shape, dt=BF16):
                    return sb("s2%s_%d" % (nm, d), list(shape), dt, st=st)
                bb = dict(XT=T_("XT", [128, 32, 64]), BT=T_("BT", [128, 512]), BF=T_("BF", [128, 4, 128]), CF=T_("CF", [128, 4, 128]),
                          DT=T_("DT", [128, 32], F32), DA=T_("DA", [128, 32], F32), cum=T_("cum", [128, 32], F32), ncum=T_("ncum", [128, 32], F32),
                          Ecum=T_("Ecum", [128, 32], F32), Gd=T_("Gd", [128, 32], F32), Etot=T_("Etot", [128, 32], F32),
                          xdt=T_("xdt", [128, 32, 64]), xs=T_("xs", [128, 32, 64]), rhsA=T_("rhsA", [128, 32, 128], F32),
                          CBT=T_("CBT", [128, 4, 128], F32), seg0=T_("seg0", [128, 8, 128], F32), seg1=T_("seg1", [128, 8, 128], F32),
                          MT0=T_("MT0", [128, 8, 128]), MT1=T_("MT1", [128, 8, 128]),
                          H32=T_("H32", [128, 4, 512], F32), Hb=T_("Hb", [128, 4, 512]), yt0=T_("yt0", [128, 512], F32), yt1=T_("yt1", [128, 512], F32),
                          ti0=T_("ti0", [128, 512], F32), ti1=T_("ti1", [128, 512], F32))
                S.op("pool", lambda e, bb=bb: e.memset(bb["H32"][:], 0.0), writes=["H32_%d" % d])
                S.op("pool", lambda e, bb=bb: e.memset(bb["Hb"][:], 0.0), writes=["Hb_%d" % d])
                B.append(bb)

            def k_(nm, d):
                return "s2%s_%d" % (nm, d)

            for step in range(nsteps):
                info = []
                for d in range(2):
                    c = step if d == 0 else ((1 - step) if step < 2 else (35 - step))
                    info.append((d, c, c * 128, B[d]))
                for d, c, t0, bb in info:
                    S.dma("sp", bb["XT"][:], XT_s[t0:t0 + 128, :].rearrange("t (h p) -> t h p", h=32), writes=[k_("XT", d)])
                    S.dma("sp", bb["BT"][:], BT_s[t0:t0 + 128, :], writes=[k_("BT", d)])
                    S.dma("sp", bb["BF"][:], BF_s[:, :, t0:t0 + 128].rearrange("g n t -> n g t"), writes=[k_("BF", d)])
                    if c >= 2:
                        S.dma("sp", bb["CF"][:], CF_s[:, :, t0:t0 + 128].rearrange("g n t -> n g t"), writes=[k_("CF", d)])
                    S.dma("sp", bb["DT"][:], DT_s[t0:t0 + 128, d * 32:(d + 1) * 32], writes=[k_("DT", d)])
                    S.dma("sp", bb["DA"][:], DA_s[t0:t0 + 128, d * 32:(d + 1) * 32], writes=[k_("DA", d)])
                for d, c, t0, bb in info:
                    mi = 1 if d == 0 else 3
                    pc, pck = nextps_()
                    S.op("pe", lambda e, bb=bb, pc=pc, mi=mi: e.matmul(pc[:, 0:32], msk[:, mi, :], bb["DA"][:], start=True, stop=True), reads=["msk", k_("DA", d)], writes=[pck])
                    S.op("pe", lambda e, bb=bb, pc=pc: e.matmul(pc[:, 32:64], ones128[:], bb["DA"][:], start=True, stop=True), reads=["ones128", k_("DA", d)], writes=[pck])
                    S.op("act", lambda e, bb=bb, pc=pc: e.copy(bb["cum"][:], pc[:, 0:32]), reads=[pck], writes=[k_("cum", d)])
                    S.op("dve", lambda e, bb=bb: e.tensor_scalar(bb["ncum"][:], bb["cum"][:], -1.0, None, ALU.mult), reads=[k_("cum", d)], writes=[k_("ncum", d)])
                    S.op("act", lambda e, bb=bb: e.activation(bb["Ecum"][:], bb["cum"][:], AF.Exp), reads=[k_("cum", d)], writes=[k_("Ecum", d)])
                    S.op("dve", lambda e, bb=bb, pc=pc: e.tensor_tensor(bb["Gd"][:], pc[:, 32:64], bb["cum"][:], ALU.subtract), reads=[pck, k_("cum", d)], writes=[k_("Gd", d)])
                    S.op("act", lambda e, bb=bb: e.activation(bb["Gd"][:], bb["Gd"][:], AF.Exp), reads=[k_("Gd", d)], writes=[k_("Gd", d)])
                    S.op("act", lambda e, bb=bb, pc=pc: e.activation(bb["Etot"][:], pc[:, 32:64], AF.Exp), reads=[pck], writes=[k_("Etot", d)])
                    S.op("dve", lambda e, bb=bb: e.tensor_tensor(bb["xdt"][:], bb["XT"][:], bb["DT"][:].unsqueeze(2).to_broadcast([128, 32, 64]), ALU.mult), reads=[k_("XT", d), k_("DT", d)], writes=[k_("xdt", d)])
                    S.op("dve", lambda e, bb=bb: e.tensor_tensor(bb["xs"][:], bb["xdt"][:], bb["Gd"][:].unsqueeze(2).to_broadcast([128, 32, 64]), ALU.mult), reads=[k_("xdt", d), k_("Gd", d)], writes=[k_("xs", d)])
                    if c >= 2:
                        S.op("dve", lambda e, bb=bb, mi=mi: e.tensor_tensor(bb["rhsA"][:], msk[:, mi, :].unsqueeze(1).to_broadcast([128, 32, 128]), bb["DA"][:].unsqueeze(2).to_broadcast([128, 32, 128]), ALU.mult), reads=["msk", k_("DA", d)], writes=[k_("rhsA", d)])
                        pcb, pcbk = nextps_()
                        for g in range(4):
                            S.op("pe", lambda e, bb=bb, g=g, pcb=pcb: e.matmul(pcb[:, g * 128:(g + 1) * 128], bb["BF"][:, g, :], bb["CF"][:, g, :], start=True, stop=True), reads=[k_("BF", d), k_("CF", d)], writes=[pcbk])
                        S.op("act", lambda e, bb=bb, pcb=pcb: e.copy(bb["CBT"][:], pcb[:].rearrange("p (g t) -> p g t", g=4)), reads=[pcbk], writes=[k_("CBT", d)])
                for g in range(4):
                    for d, c, t0, bb in info:
                        if c < 2:
                            continue
                        sgn, mtn, ytn, tin = "seg%d" % (g % 2), "MT%d" % (g % 2), "yt%d" % (g % 2), "ti%d" % (g % 2)
                        for half in range(2):
                            p4, p4k = nextps_()
                            for e4 in range(4):
                                h = g * 8 + half * 4 + e4
                                o_ = p4[:, e4 * 128:(e4 + 1) * 128]
                                S.op("pe", lambda e, bb=bb, h=h, o_=o_: e.matmul(o_, ones128[:], bb["rhsA"][:, h, :], start=True, stop=False), reads=["ones128", k_("rhsA", d)], writes=[p4k])
                                S.op("pe", lambda e, d=d, o_=o_: e.matmul(o_, ident32[:], negm[:, d, :], start=False, stop=True), reads=["ident32", "negm"], writes=[p4k])
                            for e4 in range(4):
                                h = g * 8 + half * 4 + e4
                                S.op("act", lambda e, bb=bb, h=h, e4=e4, half=half, p4=p4, sgn=sgn: e.activation(bb[sgn][:, half * 4 + e4, :], p4[:, e4 * 128:(e4 + 1) * 128], AF.Exp, bias=bb["ncum"][:, h:h + 1]),
                                     reads=[p4k, k_("ncum", d)], writes=[k_(sgn, d)])
                        S.op("dve", lambda e, bb=bb, g=g, sgn=sgn, mtn=mtn: e.tensor_tensor(bb[mtn][:], bb[sgn][:], bb["CBT"][:, g, :].unsqueeze(1).to_broadcast([128, 8, 128]), ALU.mult),
                             reads=[k_(sgn, d), k_("CBT", d)], writes=[k_(mtn, d)])
                        py, pyk = nextps_(); pi_, pik = nextps_()
                        for e8 in range(8):
                            S.op("pe", lambda e, bb=bb, g=g, e8=e8, py=py, mtn=mtn: e.matmul(py[:, e8 * 64:(e8 + 1) * 64], bb[mtn][:, e8, :], bb["xdt"][:, g * 8 + e8, :], start=True, stop=True),
                                 reads=[k_(mtn, d), k_("xdt", d)], writes=[pyk])
                        S.op("pe", lambda e, bb=bb, g=g, pi_=pi_: e.matmul(pi_[:], bb["CF"][:, g, :], bb["Hb"][:, g, :], start=True, stop=True), reads=[k_("CF", d), k_("Hb", d)], writes=[pik])
                        S.op("dve", lambda e, bb=bb, g=g, pi_=pi_, tin=tin: e.tensor_tensor(bb[tin][:].rearrange("p (h q) -> p h q", h=8), pi_[:].rearrange("p (h q) -> p h q", h=8), bb["Ecum"][:, g * 8:(g + 1) * 8].unsqueeze(2).to_broadcast([128, 8, 64]), ALU.mult),
                             reads=[pik, k_("Ecum", d)], writes=[k_(tin, d)])
                        S.op("dve", lambda e, bb=bb, py=py, tin=tin, ytn=ytn: e.tensor_tensor(bb[ytn][:], py[:], bb[tin][:], ALU.add), reads=[pyk, k_(tin, d)], writes=[k_(ytn, d)])
                        l0 = t0 - CTX
                        S.dma("pool", YD_s[d, l0:l0 + 128, g * 512:(g + 1) * 512], bb[ytn][:], reads=[k_(ytn, d)])
                for g in range(4):
                    for d, c, t0, bb in info:
                        ph, phk = nextps_()
                        S.op("pe", lambda e, bb=bb, g=g, ph=ph: e.matmul(ph[:], bb["BT"][:, g * 128:(g + 1) * 128], bb["xs"][:, g * 8:(g + 1) * 8, :].rearrange("p h q -> p (h q)"), start=True, stop=True),
                             reads=[k_("BT", d), k_("xs", d)], writes=[phk])
                        S.op("dve", lambda e, bb=bb, g=g: e.tensor_tensor(bb["H32"][:, g, :].rearrange("p (h q) -> p h q", h=8), bb["H32"][:, g, :].rearrange("p (h q) -> p h q", h=8), bb["Etot"][:, g * 8:(g + 1) * 8].unsqueeze(2).to_broadcast([128, 8, 64]), ALU.mult),
                             reads=[k_("H32", d), k_("Etot", d)], writes=[k_("H32", d)])
                        S.op("dve", lambda e, bb=bb, g=g, ph=ph: e.tensor_tensor(bb["H32"][:, g, :], bb["H32"][:, g, :], ph[:], ALU.add), reads=[phk, k_("H32", d)], writes=[k_("H32", d)])
                        S.op("act", lambda e, bb=bb, g=g: e.copy(bb["Hb"][:, g, :], bb["H32"][:, g, :]), reads=[k_("H32", d)], writes=[k_("Hb", d)])
            S.barrier()

    if dbg == "S2":
        s2_scan(6)
        o = dout("dbg", [2, 512, 2048])
        S.dma("sp", o[0], YD_s[0, 0:512, :], reads=[])
        S.dma("sp", o[1], YD_s[1, SEQ - 512:SEQ, :], reads=[])
        S.finish()
        return
    s2_scan()

    YA_s = nc.dram_tensor("rw_ya", [1024, SEQ], BF16).ap()
    with contextlib.ExitStack() as st:
        lnw = sb("lnw", [64, 16], st=st); lnb = sb("lnb", [64, 16], st=st)
        S.dma("sp", lnw[:], rwlnw_d.rearrange("(h v) -> v h", v=64), writes=["lnw"], allow_slow_non_contiguous=True)
        S.dma("sp", lnb[:], rwlnb_d.rearrange("(h v) -> v h", v=64), writes=["lnb"], allow_slow_non_contiguous=True)
        gneps = sb("gneps", [64, 1], st=st)
        S.op("dve", lambda e: e.memset(gneps[:], 64e-5), writes=["gneps"])
        ones64 = sb("ones64", [64, 64], st=st)
        S.op("dve", lambda e: e.memset(ones64[:], 1.0), writes=["ones64"])
        def T2(nm, dt=F32):
            return [sb("r3%s_%d" % (nm, i), [64, 4, 128], dt, st=st) for i in range(2)]
        yf_, yb_, ysq_, mm_, vv_, yo_ = T2("yf"), T2("yb"), T2("ysq"), T2("mm"), T2("vv"), T2("yo", BF16)
        bvt_, gt_ = T2("bvt", BF16), T2("gt", BF16)
        it = 0
        for hg in range(1 if dbg == "R3" else 4):
            for blk in range(SEQ // 128):
                b = it % 2; it += 1
                sfx = "_%d" % b
                l0 = blk * 128
                yf, yb, ysq, mm, vv, yo, bvt, gt = (x[b] for x in (yf_, yb_, ysq_, mm_, vv_, yo_, bvt_, gt_))
                S.dma("sp", yf[:], YS[0, hg * 4:hg * 4 + 4, :, l0:l0 + 128].rearrange("h v t -> v h t"), writes=["yf" + sfx])
                S.dma("sp", yb[:], YS[1, hg * 4:hg * 4 + 4, :, l0:l0 + 128].rearrange("h v t -> v h t"), writes=["yb" + sfx])
                S.dma("sp", bvt[:], BV_s[hg * 256:(hg + 1) * 256, l0:l0 + 128].rearrange("(h v) t -> v h t", v=64), writes=["bvt" + sfx])
                S.dma("sp", gt[:], G_s[hg * 256:(hg + 1) * 256, l0:l0 + 128].rearrange("(h v) t -> v h t", v=64), writes=["gt" + sfx])
                S.op("dve", lambda e: e.tensor_tensor(yf[:], yf[:], yb[:], ALU.add), reads=["yf" + sfx, "yb" + sfx], writes=["yf" + sfx])
                S.op("act", lambda e: e.activation(ysq[:], yf[:], AF.Square), reads=["yf" + sfx], writes=["ysq" + sfx])
                p1, p1k = nextps(); p2, p2k = nextps()
                S.op("pe", lambda e: e.matmul(p1[0:64, :], ones64[:], yf[:].rearrange("p h t -> p (h t)"), start=True, stop=True), reads=["ones64", "yf" + sfx], writes=[p1k])
                S.op("pe", lambda e: e.matmul(p2[0:64, :], ones64[:], ysq[:].rearrange("p h t -> p (h t)"), start=True, stop=True), reads=["ones64", "ysq" + sfx], writes=[p2k])
                v3 = lambda t: t[0:64, :].rearrange("p (h t) -> p h t", h=4)
                S.op("act", lambda e: e.activation(mm[:], v3(p1), AF.Identity, scale=1.0 / 64), reads=[p1k], writes=["mm" + sfx])
                S.op("dve", lambda e: e.tensor_tensor(vv[:], mm[:], mm[:], ALU.mult), reads=["mm" + sfx], writes=["vv" + sfx])
                S.op("dve", lambda e: e.scalar_tensor_tensor(vv[:], v3(p2), 1.0 / 64, vv[:], ALU.mult, ALU.subtract), reads=[p2k, "vv" + sfx], writes=["vv" + sfx])
                S.op("act", lambda e: e.activation(vv[:], vv[:], AF.Sqrt, bias=gneps[:]), reads=["vv" + sfx, "gneps"], writes=["vv" + sfx])
                S.op("dve", lambda e: e.reciprocal(vv[:], vv[:]), reads=["vv" + sfx], writes=["vv" + sfx])
                S.op("dve", lambda e: e.tensor_tensor(yf[:], yf[:], mm[:], ALU.subtract), reads=["yf" + sfx, "mm" + sfx], writes=["yf" + sfx])
                S.op("dve", lambda e: e.tensor_tensor(yf[:], yf[:], vv[:], ALU.mult), reads=["yf" + sfx, "vv" + sfx], writes=["yf" + sfx])
                S.op("dve", lambda e: e.tensor_tensor(yf[:], yf[:], lnw[:, hg * 4:hg * 4 + 4].unsqueeze(2).to_broadcast([64, 4, 128]), ALU.mult), reads=["yf" + sfx, "lnw"], writes=["yf" + sfx])
                S.op("dve", lambda e: e.tensor_tensor(yf[:], yf[:], lnb[:, hg * 4:hg * 4 + 4].unsqueeze(2).to_broadcast([64, 4, 128]), ALU.add), reads=["yf" + sfx, "lnb"], writes=["yf" + sfx])
                S.op("dve", lambda e: e.tensor_tensor(yf[:], yf[:], bvt[:], ALU.add), reads=["yf" + sfx, "bvt" + sfx], writes=["yf" + sfx])
                S.op("dve", lambda e: e.tensor_tensor(yo[:], yf[:], gt[:], ALU.mult), reads=["yf" + sfx, "gt" + sfx], writes=["yo" + sfx])
                S.dma("pool", YA_s[hg * 256:(hg + 1) * 256, l0:l0 + 128].rearrange("(h v) t -> v h t", v=64), yo[:], reads=["yo" + sfx])
        S.barrier()
    if dbg == "R3":
        o = dout("dbg", [256, SEQ], BF16)
        S.dma("sp", o, YA_s[0:256, :], reads=[])
        S.finish()
        return

    ssmd_d = din("ssm_d", [32]); ssmn_d = din("ssm_norm", [2048]); projb_d = din("proj_b", [2048, 1024])
    proja_d = din("proj_a", [1024, 1024]); wout_d = din("w_out", [1024, 1024])
    n1post_d = din("norm1_post", [D]); n2pre_d = din("norm2_pre", [D]); n2post_d = din("norm2_post", [D])
    router_d = din("router", [D, 16])
    PBG_s = nc.dram_tensor("mg_pbg", [SEQ, 1024], BF16).ap()
    X1_s = nc.dram_tensor("x1_s", [SEQ, D], F32).ap()
    H2T_s = nc.dram_tensor("h2t_s", [D, SEQ], BF16).ap()
    AFT_s = nc.dram_tensor("aft_s", [16, SEQ], F32).ap()

    def rstd_of(ssq, n, key):
        S.op("act", lambda e: e.activation(ssq, ssq, AF.Sqrt, bias=epsc[:], scale=1.0 / n), reads=[key, "epsc"], writes=[key])
        S.op("dve", lambda e: e.reciprocal(ssq, ssq), reads=[key], writes=[key])

    with contextlib.ExitStack() as st:
        pbw = sb("pbw", [128, 16, 1024], BF16, st=st)
        stg = [sb("pbstg%d" % i, [128, 2, 1024], st=st) for i in range(2)]
        for q_ in range(8):
            S.dma("sp", stg[q_ % 2][:], projb_d[q_ * 256:(q_ + 1) * 256, :].rearrange("(cc p) d -> p cc d", p=128), writes=["pbstg%d" % (q_ % 2)])
            S.op("pool", lambda e, q_=q_: e.tensor_copy(pbw[:, 2 * q_:2 * q_ + 2, :], stg[q_ % 2][:]), reads=["pbstg%d" % (q_ % 2)], writes=["pbw"])
        Dbc = sb("Dbc", [128, 32], st=st); snb = sb("snb", [128, 2048], st=st)
        S.dma("sp", Dbc[:], ssmd_d.partition_broadcast(128), writes=["Dbc"])
        S.dma("sp", snb[:], ssmn_d.partition_broadcast(128), writes=["snb"])
        yf_ = [sb("s3yf%d" % i, [128, 2048], st=st) for i in range(2)]
        yb_ = [sb("s3yb%d" % i, [128, 2048], st=st) for i in range(2)]
        xt_ = [sb("s3xt%d" % i, [128, 2048], BF16, st=st) for i in range(2)]
        zs_ = [sb("s3zs%d" % i, [128, 2048], BF16, st=st) for i in range(2)]
        gb_ = [sb("s3gb%d" % i, [128, 1024], BF16, st=st) for i in range(2)]
        pbg_ = [sb("s3pbg%d" % i, [128, 1024], BF16, st=st) for i in range(2)]
        s3junk = sb("s3junk", [128, 2048], st=st)
        ynb = sb("s3ynb", [128, 2048], BF16, st=st); ynT = sb("s3ynT", [128, 16, 128], BF16, st=st)
        ssq_ = [sb("s3ss%d" % i, [128, 1], st=st) for i in range(2)]
        PBG_v = PBG_s.rearrange("(r c) d -> c r d", c=64)
        for tt in range(32 if dbg != "S3" else 2):
            b = tt % 2
            sfx = "%d" % b
            yf, yb, xt, zs, gbt, pbg, ssq = yf_[b], yb_[b], xt_[b], zs_[b], gb_[b], pbg_[b], ssq_[b]
            S.dma("sp", yf[:], YD_s[0, tt * 128:(tt + 1) * 128, :], writes=["s3yf" + sfx])
            S.dma("sp", yb[:], YD_s[1, tt * 128:(tt + 1) * 128, :], writes=["s3yb" + sfx])
            S.dma("sp", xt[:], XT_s[CTX + tt * 128:CTX + (tt + 1) * 128, :], writes=["s3xt" + sfx])
            S.dma("sp", zs[:], ZS_s[tt * 128:(tt + 1) * 128, :], writes=["s3zs" + sfx])
            S.dma("sp", gbt[:], GB_s[tt * 128:(tt + 1) * 128, :], writes=["s3gb" + sfx])
            S.op("dve", lambda e: e.tensor_tensor(yf[:], yf[:], yb[:], ALU.add), reads=["s3yf" + sfx, "s3yb" + sfx], writes=["s3yf" + sfx])
            S.op("dve", lambda e: e.tensor_tensor(yb[:].rearrange("p (h q) -> p h q", h=32), xt[:].rearrange("p (h q) -> p h q", h=32), Dbc[:].unsqueeze(2).to_broadcast([128, 32, 64]), ALU.mult),
                 reads=["s3xt" + sfx, "Dbc", "s3yb" + sfx], writes=["s3yb" + sfx])
            S.op("dve", lambda e: e.tensor_tensor(yf[:], yf[:], yb[:], ALU.add), reads=["s3yf" + sfx, "s3yb" + sfx], writes=["s3yf" + sfx])
            S.op("dve", lambda e: e.tensor_tensor(yf[:], yf[:], zs[:], ALU.mult), reads=["s3yf" + sfx, "s3zs" + sfx], writes=["s3yf" + sfx])
            S.op("act", lambda e: e.activation(s3junk[:], yf[:], AF.Square, accum_out=ssq[:]), reads=["s3yf" + sfx], writes=["s3junk", "s3ss" + sfx])
            rstd_of(ssq[:], 2048, "s3ss" + sfx)
            S.op("dve", lambda e: e.scalar_tensor_tensor(ynb[:], yf[:], ssq[:], snb[:], ALU.mult, ALU.mult), reads=["s3yf" + sfx, "s3ss" + sfx, "snb"], writes=["s3ynb"])
            for hh in range(2):
                for j in range(8):
                    cc = hh * 8 + j
                    S.op("pe", lambda e, cc=cc, j=j, hh=hh: e.transpose(PSB[hh][:, j * 128:(j + 1) * 128], ynb[:, cc * 128:(cc + 1) * 128], identb[:]), reads=["s3ynb", "identb"], writes=["PSB%d" % hh])
                S.op("act", lambda e, hh=hh: e.copy(ynT[:, hh * 8:(hh + 1) * 8, :], PSB[hh][:].rearrange("p (j t) -> p j t", j=8)), reads=["PSB%d" % hh], writes=["s3ynT"])
            for half in range(2):
                pt, pk = nextps_()
                for cc in range(16):
                    S.op("pe", lambda e, cc=cc, half=half, pt=pt: e.matmul(pt[:], ynT[:, cc, :], pbw[:, cc, half * 512:(half + 1) * 512], start=(cc == 0), stop=(cc == 15)), reads=["s3ynT", "pbw"], writes=[pk])
                S.op("dve", lambda e, half=half, pt=pt: e.tensor_tensor(pbg[:, half * 512:(half + 1) * 512], pt[:], gbt[:, half * 512:(half + 1) * 512], ALU.mult), reads=[pk, "s3gb" + sfx], writes=["s3pbg" + sfx])
            for j in range(2):
                S.dma("pool", PBG_v[2 * tt + j], pbg[j * 64:(j + 1) * 64, :], reads=["s3pbg" + sfx])
        S.barrier()
    if dbg == "S3":
        o = dout("dbg", [4, 64, 1024], BF16)
        S.dma("sp", o, PBG_v[0:4], reads=[])
        S.finish()
        return

    with contextlib.ExitStack() as st:
        def load_sq(name, src):
            wbf = sb(name, [128, 8, 1024], BF16, st=st)
            for q_ in range(4):
                S.dma("sp", stg2[q_ % 2][:], src[q_ * 256:(q_ + 1) * 256, :].rearrange("(cc p) d -> p cc d", p=128), writes=["mstg%d" % (q_ % 2)])
                S.op("pool", lambda e, q_=q_: e.tensor_copy(wbf[:, 2 * q_:2 * q_ + 2, :], stg2[q_ % 2][:]), reads=["mstg%d" % (q_ % 2)], writes=[name])
            return wbf
        stg2 = [sb("mstg%d" % i, [128, 2, 1024], st=st) for i in range(2)]
        paw = load_sq("paw", proja_d); wow = load_sq("wow", wout_d)
        rtr = sb("rtr", [128, 8, 16], st=st)
        S.dma("sp", rtr[:], router_d.rearrange("(k p) e -> p k e", p=128), writes=["rtr"])
        nb = sb("nbc", [128, 3, D], st=st)
        for i_, src in enumerate((n1post_d, n2pre_d, n2post_d)):
            S.dma("sp", nb[:, i_, :], src.partition_broadcast(128), writes=["nbc"])
        S.op("dve", lambda e: e.tensor_tensor(modL[:, 2 * D:3 * D], modL[:, 2 * D:3 * D], nb[:, 0, :], ALU.mult), reads=["nbc", "mod"], writes=["mod"])
        S.op("dve", lambda e: e.scalar_tensor_tensor(modL[:, 4 * D:5 * D], modL[:, 4 * D:5 * D], 1.0, nb[:, 1, :], ALU.add, ALU.mult), reads=["nbc", "mod"], writes=["mod"])
        S.op("dve", lambda e: e.tensor_tensor(modL[:, 5 * D:6 * D], modL[:, 5 * D:6 * D], nb[:, 2, :], ALU.mult), reads=["nbc", "mod"], writes=["mod"])
        yat_ = [sb("myat%d" % i, [128, 8, 128], BF16, st=st) for i in range(2)]
        gat_ = [sb("mgat%d" % i, [128, 1024], BF16, st=st) for i in range(2)]
        pbt_ = [sb("mpbt%d" % i, [128, 1024], BF16, st=st) for i in range(2)]
        xin_ = [sb("mxin%d" % i, [128, 1024], st=st) for i in range(2)]
        mrg = sb("mmrg", [128, 1024], BF16, st=st); mrgT = sb("mmrgT", [128, 8, 128], BF16, st=st)
        tmpm = sb("mtmp", [128, 1024], st=st); ml = sb("mml", [128, 1024], st=st); mjunk = sb("mjunk", [128, 1024], st=st)
        x1_ = [sb("mx1%d" % i, [128, 1024], st=st) for i in range(2)]
        h2 = sb("mh2", [128, 1024], st=st); h2T32 = sb("mh2T32", [128, 8, 128], st=st); h2Tb_ = [sb("mh2Tb%d" % i, [128, 8, 128], BF16, st=st) for i in range(2)]
        lg = sb("mlg", [128, 16], st=st); lgs = sb("mlgs", [128, 1], st=st); affT_ = [sb("maffT%d" % i, [16, 128], st=st) for i in range(2)]
        ssa = sb("mssa", [128, 1], st=st); ssb = sb("mssb", [128, 1], st=st)
        YA_v = YA_s.rearrange("(cc p) t -> p cc t", p=128)
        H2T_v = H2T_s.rearrange("(k p) t -> p k t", p=128)
        for tt in range(32):
            b = tt % 2
            sfx = "%d" % b
            t0 = tt * 128
            yat, gat, pbt, xin, x1, h2Tb, affT = yat_[b], gat_[b], pbt_[b], xin_[b], x1_[b], h2Tb_[b], affT_[b]
            S.dma("sp", yat[:], YA_v[:, :, t0:t0 + 128], writes=["myat" + sfx])
            S.dma("sp", gat[:], GA_s[t0:t0 + 128, :], writes=["mgat" + sfx])
            S.dma("sp", pbt[:], PBG_s[t0:t0 + 128, :], writes=["mpbt" + sfx])
            S.dma("sp", xin[:], x_d[t0:t0 + 128, :], writes=["mxin" + sfx])
            for half in range(2):
                pt, pk = nextps_()
                for cc in range(8):
                    S.op("pe", lambda e, cc=cc, half=half, pt=pt: e.matmul(pt[:], yat[:, cc, :], paw[:, cc, half * 512:(half + 1) * 512], start=(cc == 0), stop=(cc == 7)), reads=["myat" + sfx, "paw"], writes=[pk])
                hs = slice(half * 512, (half + 1) * 512)
                S.op("dve", lambda e, pt=pt, hs=hs: e.tensor_tensor(tmpm[:, hs], pt[:], gat[:, hs], ALU.mult), reads=[pk, "mgat" + sfx], writes=["mtmp"])
                S.op("dve", lambda e, hs=hs: e.tensor_tensor(mrg[:, hs], tmpm[:, hs], pbt[:, hs], ALU.add), reads=["mtmp", "mpbt" + sfx], writes=["mmrg"])
            for j in range(8):
                S.op("pe", lambda e, j=j: e.transpose(PSB[0][:, j * 128:(j + 1) * 128], mrg[:, j * 128:(j + 1) * 128], identb[:]), reads=["mmrg", "identb"], writes=["PSB0"])
            S.op("act", lambda e: e.copy(mrgT[:], PSB[0][:].rearrange("p (j t) -> p j t", j=8)), reads=["PSB0"], writes=["mmrgT"])
            for half in range(2):
                pt, pk = nextps_()
                for k in range(8):
                    S.op("pe", lambda e, k=k, half=half, pt=pt: e.matmul(pt[:], mrgT[:, k, :], wow[:, k, half * 512:(half + 1) * 512], start=(k == 0), stop=(k == 7)), reads=["mmrgT", "wow"], writes=[pk])
                S.op("act", lambda e, half=half, pt=pt: e.copy(ml[:, half * 512:(half + 1) * 512], pt[:]), reads=[pk], writes=["mml"])
            S.op("act", lambda e: e.activation(mjunk[:], ml[:], AF.Square, accum_out=ssa[:]), reads=["mml"], writes=["mjunk", "mssa"])
            rstd_of(ssa[:], D, "mssa")
            S.op("dve", lambda e: e.scalar_tensor_tensor(tmpm[:], ml[:], ssa[:], modL[:, 2 * D:3 * D], ALU.mult, ALU.mult), reads=["mml", "mssa", "mod"], writes=["mtmp"])
            S.op("dve", lambda e: e.tensor_tensor(x1[:], tmpm[:], xin[:], ALU.add), reads=["mtmp", "mxin" + sfx], writes=["mx1" + sfx])
            S.dma("pool", X1_s[t0:t0 + 128, :], x1[:], reads=["mx1" + sfx])
            S.op("act", lambda e: e.activation(mjunk[:], x1[:], AF.Square, accum_out=ssb[:]), reads=["mx1" + sfx], writes=["mjunk", "mssb"])
            rstd_of(ssb[:], D, "mssb")
            S.op("dve", lambda e: e.scalar_tensor_tensor(h2[:], x1[:], ssb[:], modL[:, 4 * D:5 * D], ALU.mult, ALU.mult), reads=["mx1" + sfx, "mssb", "mod"], writes=["mh2"])
            S.op("dve", lambda e: e.tensor_tensor(h2[:], h2[:], modL[:, 3 * D:4 * D], ALU.add), reads=["mh2", "mod"], writes=["mh2"])
            for hh in range(2):
                pt, pk = nextps_()
                for j in range(4):
                    kk_ = hh * 4 + j
                    S.op("pe", lambda e, j=j, kk_=kk_, pt=pt: e.transpose(pt[:, j * 128:(j + 1) * 128], h2[:, kk_ * 128:(kk_ + 1) * 128], ident32[:]), reads=["mh2", "ident32"], writes=[pk])
                S.op("act", lambda e, hh=hh, pt=pt: e.copy(h2T32[:, hh * 4:(hh + 1) * 4, :], pt[:].rearrange("p (j t) -> p j t", j=4)), reads=[pk], writes=["mh2T32"])
            S.op("dve", lambda e: e.tensor_copy(h2Tb[:], h2T32[:]), reads=["mh2T32"], writes=["mh2Tb" + sfx])
            S.dma("pool", H2T_v[:, :, t0:t0 + 128], h2Tb[:], reads=["mh2Tb" + sfx])
            pt, pk = nextps_()
            for k in range(8):
                S.op("pe", lambda e, k=k, pt=pt: e.matmul(pt[:, 0:16], h2T32[:, k, :], rtr[:, k, :], start=(k == 0), stop=(k == 7)), reads=["mh2T32", "rtr"], writes=[pk])
            S.op("act", lambda e, pt=pt: e.activation(lg[:], pt[:, 0:16], AF.Exp, accum_out=lgs[:]), reads=[pk], writes=["mlg", "mlgs"])
            S.op("dve", lambda e: e.reciprocal(lgs[:], lgs[:]), reads=["mlgs"], writes=["mlgs"])
            S.op("dve", lambda e: e.tensor_scalar(lg[:], lg[:], lgs[:], None, ALU.mult), reads=["mlg", "mlgs"], writes=["mlg"])
            pt2, pk2 = nextps_()
            S.op("pe", lambda e, pt2=pt2: e.transpose(pt2[0:16, 0:128], lg[:], ident32[:]), reads=["mlg", "ident32"], writes=[pk2])
            S.op("act", lambda e, pt2=pt2: e.copy(affT[:], pt2[0:16, 0:128]), reads=[pk2], writes=["maffT" + sfx])
            S.dma("pool", AFT_s[:, t0:t0 + 128], affT[:], reads=["maffT" + sfx])
        S.barrier()
    if dbg == "M":
        o = dout("dbgX1", [512, D]); S.dma("sp", o, X1_s[1024:1536], reads=[])
        o = dout("dbgH2", [D, 512], BF16); S.dma("sp", o, H2T_s[:, 1024:1536], reads=[])
        o = dout("dbgAF", [16, SEQ]); S.dma("sp", o, AFT_s, reads=[])
        S.finish()
        return

    w1_d = din("exp_w1", [16, D, 1024]); w3_d = din("exp_w3", [16, D, 1024]); w2_d = din("exp_w2", [16, 1024, D])
    selm_d = din("selm", [16, 16, 128])
    GM_s = nc.dram_tensor("gm_s", [16, SEQ], F32).ap()
    CAP = 2 * SEQ // 16
    with contextlib.ExitStack() as st:
        aff = sb("eaff", [16, SEQ], st=st); cmp_ = sb("ecmp", [16, SEQ], st=st)
        S.dma("sp", aff[:], AFT_s, writes=["eaff"])
        lo = sb("elo", [16, 1], st=st); hi = sb("ehi", [16, 1], st=st); mid = sb("emid", [16, 1], st=st)
        cnt = sb("ecnt", [16, 1], st=st); sel = sb("esel", [16, 1], st=st); dl = sb("edl", [16, 1], st=st); halfc = sb("ehalf", [16, 1], st=st)
        S.op("dve", lambda e: e.memset(lo[:], 0.0), writes=["elo"])
        S.op("dve", lambda e: e.memset(hi[:], 1.0), writes=["ehi"])
        S.op("dve", lambda e: e.memset(halfc[:], 0.5), writes=["ehalf"])
        for it in range(36):
            S.op("dve", lambda e: e.scalar_tensor_tensor(mid[:], lo[:], hi[:, 0:1], halfc[:], ALU.add, ALU.mult), reads=["elo", "ehi", "ehalf"], writes=["emid"])
            S.op("dve", lambda e: e.tensor_scalar(cmp_[:], aff[:], mid[:, 0:1], None, ALU.is_ge), reads=["eaff", "emid"], writes=["ecmp"])
            S.op("dve", lambda e: e.reduce_sum(cnt[:], cmp_[:], AX.X), reads=["ecmp"], writes=["ecnt"])
            S.op("dve", lambda e: e.tensor_scalar(sel[:], cnt[:], CAP - 0.5, None, ALU.is_ge), reads=["ecnt"], writes=["esel"])
            S.op("dve", lambda e: e.tensor_tensor(dl[:], mid[:], lo[:], ALU.subtract), reads=["emid", "elo"], writes=["edl"])
            S.op("dve", lambda e: e.scalar_tensor_tensor(lo[:], dl[:], sel[:, 0:1], lo[:], ALU.mult, ALU.add), reads=["edl", "esel", "elo"], writes=["elo"])
            S.op("dve", lambda e: e.tensor_tensor(dl[:], hi[:], mid[:], ALU.subtract), reads=["emid", "ehi"], writes=["edl"])
            S.op("dve", lambda e: e.scalar_tensor_tensor(hi[:], dl[:], sel[:, 0:1], mid[:], ALU.mult, ALU.add), reads=["edl", "esel", "emid"], writes=["ehi"])
        S.op("dve", lambda e: e.tensor_scalar(cmp_[:], aff[:], lo[:, 0:1], None, ALU.is_ge), reads=["eaff", "elo"], writes=["ecmp"])
        S.op("dve", lambda e: e.tensor_tensor(cmp_[:], cmp_[:], aff[:], ALU.mult), reads=["ecmp", "eaff"], writes=["ecmp"])
        S.dma("sp", GM_s, cmp_[:], reads=["ecmp"])
        S.barrier()
    if dbg == "E0":
        o = dout("dbg", [16, SEQ]); S.dma("sp", o, GM_s, reads=[])
        S.finish()
        return

    with contextlib.ExitStack() as st:
        selm = sb("selm_sb", [16, 16, 128], st=st)
        S.dma("sp", selm[:], selm_d, writes=["selm"])
        QT = 1024
        h2q = sb("eh2q", [128, 8, QT], BF16, st=st)
        yacc = sb("eyacc", [128, 8, QT], st=st)
        gmq = sb("egmq", [16, QT], st=st)
        wb = [sb("ew%d" % i, [128, 8, 1024], BF16, st=st) for i in range(3)]
        stg3 = [sb("estg%d" % i, [128, 2, 1024], st=st) for i in range(2)]
        hg = sb("ehg", [128, 8, 512], BF16, st=st)
        sa_ = [sb("esa%d" % i, [128, 512], st=st) for i in range(2)]
        hb_ = [sb("ehb%d" % i, [128, 512], st=st) for i in range(2)]
        gmbs = sb("egmbs", [128, 512], st=st)
        ytok = sb("eytok", [128, D], st=st); x1t = sb("ex1t", [128, D], st=st); ejunk = sb("ejunk", [128, D], st=st)
        ess = sb("eess", [128, 1], st=st)
        nstg = [0]
        NQ = SEQ // QT
        for qi in range(NQ if dbg != "E1" else 1):
            T0 = qi * QT
            S.dma("sp", h2q[:], H2T_v[:, :, T0:T0 + QT], writes=["eh2q"])
            S.dma("sp", gmq[:], GM_s[:, T0:T0 + QT], writes=["egmq"])
            S.op("pool", lambda e: e.memset(yacc[:], 0.0), writes=["eyacc"])
            for ex in range(16):
                for wi, src in enumerate((w1_d[ex], w3_d[ex], w2_d[ex])):
                    for q_ in range(4):
                        sg_ = nstg[0] % 2; nstg[0] += 1
                        S.dma("sp", stg3[sg_][:], src[q_ * 256:(q_ + 1) * 256, :].rearrange("(cc p) d -> p cc d", p=128), writes=["estg%d" % sg_])
                        eng = "pool" if (q_ % 2 == 0) else "act"
                        if eng == "pool":
                            S.op("pool", lambda e, wi=wi, q_=q_, sg_=sg_: e.tensor_copy(wb[wi][:, 2 * q_:2 * q_ + 2, :], stg3[sg_][:]), reads=["estg%d" % sg_], writes=["ew%d" % wi])
                        else:
                            S.op("act", lambda e, wi=wi, q_=q_, sg_=sg_: e.copy(wb[wi][:, 2 * q_:2 * q_ + 2, :], stg3[sg_][:]), reads=["estg%d" % sg_], writes=["ew%d" % wi])
                for tg in range(QT // 512):
                    tl = tg * 512
                    pg, pgk = nextps_()
                    S.op("pe", lambda e, pg=pg: e.matmul(pg[:], selm[:, ex, :], gmq[:, tl:tl + 512], start=True, stop=True), reads=["selm", "egmq"], writes=[pgk])
                    S.op("act", lambda e, pg=pg: e.copy(gmbs[:], pg[:]), reads=[pgk], writes=["egmbs"])
                    for ft in range(8):
                        b = ft % 2
                        pa, pak = nextps_(); pb, pbk = nextps_()
                        for k in range(8):
                            S.op("pe", lambda e, k=k, pa=pa: e.matmul(pa[:], wb[0][:, k, ft * 128:(ft + 1) * 128], h2q[:, k, tl:tl + 512], start=(k == 0), stop=(k == 7)), reads=["ew0", "eh2q"], writes=[pak])
                        for k in range(8):
                            S.op("pe", lambda e, k=k, pb=pb: e.matmul(pb[:], wb[1][:, k, ft * 128:(ft + 1) * 128], h2q[:, k, tl:tl + 512], start=(k == 0), stop=(k == 7)), reads=["ew1", "eh2q"], writes=[pbk])
                        S.op("act", lambda e, pa=pa: e.activation(sa_[b][:], pa[:], AF.Silu), reads=[pak], writes=["esa%d" % b])
                        S.op("dve", lambda e, pb=pb: e.tensor_tensor(hb_[b][:], sa_[b][:], pb[:], ALU.mult), reads=["esa%d" % b, pbk], writes=["ehb%d" % b])
                        S.op("pool", lambda e: e.tensor_tensor(hg[:, ft, :], hb_[b][:], gmbs[:], ALU.mult), reads=["ehb%d" % b, "egmbs"], writes=["ehg"])
                    for dt_ in range(8):
                        py, pyk = nextps_()
                        for ft in range(8):
                            S.op("pe", lambda e, ft=ft, py=py: e.matmul(py[:], wb[2][:, ft, dt_ * 128:(dt_ + 1) * 128], hg[:, ft, :], start=(ft == 0), stop=(ft == 7)), reads=["ew2", "ehg"], writes=[pyk])
                        S.op("dve", lambda e, py=py: e.tensor_tensor(yacc[:, dt_, tl:tl + 512], yacc[:, dt_, tl:tl + 512], py[:], ALU.add), reads=[pyk, "eyacc"], writes=["eyacc"])
            for tt in range(QT // 128):
                t0 = T0 + tt * 128
                S.dma("sp", x1t[:], X1_s[t0:t0 + 128, :], writes=["ex1t"])
                for hh in range(2):
                    pt, pk = nextps_()
                    for j in range(4):
                        dd = hh * 4 + j
                        S.op("pe", lambda e, j=j, dd=dd, pt=pt: e.transpose(pt[:, j * 128:(j + 1) * 128], yacc[:, dd, tt * 128:(tt + 1) * 128], ident32[:]), reads=["eyacc", "ident32"], writes=[pk])
                    S.op("act", lambda e, hh=hh, pt=pt: e.copy(ytok[:, hh * 512:(hh + 1) * 512], pt[:]), reads=[pk], writes=["eytok"])
                S.op("act", lambda e: e.activation(ejunk[:], ytok[:], AF.Square, accum_out=ess[:]), reads=["eytok"], writes=["ejunk", "eess"])
                rstd_of(ess[:], D, "eess")
                S.op("dve", lambda e: e.scalar_tensor_tensor(ytok[:], ytok[:], ess[:], modL[:, 5 * D:6 * D], ALU.mult, ALU.mult), reads=["eytok", "eess", "mod"], writes=["eytok"])
                S.op("dve", lambda e: e.tensor_tensor(ytok[:], ytok[:], x1t[:], ALU.add), reads=["eytok", "ex1t"], writes=["eytok"])
                if out_d is not None:
                    S.dma("pool", out_d[t0:t0 + 128, :], ytok[:], reads=["eytok"])
                elif dbg == "E1":
                    if tt == 0:
                        dbg_o = dout("dbg", [QT, D])
                    S.dma("pool", dbg_o[tt * 128:(tt + 1) * 128, :], ytok[:], reads=["eytok"])
        S.barrier()

    S.finish()


def _prep_inputs(inputs):
    sq = {}
    for k, v in inputs.items():
        v = np.asarray(v)
        if k in ("x", "c", "ctx", "c_ctx"):
            sq[k] = v
        elif k == "rw_rk":
            sq[k] = v[0].reshape(1024)
        elif k in ("ssm_dt_bias", "ssm_a_log"):
            sq[k] = v[0].reshape(64)
        else:
            sq[k] = v[0]
    return sq


def kernel(**inputs):
    sq = _prep_inputs(inputs)
    nc = build(None)
    cst = consts()
    in_maps = []
    for b in range(8):
        full = dict(sq)
        full["x"] = sq["x"][b]
        full["ctx"] = sq["ctx"][b]
        full["c"] = sq["c"][b]
        full.update(cst)
        in_maps.append({k: np.ascontiguousarray(full[k], dtype=np.float32) for k in INPUT_NAMES})
    res = run_bass_kernel_spmd(nc, in_maps, core_ids=list(range(8)))
    return np.stack([np.asarray(r["out"], dtype=np.float32) for r in res.results], axis=0)
```

```python
import contextlib
import math
import numpy as np
import ml_dtypes
import concourse.bass as bass
import concourse.mybir as mybir
from concourse.bass_utils import run_bass_kernel_spmd

F32 = mybir.dt.float32
BF16 = mybir.dt.bfloat16
AF = mybir.ActivationFunctionType
ALU = mybir.AluOpType
AX = mybir.AxisListType

D = 1024
SEQ = 4096
CTX = 256
NTOK = SEQ + CTX
PADW = NTOK + 4
C0 = 1
L0 = CTX + 3
N_IN = 10688
EPS = 1e-6

SEM_CAP = 24000
NDMA_SEM = 12


class Sched:
    def __init__(self, nc, stack):
        self.nc = nc
        self.stack = stack
        self.engs = {"pe": nc.tensor, "dve": nc.vector, "act": nc.scalar, "pool": nc.gpsimd, "sp": nc.sync}
        self.sem = {}
        self.cnt = {}
        self.nsem = 0
        for e in self.engs:
            self._new_sem(e)
        self.dq = {"sp": "sp", "pool": "pool", "act": "act"}
        self.dsem = {q: [self._mk_sem("d%s%d" % (q, i)) for i in range(NDMA_SEM)] for q in self.dq}
        self.dcnt = {q: 0 for q in self.dq}
        self.seen = {e: {} for e in self.engs}
        self.lastw = {}
        self.reads = {}
        self.nins = 0

    def _mk_sem(self, name):
        self.nsem += 1
        return self.stack.enter_context(self.nc.semaphore("s%d_%s" % (self.nsem, name)))

    def _new_sem(self, e):
        self.sem[e] = self._mk_sem(e)
        self.cnt[e] = 0

    def _wait(self, e, ev):
        sem, val, src = ev
        sid = id(sem)
        if self.seen[e].get(sid, 0) >= val:
            return
        self.seen[e][sid] = val
        self.engs[e].wait_ge(sem, val)
        self.nins += 1

    def _deps(self, e, reads, writes, me):
        evs = []
        for k in reads:
            w = self.lastw.get(k)
            if w is not None and not (w[2] == me and me == "pe"):
                evs.append(w)
        for k in writes:
            w = self.lastw.get(k)
            if w is not None and w[2] != me:
                evs.append(w)
            for r in self.reads.get(k, ()):
                if r[2] != me:
                    evs.append(r)
        for ev in evs:
            self._wait(e, ev)

    def _record(self, ev, reads, writes):
        for k in reads:
            lst = self.reads.setdefault(k, [])
            lst.append(ev)
            if len(lst) > 12:
                d = {}
                for r in lst:
                    d[(r[2], id(r[0]))] = r
                self.reads[k] = list(d.values())
        for k in writes:
            self.lastw[k] = ev
            self.reads[k] = []

    def op(self, e, fn, reads=(), writes=()):
        self._deps(e, reads, writes, e)
        if self.cnt[e] >= SEM_CAP:
            self._new_sem(e)
        ins = fn(self.engs[e])
        self.cnt[e] += 1
        ins.then_inc(self.sem[e], 1)
        self.nins += 1
        ev = (self.sem[e], self.cnt[e], e)
        self._record(ev, reads, writes)
        return ev

    def dma(self, q, out, in_, reads=(), writes=(), **kw):
        e = self.dq[q]
        self._deps(e, reads, writes, None)
        i = self.dcnt[q]
        slot = i % NDMA_SEM
        rnd = i // NDMA_SEM
        sem = self.dsem[q][slot]
        if rnd > 0:
            self._wait(e, (sem, 16 * rnd, "dma" + q))
        self.engs[e].dma_start(out=out, in_=in_, **kw).then_inc(sem, 16)
        self.dcnt[q] += 1
        self.nins += 1
        ev = (sem, 16 * (rnd + 1), "dma" + q)
        self._record(ev, reads, writes)
        return ev

    def _all_events(self):
        evs = []
        for e in self.engs:
            if self.cnt[e] > 0:
                evs.append((self.sem[e], self.cnt[e], e))
        for q in self.dq:
            n = self.dcnt[q]
            for slot in range(NDMA_SEM):
                k = (n - slot + NDMA_SEM - 1) // NDMA_SEM if n > slot else 0
                if k > 0:
                    evs.append((self.dsem[q][slot], 16 * k, "dma" + q))
        return evs

    def barrier(self):
        evs = self._all_events()
        for e in self.engs:
            for ev in evs:
                if ev[2] != e:
                    self._wait(e, ev)
        self.lastw.clear()
        self.reads.clear()

    def finish(self, e="sp"):
        for ev in self._all_events():
            if ev[2] != e:
                self._wait(e, ev)


INPUT_NAMES = []


def consts():
    i = np.arange(128)
    row, col = i[:, None], i[None, :]
    masks = np.stack([col > row, col >= row, col < row, col <= row]).astype(np.float32)
    blk = (row // 64 == col // 64).astype(np.float32)
    selm = np.zeros((16, 16, 128), np.float32)
    for e_ in range(16):
        selm[e_, e_, :] = 1.0
    return {"ident": np.eye(128, dtype=np.float32), "masks": masks, "blk64": blk, "selm": selm}


def build(dbg=None):
    nc = bass.Bass("TRN2", target_bir_lowering=False)
    stack = contextlib.ExitStack()
    with stack:
        _emit(nc, stack, dbg)
    return nc


def _emit(nc, stack, dbg):
    S = Sched(nc, stack)

    del INPUT_NAMES[:]

    def din(name, shape, dt=F32):
        INPUT_NAMES.append(name)
        return nc.dram_tensor(name, list(shape), dt, kind="ExternalInput").ap()

    def dout(name, shape, dt=F32):
        return nc.dram_tensor(name, list(shape), dt, kind="ExternalOutput").ap()

    def sb(name, shape, dt=F32, st=None):
        return (st or stack).enter_context(nc.sbuf_tensor(name, list(shape), dt))

    def ps(name, shape, dt=F32, st=None):
        return (st or stack).enter_context(nc.psum_tensor(name, list(shape), dt))

    x_d = din("x", [SEQ, D])
    ctx_d = din("ctx", [CTX, D])
    c_d = din("c", [D])
    cctx_d = din("c_ctx", [D])
    adaw_d = din("ada_w", [D, 6 * D])
    adab_d = din("ada_b", [6 * D])
    n1pre_d = din("norm1_pre", [D])
    win_d = din("w_in", [D, N_IN])
    ident_d = din("ident", [128, 128])
    out_d = dout("out", [SEQ, D]) if dbg is None else None

    ident32 = sb("ident32", [128, 128])
    identb = sb("identb", [128, 128], BF16)
    S.dma("sp", ident32[:], ident_d, writes=["ident32"])
    S.op("dve", lambda e: e.tensor_copy(identb[:], ident32[:]), reads=["ident32"], writes=["identb"])

    blk_d = din("blk64", [128, 128]); masks_d = din("masks", [4, 128, 128])
    blk32 = sb("blk32", [128, 128])
    S.dma("sp", blk32[:], blk_d, writes=["blk32"])
    msk = sb("msk", [128, 4, 128])
    S.dma("sp", msk[:], masks_d.rearrange("m p c -> p m c"), writes=["msk"])
    onec = sb("onec", [128, 1])
    S.op("dve", lambda e: e.memset(onec[:], 1.0), writes=["onec"])
    epsc = sb("epsc", [128, 1])
    S.op("dve", lambda e: e.memset(epsc[:], EPS), writes=["epsc"])
    S.barrier()

    PS = [ps("ps%d" % i, [128, 512]) for i in range(6)]
    PSB = [ps("psb%d" % i, [128, 1024], BF16) for i in range(2)]

    def run_lanes(items, body, L):
        items = list(items)
        pos = [0]
        active = {}
        for lane in range(L):
            if pos[0] < len(items):
                active[lane] = body(items[pos[0]], lane); pos[0] += 1
        while active:
            for lane in list(active.keys()):
                try:
                    next(active[lane])
                except StopIteration:
                    if pos[0] < len(items):
                        active[lane] = body(items[pos[0]], lane); pos[0] += 1
                    else:
                        del active[lane]

    psn = [0]

    def nextps_():
        i = psn[0] % 6
        psn[0] += 1
        return PS[i], "PS%d" % i

    g2t = sb("g2t", [128, D])
    stM = contextlib.ExitStack()
    stack.callback(stM.close)
    modL = sb("modL", [128, 6 * D], st=stM)
    modC = sb("modC", [128, 2 * D], st=stM)
    with contextlib.ExitStack() as st:
        cs = sb("cs", [128, 2, 8], st=st)
        S.dma("sp", cs[:, 0, :], c_d.rearrange("(k p) -> p k", p=128), writes=["cs"], allow_slow_non_contiguous=True)
        S.dma("sp", cs[:, 1, :], cctx_d.rearrange("(k p) -> p k", p=128), writes=["cs"], allow_slow_non_contiguous=True)
        css = sb("css", [128, 2, 8], st=st)
        S.op("act", lambda e: e.activation(css[:], cs[:], AF.Silu), reads=["cs"], writes=["css"])
        cbc = sb("cbc", [128, 2, 8, 128], st=st)
        for w in range(2):
            S.op("dve", lambda e, w=w: e.tensor_copy(cbc[:, w], css[:, w, :].unsqueeze(2).to_broadcast([128, 8, 128])),
                 reads=["css"], writes=["cbc"])
        adab = sb("adab", [128, 6 * D], st=st)
        S.dma("sp", adab[:], adab_d.partition_broadcast(128), writes=["adab"])
        wbuf = [sb("adawbuf%d" % i, [128, 8, 512], st=st) for i in range(2)]
        jobs = [(0, j) for j in range(12)] + [(1, j) for j in range(4)]
        for n, (w, j) in enumerate(jobs):
            wb = wbuf[n % 2]
            S.dma("sp", wb[:], adaw_d[:, j * 512:(j + 1) * 512].rearrange("(k p) c -> p k c", p=128),
                  writes=["adaw%d" % (n % 2)])
            pt = PS[n % 2]
            for k in range(8):
                S.op("pe", lambda e, k=k, wb=wb, pt=pt, w=w: e.matmul(pt[:], cbc[:, w, k, :], wb[:, k, :], start=(k == 0), stop=(k == 7)),
                     reads=["cbc", "adaw%d" % (n % 2)], writes=["PS%d" % (n % 2)])
            dst = (modL if w == 0 else modC)[:, j * 512:(j + 1) * 512]
            S.op("dve", lambda e, dst=dst, pt=pt, j=j: e.tensor_tensor(dst, pt[:], adab[:, j * 512:(j + 1) * 512], ALU.add),
                 reads=["PS%d" % (n % 2), "adab"], writes=["mod"])
        n1pre = sb("n1pre", [128, D], st=st)
        S.dma("sp", n1pre[:], n1pre_d.partition_broadcast(128), writes=["n1pre"])
        for m in (modL, modC):
            S.op("dve", lambda e, m=m: e.scalar_tensor_tensor(m[:, D:2 * D], m[:, D:2 * D], 1.0, n1pre[:], ALU.add, ALU.mult),
                 reads=["mod", "n1pre"], writes=["mod"])
        S.barrier()

    stH = contextlib.ExitStack()
    stack.callback(stH.close)
    hT = sb("hT", [128, 8, PADW], BF16, st=stH)
    S.op("pool", lambda e: e.memset(hT[:, :, 0:1], 0.0), writes=["hT"])
    S.op("pool", lambda e: e.memset(hT[:, :, CTX + 1:CTX + 3], 0.0), writes=["hT"])
    S.op("pool", lambda e: e.memset(hT[:, :, PADW - 1:PADW], 0.0), writes=["hT"])
    with contextlib.ExitStack() as st:
        xt = [sb("xt%d" % i, [128, D], st=st) for i in range(2)]
        junk = sb("junk", [128, D], st=st)
        hb = [sb("hb%d" % i, [128, D], BF16, st=st) for i in range(2)]
        t32 = sb("t32", [128, D], st=st)
        ss = [sb("ss%d" % i, [128, 1], st=st) for i in range(2)]
        for tt in range(NTOK // 128):
            b = tt % 2
            src = ctx_d[tt * 128:(tt + 1) * 128, :] if tt < 2 else x_d[(tt - 2) * 128:(tt - 1) * 128, :]
            m = modC if tt < 2 else modL
            S.dma("sp", xt[b][:], src, writes=["xt%d" % b])
            S.op("act", lambda e, b=b: e.activation(junk[:], xt[b][:], AF.Square, accum_out=ss[b][:]),
                 reads=["xt%d" % b], writes=["junk", "ss%d" % b])
            S.op("act", lambda e, b=b: e.activation(ss[b][:], ss[b][:], AF.Sqrt, bias=epsc[:], scale=1.0 / D),
                 reads=["ss%d" % b], writes=["ss%d" % b])
            S.op("dve", lambda e, b=b: e.reciprocal(ss[b][:], ss[b][:]),
                 reads=["ss%d" % b], writes=["ss%d" % b])
            S.op("dve", lambda e, b=b, m=m: e.scalar_tensor_tensor(t32[:], xt[b][:], ss[b][:], m[:, D:2 * D], ALU.mult, ALU.mult),
                 reads=["xt%d" % b, "ss%d" % b, "mod"], writes=["t32"])
            S.op("pool", lambda e, b=b, m=m: e.tensor_tensor(hb[b][:], t32[:], m[:, 0:D], ALU.add),
                 reads=["t32", "mod"], writes=["hb%d" % b])
            pt = PSB[b]
            for k in range(8):
                S.op("pe", lambda e, k=k, b=b, pt=pt: e.transpose(pt[:, k * 128:(k + 1) * 128], hb[b][:, k * 128:(k + 1) * 128], identb[:]),
                     reads=["hb%d" % b, "identb"], writes=["PSB%d" % b])
            pp = (C0 + tt * 128) if tt < 2 else (L0 + (tt - 2) * 128)
            S.op("act", lambda e, pp=pp, pt=pt: e.copy(hT[:, :, pp:pp + 128], pt[:].rearrange("p (k t) -> p k t", k=8)),
                 reads=["PSB%d" % b], writes=["hT"])
        S.barrier()

    if dbg == "B":
        o = dout("dbg", [128, 8, PADW], BF16)
        S.dma("sp", o, hT[:], reads=["hT"])
        o2 = dout("dbg2", [128, 6 * D])
        S.dma("sp", o2, modL[:], reads=["mod"])
        S.finish()
        return

    rwmu_d = din("rw_mu", [2, 3456]); rww0_d = din("rw_w0", [2, 1024]); rww2_d = din("rw_w2", [2, 64, 1024])
    rwa0_d = din("rw_a0", [2, 1024]); rwa2_d = din("rw_a2", [2, 64, 1024]); rwg2_d = din("rw_g2", [128, 1024])
    rwkk_d = din("rw_kk", [1024]); rwka_d = din("rw_ka", [1024]); rwrk_d = din("rw_rk", [1024])
    rwlnw_d = din("rw_ln_w", [1024]); rwlnb_d = din("rw_ln_b", [1024])
    A2_s = nc.dram_tensor("rw_a2s", [1024, 2, NTOK], BF16).ap()
    D2_s = nc.dram_tensor("rw_d2s", [2, 1024, 2, NTOK], BF16).ap()
    LWT_s = nc.dram_tensor("rw_lwt", [2, NTOK, 1024], F32).ap()
    VT_s = nc.dram_tensor("rw_vt", [NTOK, 1024], BF16).ap()
    G_s = nc.dram_tensor("rw_gs", [1024, SEQ], BF16).ap()
    BV_s = nc.dram_tensor("rw_bvs", [1024, SEQ], BF16).ap()
    SKIP_RW = dbg in ("S1", "S2", "S3")
    TG = [(C0, 256, 0)] + [(L0 + 256 * g, 256, CTX + 256 * g) for g in range(16)]
    NEG_E = -math.exp(-0.5)

    with contextlib.ExitStack() as st:
        def cols(name, src):
            t = sb(name, [128, 8], st=st)
            S.dma("sp", t[:], src.rearrange("(h p) -> p h", p=128), writes=[name], allow_slow_non_contiguous=True)
            return t
        kkw = cols("kkw", rwkk_d); kaw = cols("kaw", rwka_d); rkw = cols("rkw", rwrk_d)
        w0c = [cols("w0c%d" % d, rww0_d[d]) for d in range(2)]
        a0c = [cols("a0c%d" % d, rwa0_d[d]) for d in range(2)]
        omka = sb("omka", [128, 8], st=st)
        S.op("dve", lambda e: e.tensor_scalar(omka[:], kaw[:], -1.0, 1.0, ALU.mult, ALU.add), reads=["kaw"], writes=["omka"])
        mu_all = sb("mu_all", [128, 27, 2], st=st)
        for m_ in range(2):
            S.dma("sp", mu_all[:, :, m_], rwmu_d[m_].rearrange("(t p) -> p t", p=128), writes=["mu_all"], allow_slow_non_contiguous=True)
        c0_all = sb("c0_all", [128, 27], st=st)
        S.op("dve", lambda e: e.tensor_tensor(c0_all[:], mu_all[:, :, 0], mu_all[:, :, 1], ALU.add), reads=["mu_all"], writes=["c0_all"])
        S.op("dve", lambda e: e.tensor_scalar(c0_all[:], c0_all[:], -1.0, 1.0, ALU.mult, ALU.add), reads=["c0_all"], writes=["c0_all"])
        lw32 = sb("lw32", [128, 1024], st=st)
        w2b = sb("w2b", [128, 1024], BF16, st=st); a2b = sb("a2b", [128, 1024], BF16, st=st); g2b = sb("g2b", [128, 1024], BF16, st=st)
        for dst, src in ((w2b, rww2_d.rearrange("d e c -> (d e) c")), (a2b, rwa2_d.rearrange("d e c -> (d e) c")), (g2b, rwg2_d)):
            S.dma("sp", lw32[:], src, writes=["lw32"])
            S.op("dve", lambda e, dst=dst: e.tensor_copy(dst[:], lw32[:]), reads=["lw32"], writes=["lorab"])
        th = sb("th", [128, NTOK], BF16, st=st); xab = sb("xab", [128, NTOK], BF16, st=st); sg = sb("sg", [128, NTOK], BF16, st=st)
        w32 = [sb("w32_%d" % i, [128, 8, 128], st=st) for i in range(2)]
        wbt = [sb("wbt_%d" % i, [128, 8, 128], BF16, st=st) for i in range(4)]
        nw = [0]

        def load_w(ct):
            i = nw[0]; nw[0] += 1
            a, b = w32[i % 2], wbt[i % 4]
            S.dma("sp", a[:], win_d[:, ct * 128:(ct + 1) * 128].rearrange("(k p) c -> p k c", p=128), writes=["w32_%d" % (i % 2)])
            S.op("pool", lambda e: e.tensor_copy(b[:], a[:]), reads=["w32_%d" % (i % 2)], writes=["wbt_%d" % (i % 4)])
            return b, "wbt_%d" % (i % 4)

        def proj(wb, wkey, pt, pkey, p0, n):
            for k in range(8):
                S.op("pe", lambda e, k=k: e.matmul(pt[:, 0:n + 2], wb[:, k, :], hT[:, k, p0 - 1:p0 + n + 1], start=(k == 0), stop=(k == 7)),
                     reads=[wkey, "hT"], writes=[pkey])

        def shift(pt, pkey, dst, dkey, ct, n):
            S.op("act", lambda e: e.activation(dst, pt[:, 1:n + 1], AF.Identity, scale=c0_all[:, ct:ct + 1]),
                 reads=[pkey, "c0_all"], writes=[dkey])
            S.op("dve", lambda e: e.scalar_tensor_tensor(dst, pt[:, 0:n], mu_all[:, ct, 0:1], dst, ALU.mult, ALU.add),
                 reads=[pkey, "mu_all", dkey], writes=[dkey])
            S.op("dve", lambda e: e.scalar_tensor_tensor(dst, pt[:, 2:n + 2], mu_all[:, ct, 1:2], dst, ALU.mult, ALU.add),
                 reads=[pkey, "mu_all", dkey], writes=[dkey])

        u32 = [sb("u32_%d" % i, [128, 256], st=st) for i in range(2)]
        for ct, dst, fn in (() if SKIP_RW else ((24, th, AF.Tanh), (25, xab, AF.Identity), (26, sg, AF.Sigmoid))):
            wb, wkey = load_w(ct)
            for gi, (p0, n, t0) in enumerate(TG):
                pt, pkey = PS[gi % 2], "PS%d" % (gi % 2)
                proj(wb, wkey, pt, pkey, p0, n)
                u = u32[gi % 2]; ukey = "u32_%d" % (gi % 2)
                shift(pt, pkey, u[:], ukey, ct, n)
                S.op("act", lambda e, dst=dst, fn=fn, u=u, t0=t0, n=n: e.activation(dst[:, t0:t0 + n], u[:], fn),
                     reads=[ukey], writes=["lorares"])

        def t32(name, shape=(128, 256), dt=F32):
            return [sb("%s_%d" % (name, i), list(shape), dt, st=st) for i in range(2)]
        ru_, ku_, vu_, kq_, sq_, rs_, kk_ = [t32(nm) for nm in ("ru", "ku", "vu", "kq", "sq", "rs", "kk")]
        lw_, a_, tm_, ks_ = [t32(nm) for nm in ("lw", "aa", "tm", "ks")]
        A2t_ = t32("A2t", (128, 2, 256), BF16); D2t_ = [t32("D2t%d" % d, (128, 2, 256), BF16) for d in range(2)]
        lwT_ = [t32("lwT%d" % d, (128, 2, 128)) for d in range(2)]
        vb_ = t32("vb", (128, 256), BF16); vT_ = t32("vT", (128, 2, 128), BF16)
        gb_ = t32("gb", (128, 256), BF16); bv_ = t32("bv", (128, 256), BF16)
        for hp in range(0 if SKIP_RW else 8):
            wr, wrk = load_w(hp); wk, wkk = load_w(8 + hp); wv, wvk = load_w(16 + hp)
            for gi, (p0, n, t0) in enumerate(TG):
                b = gi % 2
                sfx = "_%d" % b
                ru, ku, vu, kq, sq, rs, kk = (x[b] for x in (ru_, ku_, vu_, kq_, sq_, rs_, kk_))
                lw, aa, tm, ks = (x[b] for x in (lw_, a_, tm_, ks_))
                A2t = A2t_[b]; vb = vb_[b]; vT = vT_[b]; gb = gb_[b]; bv = bv_[b]
                for (wb, wkey, pi, dst, dk, ct) in ((wr, wrk, 0, ru, "ru", hp), (wk, wkk, 1, ku, "ku", 8 + hp), (wv, wvk, 2, vu, "vu", 16 + hp)):
                    proj(wb, wkey, PS[pi], "PS%d" % pi, p0, n)
                    shift(PS[pi], "PS%d" % pi, dst[:], dk + sfx, ct, n)
                S.op("dve", lambda e: e.tensor_scalar(kq[:], ku[:], kkw[:, hp:hp + 1], None, ALU.mult), reads=["ku" + sfx, "kkw"], writes=["kq" + sfx])
                S.op("act", lambda e: e.activation(sq[:], kq[:], AF.Square), reads=["kq" + sfx], writes=["sq" + sfx])
                S.op("pe", lambda e: e.matmul(PS[3][:, 0:n], blk32[:], sq[:], start=True, stop=True), reads=["blk32", "sq" + sfx], writes=["PS3"])
                S.op("dve", lambda e: e.tensor_scalar(rs[:], PS[3][:, 0:n], 1e-12, None, ALU.max), reads=["PS3"], writes=["rs" + sfx])
                S.op("act", lambda e: e.activation(rs[:], rs[:], AF.Sqrt), reads=["rs" + sfx], writes=["rs" + sfx])
                S.op("dve", lambda e: e.reciprocal(rs[:], rs[:]), reads=["rs" + sfx], writes=["rs" + sfx])
                S.op("dve", lambda e: e.tensor_tensor(kk[:], kq[:], rs[:], ALU.mult), reads=["kq" + sfx, "rs" + sfx], writes=["kk" + sfx])
                S.op("dve", lambda e: e.tensor_scalar(A2t[:, 0, :], kk[:], -1.0, None, ALU.mult), reads=["kk" + sfx], writes=["A2t" + sfx])
                S.op("act", lambda e: e.copy(A2t[:, 1, :], ru[:]), reads=["ru" + sfx], writes=["A2t" + sfx])
                S.dma("pool", A2_s[hp * 128:(hp + 1) * 128, :, t0:t0 + n], A2t[:], reads=["A2t" + sfx])
                for d in range(2):
                    D2t = D2t_[d][b]; lwT = lwT_[d][b]
                    dk = "d%d%s" % (d, sfx)
                    S.op("pe", lambda e, d=d: e.matmul(PS[4][:, 0:n], w2b[d * 64:(d + 1) * 64, hp * 128:(hp + 1) * 128], th[d * 64:(d + 1) * 64, t0:t0 + n], start=True, stop=True),
                         reads=["lorab", "lorares"], writes=["PS4"])
                    S.op("act", lambda e, d=d: e.activation(lw[:], PS[4][:, 0:n], AF.Sigmoid, bias=w0c[d][:, hp:hp + 1]), reads=["PS4", "w0c%d" % d], writes=["lw" + sfx])
                    S.op("dve", lambda e: e.tensor_scalar(lw[:], lw[:], NEG_E, None, ALU.mult), reads=["lw" + sfx], writes=["lw" + sfx])
                    for j in range(2):
                        S.op("pe", lambda e, j=j: e.transpose(PS[5][:, j * 128:(j + 1) * 128], lw[:, j * 128:(j + 1) * 128], ident32[:]),
                             reads=["lw" + sfx, "ident32"], writes=["PS5"])
                    S.op("act", lambda e, lwT=lwT: e.copy(lwT[:], PS[5][:, 0:256].rearrange("p (j c) -> p j c", j=2)), reads=["PS5"], writes=["lwT" + dk])
                    S.dma("pool", LWT_s[d, t0:t0 + n, hp * 128:(hp + 1) * 128].rearrange("(j p) c -> p j c", p=128), lwT[:], reads=["lwT" + dk])
                    S.op("pe", lambda e, d=d: e.matmul(PS[4][:, 0:n], a2b[d * 64:(d + 1) * 64, hp * 128:(hp + 1) * 128], xab[d * 64:(d + 1) * 64, t0:t0 + n], start=True, stop=True),
                         reads=["lorab", "lorares"], writes=["PS4"])
                    S.op("act", lambda e, d=d: e.activation(aa[:], PS[4][:, 0:n], AF.Sigmoid, bias=a0c[d][:, hp:hp + 1]), reads=["PS4", "a0c%d" % d], writes=["aa" + sfx])
                    S.op("dve", lambda e, D2t=D2t: e.tensor_tensor(D2t[:, 0, :], kk[:], aa[:], ALU.mult), reads=["kk" + sfx, "aa" + sfx], writes=["D2t" + dk])
                    S.op("dve", lambda e: e.tensor_scalar(tm[:], aa[:], kaw[:, hp:hp + 1], omka[:, hp:hp + 1], ALU.mult, ALU.add), reads=["aa" + sfx, "kaw", "omka"], writes=["tm" + sfx])
                    S.op("dve", lambda e: e.tensor_tensor(tm[:], tm[:], ku[:], ALU.mult), reads=["tm" + sfx, "ku" + sfx], writes=["tm" + sfx])
                    S.op("act", lambda e, D2t=D2t: e.copy(D2t[:, 1, :], tm[:]), reads=["tm" + sfx], writes=["D2t" + dk])
                    if d == 0:
                        S.op("act", lambda e: e.copy(ks[:], tm[:]), reads=["tm" + sfx], writes=["ks" + sfx])
                    else:
                        S.op("dve", lambda e: e.tensor_tensor(ks[:], ks[:], tm[:], ALU.add), reads=["tm" + sfx, "ks" + sfx], writes=["ks" + sfx])
                    S.dma("pool", D2_s[d, hp * 128:(hp + 1) * 128, :, t0:t0 + n], D2t[:], reads=["D2t" + dk])
                S.op("act", lambda e: e.copy(vb[:], vu[:]), reads=["vu" + sfx], writes=["vb" + sfx])
                for j in range(2):
                    S.op("pe", lambda e, j=j: e.transpose(PSB[0][:, j * 128:(j + 1) * 128], vb[:, j * 128:(j + 1) * 128], identb[:]),
                         reads=["vb" + sfx, "identb"], writes=["PSB0"])
                S.op("act", lambda e: e.copy(vT[:], PSB[0][:, 0:256].rearrange("p (j c) -> p j c", j=2)), reads=["PSB0"], writes=["vT" + sfx])
                S.dma("pool", VT_s[t0:t0 + n, hp * 128:(hp + 1) * 128].rearrange("(j p) c -> p j c", p=128), vT[:], reads=["vT" + sfx])
                if t0 >= CTX:
                    l0 = t0 - CTX
                    S.op("pe", lambda e: e.matmul(PS[4][:, 0:n], g2b[:, hp * 128:(hp + 1) * 128], sg[:, t0:t0 + n], start=True, stop=True),
                         reads=["lorab", "lorares"], writes=["PS4"])
                    S.op("act", lambda e: e.copy(gb[:], PS[4][:, 0:n]), reads=["PS4"], writes=["gb" + sfx])
                    S.dma("pool", G_s[hp * 128:(hp + 1) * 128, l0:l0 + n], gb[:], reads=["gb" + sfx])
                    S.op("dve", lambda e: e.scalar_tensor_tensor(ks[:], ks[:], rkw[:, hp:hp + 1], ru[:], ALU.mult, ALU.mult), reads=["ks" + sfx, "rkw", "ru" + sfx], writes=["ks" + sfx])
                    S.op("pe", lambda e: e.matmul(PS[3][:, 0:n], blk32[:], ks[:], start=True, stop=True), reads=["blk32", "ks" + sfx], writes=["PS3"])
                    S.op("dve", lambda e: e.tensor_tensor(bv[:], PS[3][:, 0:n], vu[:], ALU.mult), reads=["PS3", "vu" + sfx], writes=["bv" + sfx])
                    S.dma("pool", BV_s[hp * 128:(hp + 1) * 128, l0:l0 + n], bv[:], reads=["bv" + sfx])
        S.barrier()

    if dbg == "R1":
        o = dout("dbg", [4, 128, 4, NTOK], BF16)
        S.dma("sp", o[0, :, 0:2, :], A2_s[0:128], reads=[])
        S.dma("sp", o[1, :, 0:2, :], D2_s[0, 0:128], reads=[])
        S.dma("sp", o[2, :, 0:2, :], D2_s[1, 0:128], reads=[])
        S.dma("sp", o[3, :, 0, 0:SEQ], G_s[0:128], reads=[])
        S.dma("sp", o[3, :, 1, 0:SEQ], BV_s[0:128], reads=[])
        o2 = dout("dbg2", [2, NTOK, 128])
        S.dma("sp", o2[0], LWT_s[0, :, 0:128], reads=[])
        S.dma("sp", o2[1], LWT_s[1, :, 0:128], reads=[])
        o3 = dout("dbg3", [NTOK, 128], BF16)
        S.dma("sp", o3, VT_s[:, 0:128], reads=[])
        S.finish()
        return

    convw_d = din("ssm_conv_w", [3072, 5]); convb_d = din("ssm_conv_b", [3072])
    dtb_d = din("ssm_dt_bias", [64]); alog_d = din("ssm_a_log", [64])
    XT_s = nc.dram_tensor("ss_xt", [NTOK, 2048], BF16).ap()
    BT_s = nc.dram_tensor("ss_bt", [NTOK, 512], BF16).ap()
    BF_s = nc.dram_tensor("ss_bf", [4, 128, NTOK], BF16).ap()
    CF_s = nc.dram_tensor("ss_cf", [4, 128, NTOK], BF16).ap()
    ZS_s = nc.dram_tensor("ss_zs", [SEQ, 2048], BF16).ap()
    DT_s = nc.dram_tensor("ss_dt", [NTOK, 64], F32).ap()
    DA_s = nc.dram_tensor("ss_da", [NTOK, 64], F32).ap()
    GA_s = nc.dram_tensor("mg_ga", [SEQ, 1024], BF16).ap()
    GB_s = nc.dram_tensor("mg_gb", [SEQ, 1024], BF16).ap()
    hT_cm = hT[:, :, L0:L0 + SEQ].rearrange("p k (r c) -> p k c r", c=64)
    XBC0 = 3456 + 2048
    DT0 = XBC0 + 3072
    GATE0 = 3456 + 5184
    with contextlib.ExitStack() as st:
        cw = sb("cw", [128, 24, 5], st=st); cb = sb("cb", [128, 24], st=st)
        S.dma("sp", cw[:], convw_d.rearrange("(t p) j -> p t j", p=128), writes=["cw"], allow_slow_non_contiguous=True)
        S.dma("sp", cb[:], convb_d.rearrange("(t p) -> p t", p=128), writes=["cb"], allow_slow_non_contiguous=True)
        NL = 4
        w32 = [sb("s1w32_%d" % i, [128, 8, 128], st=st) for i in range(NL)]
        wbt = [sb("s1wbt_%d" % i, [128, 8, 128], BF16, st=st) for i in range(NL)]
        raw_ = [sb("s1raw_%d" % i, [128, 260], st=st) for i in range(2 * NL)]
        acc_ = [sb("s1acc_%d" % i, [128, 256], st=st) for i in range(2 * NL)]
        ob_ = [sb("s1ob_%d" % i, [128, 256], BF16, st=st) for i in range(2 * NL)]
        oT_ = [sb("s1oT_%d" % i, [128, 2, 128], BF16, st=st) for i in range(2 * NL)]

        def conv_body(ct, lane):
            a, wb = w32[lane], wbt[lane]
            wkey = "s1wbt_%d" % lane
            S.dma("sp", a[:], win_d[:, XBC0 + ct * 128:XBC0 + (ct + 1) * 128].rearrange("(k p) c -> p k c", p=128), writes=["s1w32_%d" % lane])
            yield
            S.op("pool", lambda e: e.tensor_copy(wb[:], a[:]), reads=["s1w32_%d" % lane], writes=[wkey])
            yield
            for gi in range(17):
                b = lane * 2 + gi % 2
                sfx = "_%d" % b
                raw, acc, ob, oT = raw_[b], acc_[b], ob_[b], oT_[b]
                pt, pk = PS[lane], "PS%d" % lane
                pbt, pbk = PSB[lane % 2], "PSB%d" % (lane % 2)
                s0 = 0 if gi == 0 else CTX + 256 * (gi - 1)
                if gi == 0:
                    for k in range(8):
                        S.op("pe", lambda e, k=k: e.matmul(pt[:, 2:258], wb[:, k, :], hT[:, k, C0:C0 + 256], start=(k == 0), stop=(k == 7)), reads=[wkey, "hT"], writes=[pk])
                else:
                    g = gi - 1
                    for k in range(8):
                        S.op("pe", lambda e, k=k, g=g: e.matmul(pt[:, 2:258].rearrange("p (c r) -> p c r", c=4), wb[:, k, :], hT_cm[:, k, 4 * g:4 * g + 4, :], start=(k == 0), stop=(k == 7)), reads=[wkey, "hT"], writes=[pk])
                    if g > 0:
                        for k in range(8):
                            S.op("pe", lambda e, k=k, g=g: e.matmul(pt[:, 0:2], wb[:, k, :], hT_cm[:, k, 4 * g - 1, 62:64], start=(k == 0), stop=(k == 7)), reads=[wkey, "hT"], writes=[pk])
                    if g < 15:
                        for k in range(8):
                            S.op("pe", lambda e, k=k, g=g: e.matmul(pt[:, 258:260], wb[:, k, :], hT_cm[:, k, 4 * g + 4, 0:2], start=(k == 0), stop=(k == 7)), reads=[wkey, "hT"], writes=[pk])
                yield
                S.op("act", lambda e: e.copy(raw[:], pt[:, 0:260]), reads=[pk], writes=["s1raw" + sfx])
                if gi <= 1:
                    S.op("pool", lambda e: e.memset(raw[:, 0:2], 0.0), writes=["s1raw" + sfx])
                if gi == 0 or gi == 16:
                    S.op("pool", lambda e: e.memset(raw[:, 258:260], 0.0), writes=["s1raw" + sfx])
                yield
                S.op("dve", lambda e: e.tensor_scalar(acc[:], raw[:, 0:256], cw[:, ct, 0:1], cb[:, ct:ct + 1], ALU.mult, ALU.add), reads=["s1raw" + sfx, "cw", "cb"], writes=["s1acc" + sfx])
                for j in range(1, 5):
                    S.op("dve", lambda e, j=j: e.scalar_tensor_tensor(acc[:], raw[:, j:j + 256], cw[:, ct, j:j + 1], acc[:], ALU.mult, ALU.add), reads=["s1raw" + sfx, "cw", "s1acc" + sfx], writes=["s1acc" + sfx])
                yield
                S.op("act", lambda e: e.activation(ob[:], acc[:], AF.Silu), reads=["s1acc" + sfx], writes=["s1ob" + sfx])
                yield
                if ct >= 16:
                    dst = (BF_s if ct < 20 else CF_s)[(ct - 16) % 4, :, s0:s0 + 256]
                    S.dma("pool", dst, ob[:], reads=["s1ob" + sfx])
                if ct < 20:
                    for j in range(2):
                        S.op("pe", lambda e, j=j: e.transpose(pbt[:, j * 128:(j + 1) * 128], ob[:, j * 128:(j + 1) * 128], identb[:]), reads=["s1ob" + sfx, "identb"], writes=[pbk])
                    S.op("act", lambda e: e.copy(oT[:], pbt[:, 0:256].rearrange("p (j c) -> p j c", j=2)), reads=[pbk], writes=["s1oT" + sfx])
                    yield
                    if ct < 16:
                        dst = XT_s[s0:s0 + 256, ct * 128:(ct + 1) * 128]
                    else:
                        dst = BT_s[s0:s0 + 256, (ct - 16) * 128:(ct - 15) * 128]
                    S.dma("pool", dst.rearrange("(j p) c -> p j c", p=128), oT[:], reads=["s1oT" + sfx])
                yield

        run_lanes(range(24), conv_body, NL)
        S.barrier()

    def tiles_for(order, with_ctx):
        out = []
        if with_ctx:
            for j in range(2):
                out.append(((lambda j: (lambda k: hT[:, k, C0 + j * 128:C0 + (j + 1) * 128]))(j), 128, j * 128))
        base = CTX if with_ctx else 0
        if order == "ssm":
            for c in range(64):
                out.append(((lambda c: (lambda k: hT_cm[:, k, c, :]))(c), 64, base + c * 64))
        else:
            for tt in range(32):
                out.append(((lambda tt: (lambda k: hT[:, k, L0 + tt * 128:L0 + (tt + 1) * 128]))(tt), 128, base + tt * 128))
        return out

    for name, col0, ncol, fn, dst_s, order in (("z", 3456, 2048, AF.Silu, ZS_s, "ssm"), ("gb", GATE0 + 1024, 1024, AF.Sigmoid, GB_s, "ssm"), ("ga", GATE0, 1024, AF.Sigmoid, GA_s, "nat")):
        with contextlib.ExitStack() as st:
            wz = sb("wz" + name, [128, 8, ncol], BF16, st=st)
            stg = [sb("wzs%s%d" % (name, i), [128, 8, 512], st=st) for i in range(2)]
            for cbk in range(ncol // 512):
                S.dma("sp", stg[cbk % 2][:], win_d[:, col0 + cbk * 512:col0 + (cbk + 1) * 512].rearrange("(k p) c -> p k c", p=128), writes=["wzs%d" % (cbk % 2)])
                S.op("pool", lambda e, cbk=cbk: e.tensor_copy(wz[:, :, cbk * 512:(cbk + 1) * 512], stg[cbk % 2][:]), reads=["wzs%d" % (cbk % 2)], writes=["wz"])
            zt_ = [sb("zt%s%d" % (name, i), [128, ncol], BF16, st=st) for i in range(2)]
            for ti, (lf, nr, r0) in enumerate(tiles_for(order, False)):
                zt = zt_[ti % 2]
                for cbk in range(ncol // 512):
                    pt, pk = nextps_()
                    for k in range(8):
                        S.op("pe", lambda e, k=k, cbk=cbk, pt=pt: e.matmul(pt[0:nr, :], lf(k), wz[:, k, cbk * 512:(cbk + 1) * 512], start=(k == 0), stop=(k == 7)), reads=["wz", "hT"], writes=[pk])
                    S.op("act", lambda e, cbk=cbk, pt=pt: e.activation(zt[0:nr, cbk * 512:(cbk + 1) * 512], pt[0:nr, :], fn), reads=[pk], writes=["zt%d" % (ti % 2)])
                S.dma("pool", dst_s[r0:r0 + nr, :], zt[0:nr, :], reads=["zt%d" % (ti % 2)])
            S.barrier()
    with contextlib.ExitStack() as st:
        wd32 = sb("wd32", [128, 8, 64], st=st); wdb = sb("wdb", [128, 8, 64], BF16, st=st)
        S.dma("sp", wd32[:], win_d[:, DT0:DT0 + 64].rearrange("(k p) c -> p k c", p=128), writes=["wd32"])
        S.op("dve", lambda e: e.tensor_copy(wdb[:], wd32[:]), reads=["wd32"], writes=["wdb"])
        dtb = sb("dtb", [128, 64], st=st); aneg = sb("aneg", [128, 64], st=st)
        S.dma("sp", dtb[:], dtb_d.partition_broadcast(128), writes=["dtb"])
        S.dma("sp", aneg[:], alog_d.partition_broadcast(128), writes=["aneg"])
        S.op("act", lambda e: e.activation(aneg[:], aneg[:], AF.Exp), reads=["aneg"], writes=["aneg"])
        S.op("dve", lambda e: e.tensor_scalar(aneg[:], aneg[:], -1.0, None, ALU.mult), reads=["aneg"], writes=["aneg"])
        dx_ = [sb("dx%d" % i, [128, 64], st=st) for i in range(2)]
        da_ = [sb("dax%d" % i, [128, 64], st=st) for i in range(2)]
        for ti, (lf, nr, r0) in enumerate(tiles_for("ssm", True)):
            b = ti % 2
            dx, da = dx_[b], da_[b]
            pt, pk = nextps_()
            for k in range(8):
                S.op("pe", lambda e, k=k: e.matmul(pt[0:nr, 0:64], lf(k), wdb[:, k, :], start=(k == 0), stop=(k == 7)), reads=["wdb", "hT"], writes=[pk])
            S.op("dve", lambda e: e.scalar_tensor_tensor(dx[0:nr, :], pt[0:nr, 0:64], 30.0, dtb[0:nr, :], ALU.min, ALU.add), reads=[pk, "dtb"], writes=["dx%d" % b])
            S.op("act", lambda e: e.activation(dx[0:nr, :], dx[0:nr, :], AF.Exp), reads=["dx%d" % b], writes=["dx%d" % b])
            S.op("act", lambda e: e.activation(dx[0:nr, :], dx[0:nr, :], AF.Ln, bias=onec[0:nr, :]), reads=["dx%d" % b, "onec"], writes=["dx%d" % b])
            S.op("dve", lambda e: e.tensor_tensor(da[0:nr, :], dx[0:nr, :], aneg[0:nr, :], ALU.mult), reads=["dx%d" % b, "aneg"], writes=["dax%d" % b])
            S.dma("pool", DT_s[r0:r0 + nr, :], dx[0:nr, :], reads=["dx%d" % b])
            S.dma("pool", DA_s[r0:r0 + nr, :], da[0:nr, :], reads=["dax%d" % b])
        S.barrier()
    if dbg == "S1":
        o = dout("dbgX", [512, 2048], BF16); S.dma("sp", o[0:256], XT_s[0:256], reads=[]); S.dma("sp", o[256:512], XT_s[NTOK - 256:NTOK], reads=[])
        o = dout("dbgB", [NTOK, 512], BF16); S.dma("sp", o, BT_s, reads=[])
        o = dout("dbgBF", [128, NTOK], BF16); S.dma("sp", o, BF_s[1], reads=[])
        o = dout("dbgC", [128, NTOK], BF16); S.dma("sp", o, CF_s[2], reads=[])
        o = dout("dbgZ", [256, 2048], BF16); S.dma("sp", o, ZS_s[1024:1280], reads=[])
        o = dout("dbgGA", [256, 1024], BF16); S.dma("sp", o, GA_s[1024:1280], reads=[])
        o = dout("dbgGB", [256, 1024], BF16); S.dma("sp", o, GB_s[1024:1280], reads=[])
        o = dout("dbgDT", [NTOK, 64]); S.dma("sp", o, DT_s, reads=[])
        o = dout("dbgDA", [NTOK, 64]); S.dma("sp", o, DA_s, reads=[])
        S.finish()
        return

    S.barrier()
    stH.close()

    YS = nc.dram_tensor("rw_ys", [2, 16, 64, SEQ], F32).ap()
    nextps = nextps_

    r2cnt = [0]

    def r2_round(streams, nchunks=34):
        r2cnt[0] += 1
        rid = r2cnt[0]
        with contextlib.ExitStack() as st:
            B = []
            for si, (d, hg) in enumerate(streams):
                def T_(nm, shape, dt=BF16):
                    return sb("r2%s_%d_%d" % (nm, si, rid), list(shape), dt, st=st)
                bb = dict(
                    lwT=T_("lwT", [128, 256], F32), A2c=T_("A2c", [64, 4, 2, 128]), D2c=T_("D2c", [64, 4, 2, 128]), Vc=T_("Vc", [128, 4, 64]),
                    E1=T_("E1", [64, 4, 128], F32), E2=T_("E2", [64, 4, 128], F32), E3=T_("E3", [64, 4, 128], F32),
                    alt=T_("alt", [64, 4, 128]), rt=T_("rt", [64, 4, 128]), bet=T_("bet", [64, 4, 128]), kat=T_("kat", [64, 4, 128]),
                    W0=T_("W0", [128, 4, 128], F32), W1=T_("W1", [128, 4, 128], F32), N0=T_("N0", [128, 4, 128], F32), N1=T_("N1", [128, 4, 128], F32),
                    XA=T_("XA", [128, 4, 64]),
                    Wak=T_("Wak", [128, 4, 128]), Mrb=T_("Mrb", [128, 4, 128]), Mrk=T_("Mrk", [128, 4, 128]),
                    TT=T_("TT", [128, 2, 4, 64]), X=T_("X", [128, 4, 128], F32), AtF=T_("AtF", [64, 4, 128]), U=T_("U", [128, 4, 64]),
                    Z32=T_("Z32", [64, 4, 64], F32), Zb=T_("Zb", [64, 4, 64]), tz=T_("tz", [64, 4, 64], F32), Yt=T_("Yt", [64, 4, 128], F32),
                )
                S.op("pool", lambda e, bb=bb: e.memset(bb["Z32"][:], 0.0), writes=["Z32_%d" % si])
                S.op("pool", lambda e, bb=bb: e.memset(bb["Zb"][:], 0.0), writes=["Zb_%d" % si])
                B.append(bb)

            def k_(nm, si):
                return "%s_%d" % (nm, si)

            for step in range(nchunks):
                info = []
                for si, (d, hg) in enumerate(streams):
                    if d == 0:
                        c = step
                    else:
                        c = (1 - step) if step < 2 else (35 - step)
                    info.append((si, d, hg, c, c * 128, B[si]))
                for si, d, hg, c, t0, bb in info:
                    ch0 = hg * 256
                    S.dma("sp", bb["lwT"][:], LWT_s[d, t0:t0 + 128, ch0:ch0 + 256], writes=[k_("lwT", si)])
                    for a_ in range(2):
                        S.dma("sp", bb["A2c"][:, :, a_, :], A2_s[ch0:ch0 + 256, a_, t0:t0 + 128].rearrange("(h k) t -> k h t", k=64), writes=[k_("A2c", si)])
                        S.dma("sp", bb["D2c"][:, :, a_, :], D2_s[d, ch0:ch0 + 256, a_, t0:t0 + 128].rearrange("(h k) t -> k h t", k=64), writes=[k_("D2c", si)])
                    S.dma("sp", bb["Vc"][:], VT_s[t0:t0 + 128, ch0:ch0 + 256].rearrange("t (h v) -> t h v", h=4), writes=[k_("Vc", si)])
                for si, d, hg, c, t0, bb in info:
                    mi, me = (1, 0) if d == 0 else (3, 2)
                    pi_, pik = nextps(); pe_, pek = nextps()
                    for h in range(4):
                        S.op("pe", lambda e, h=h, bb=bb, pi_=pi_, mi=mi: e.matmul(pi_[0:64, h * 128:(h + 1) * 128], bb["lwT"][:, h * 64:(h + 1) * 64], msk[:, mi, :], start=True, stop=True),
                             reads=[k_("lwT", si), "msk"], writes=[pik])
                    for h in range(4):
                        S.op("pe", lambda e, h=h, bb=bb, pe_=pe_, me=me: e.matmul(pe_[0:64, h * 128:(h + 1) * 128], bb["lwT"][:, h * 64:(h + 1) * 64], msk[:, me, :], start=True, stop=True),
                             reads=[k_("lwT", si), "msk"], writes=[pek])
                    v3 = lambda t: t[0:64, :].rearrange("p (h t) -> p h t", h=4)
                    S.op("act", lambda e, bb=bb, pe_=pe_: e.activation(bb["E1"][:], v3(pe_), AF.Exp), reads=[pek], writes=[k_("E1", si)])
                    S.op("act", lambda e, bb=bb, pi_=pi_: e.activation(bb["E2"][:], v3(pi_), AF.Exp), reads=[pik], writes=[k_("E2", si)])
                    S.op("act", lambda e, bb=bb, pi_=pi_: e.activation(bb["E3"][:], v3(pi_), AF.Exp, scale=-1.0), reads=[pik], writes=[k_("E3", si)])
                for si, d, hg, c, t0, bb in info:
                    for dst, src, a_, E in (("alt", "A2c", 0, "E1"), ("rt", "A2c", 1, "E2"), ("bet", "D2c", 0, "E3"), ("kat", "D2c", 1, "E3")):
                        S.op("dve", lambda e, bb=bb, dst=dst, src=src, a_=a_, E=E: e.tensor_tensor(bb[dst][:], bb[src][:, :, a_, :], bb[E][:], ALU.mult),
                             reads=[k_(src, si), k_(E, si)], writes=[k_(dst, si)])
                for si, d, hg, c, t0, bb in info:
                    Wm, Nm, Mm = (0, 2, 1) if d == 0 else (2, 0, 3)
                    for dst, L, R_, mm in (("W0", "bet", "alt", Wm), ("N0", "alt", "bet", Nm), ("Wak", "kat", "alt", Wm), ("Mrb", "bet", "rt", Mm), ("Mrk", "kat", "rt", Mm)):
                        pa, pk = nextps()
                        for h in range(4):
                            S.op("pe", lambda e, h=h, bb=bb, pa=pa, L=L, R_=R_: e.matmul(pa[:, h * 128:(h + 1) * 128], bb[L][:, h, :], bb[R_][:, h, :], start=True, stop=True),
                                 reads=[k_(L, si), k_(R_, si)], writes=[pk])
                        S.op("dve", lambda e, bb=bb, pa=pa, dst=dst, mm=mm: e.tensor_tensor(bb[dst][:], pa[:].rearrange("p (h t) -> p h t", h=4), msk[:, mm, :].unsqueeze(1).to_broadcast([128, 4, 128]), ALU.mult),
                             reads=[pk, "msk"], writes=[k_(dst, si)])
                    for j, src in enumerate(("alt", "bet", "kat")):
                        for h in range(4):
                            S.op("pe", lambda e, h=h, j=j, bb=bb, src=src: e.transpose(PSB[0][:, (j * 4 + h) * 64:(j * 4 + h + 1) * 64], bb[src][:, h, :], identb[0:64, 0:64]),
                                 reads=[k_(src, si), "identb"], writes=["PSB0"])
                    S.op("act", lambda e, bb=bb: e.copy(bb["X"][:, :, 0:64], PSB[0][:, 0:256].rearrange("p (h k) -> p h k", h=4)), reads=["PSB0"], writes=[k_("X", si)])
                    S.op("act", lambda e, bb=bb: e.copy(bb["TT"][:], PSB[0][:, 256:768].rearrange("p (j h k) -> p j h k", j=2, h=4)), reads=["PSB0"], writes=[k_("TT", si)])
                    pa, pk = nextps()
                    for h in range(4):
                        S.op("pe", lambda e, h=h, bb=bb, pa=pa: e.matmul(pa[:, h * 64:(h + 1) * 64], bb["Wak"][:, h, :], bb["Vc"][:, h, :], start=True, stop=True),
                             reads=[k_("Wak", si), k_("Vc", si)], writes=[pk])
                    S.op("act", lambda e, bb=bb, pa=pa: e.copy(bb["X"][:, :, 64:128], pa[:, 0:256].rearrange("p (h v) -> p h v", h=4)), reads=[pk], writes=[k_("X", si)])
                for lvl in range(7):
                    for si, d, hg, c, t0, bb in info:
                        Wc, Nc = ("W0", "N0") if lvl % 2 == 0 else ("W1", "N1")
                        Wn, Nn = ("W1", "N1") if lvl % 2 == 0 else ("W0", "N0")
                        px, pxk = nextps()
                        for h in range(4):
                            S.op("pe", lambda e, h=h, bb=bb, px=px, Wc=Wc: e.matmul(px[:, h * 128:(h + 1) * 128], bb[Wc][:, h, :], bb["X"][:, h, :], start=True, stop=True),
                                 reads=[k_(Wc, si), k_("X", si)], writes=[pxk])
                        if lvl < 6:
                            pw, pwk = nextps(); pn, pnk = nextps()
                            for h in range(4):
                                S.op("pe", lambda e, h=h, bb=bb, pw=pw, Wc=Wc, Nc=Nc: e.matmul(pw[:, h * 128:(h + 1) * 128], bb[Nc][:, h, :], bb[Wc][:, h, :], start=True, stop=True),
                                     reads=[k_(Wc, si), k_(Nc, si)], writes=[pwk])
                            for h in range(4):
                                S.op("pe", lambda e, h=h, bb=bb, pn=pn, Wc=Wc, Nc=Nc: e.matmul(pn[:, h * 128:(h + 1) * 128], bb[Wc][:, h, :], bb[Nc][:, h, :], start=True, stop=True),
                                     reads=[k_(Wc, si), k_(Nc, si)], writes=[pnk])
                        S.op("dve", lambda e, bb=bb, px=px: e.tensor_tensor(bb["X"][:], px[:].rearrange("p (h t) -> p h t", h=4), bb["X"][:], ALU.add),
                             reads=[pxk, k_("X", si)], writes=[k_("X", si)])
                        if lvl < 6:
                            S.op("act", lambda e, bb=bb, pw=pw, Wn=Wn: e.copy(bb[Wn][:], pw[:].rearrange("p (h t) -> p h t", h=4)), reads=[pwk], writes=[k_(Wn, si)])
                            S.op("dve", lambda e, bb=bb, pn=pn, Nn=Nn: e.tensor_copy(bb[Nn][:], pn[:].rearrange("p (h t) -> p h t", h=4)), reads=[pnk], writes=[k_(Nn, si)])
                for si, d, hg, c, t0, bb in info:
                    S.op("act", lambda e, bb=bb: e.copy(bb["XA"][:], bb["X"][:, :, 0:64]), reads=[k_("X", si)], writes=[k_("XA", si)])
                    for h in range(4):
                        S.op("pe", lambda e, h=h, bb=bb: e.transpose(PSB[1][0:64, h * 128:(h + 1) * 128], bb["XA"][:, h, :], identb[:]),
                             reads=[k_("XA", si), "identb"], writes=["PSB1"])
                    S.op("act", lambda e, bb=bb: e.copy(bb["AtF"][:], PSB[1][0:64, 0:512].rearrange("p (h t) -> p h t", h=4)), reads=["PSB1"], writes=[k_("AtF", si)])
                for si, d, hg, c, t0, bb in info:
                    pa, pk = nextps()
                    for h in range(4):
                        S.op("pe", lambda e, h=h, bb=bb, pa=pa: e.matmul(pa[:, h * 64:(h + 1) * 64], bb["AtF"][:, h, :], bb["Zb"][:, h, :], start=True, stop=True),
                             reads=[k_("AtF", si), k_("Zb", si)], writes=[pk])
                    S.op("dve", lambda e, bb=bb, pa=pa: e.tensor_tensor(bb["U"][:], pa[:, 0:256].rearrange("p (h v) -> p h v", h=4), bb["X"][:, :, 64:128], ALU.add),
                         reads=[pk, k_("X", si)], writes=[k_("U", si)])
                for si, d, hg, c, t0, bb in info:
                    if c >= 2:
                        py, pyk = nextps()
                        for h in range(4):
                            o_ = py[0:64, h * 128:(h + 1) * 128]
                            S.op("pe", lambda e, h=h, bb=bb, o_=o_: e.matmul(o_, bb["Zb"][:, h, :], bb["rt"][:, h, :], start=True, stop=False), reads=[k_("Zb", si), k_("rt", si)], writes=[pyk])
                            S.op("pe", lambda e, h=h, bb=bb, o_=o_: e.matmul(o_, bb["U"][:, h, :], bb["Mrb"][:, h, :], start=False, stop=False), reads=[k_("U", si), k_("Mrb", si)], writes=[pyk])
                            S.op("pe", lambda e, h=h, bb=bb, o_=o_: e.matmul(o_, bb["Vc"][:, h, :], bb["Mrk"][:, h, :], start=False, stop=True), reads=[k_("Vc", si), k_("Mrk", si)], writes=[pyk])
                        S.op("act", lambda e, bb=bb, py=py: e.copy(bb["Yt"][:], py[0:64, :].rearrange("p (h t) -> p h t", h=4)), reads=[pyk], writes=[k_("Yt", si)])
                        l0 = t0 - CTX
                        S.dma("pool", YS[d, hg * 4:hg * 4 + 4, :, l0:l0 + 128].rearrange("h v t -> v h t"), bb["Yt"][:], reads=[k_("Yt", si)])
                    pz, pzk = nextps()
                    for h in range(4):
                        o_ = pz[0:64, h * 64:(h + 1) * 64]
                        S.op("pe", lambda e, h=h, bb=bb, o_=o_: e.matmul(o_, bb["TT"][:, 0, h, :], bb["U"][:, h, :], start=True, stop=False), reads=[k_("TT", si), k_("U", si)], writes=[pzk])
                        S.op("pe", lambda e, h=h, bb=bb, o_=o_: e.matmul(o_, bb["TT"][:, 1, h, :], bb["Vc"][:, h, :], start=False, stop=True), reads=[k_("TT", si), k_("Vc", si)], writes=[pzk])
                    last = 127 if d == 0 else 0
                    S.op("dve", lambda e, bb=bb, pz=pz: e.tensor_tensor(bb["tz"][:], pz[0:64, 0:256].rearrange("p (h v) -> p h v", h=4), bb["Z32"][:], ALU.add),
                         reads=[pzk, k_("Z32", si)], writes=[k_("tz", si)])
                    S.op("dve", lambda e, bb=bb, last=last: e.tensor_tensor(bb["Z32"][:], bb["tz"][:], bb["E2"][:, :, last:last + 1].to_broadcast([64, 4, 64]), ALU.mult),
                         reads=[k_("tz", si), k_("E2", si)], writes=[k_("Z32", si)])
                    S.op("act", lambda e, bb=bb: e.copy(bb["Zb"][:], bb["Z32"][:]), reads=[k_("Z32", si)], writes=[k_("Zb", si)])
            S.barrier()
            if dbg == "R2a":
                bb = B[0]
                o = dout("dbgA", [64, 4, 4, 128], BF16)
                for j, nm in enumerate(("alt", "rt", "bet", "kat")):
                    S.dma("sp", o[:, j], bb[nm][:], reads=[])
                o = dout("dbgB", [128, 4, 128])
                S.dma("sp", o, bb["X"][:], reads=[])
                o = dout("dbgC", [128, 4, 64], BF16)
                S.dma("sp", o, bb["U"][:], reads=[])
                o = dout("dbgD", [64, 4, 64])
                S.dma("sp", o, bb["Z32"][:], reads=[])
                o = dout("dbgE", [64, 3, 4, 128])
                for j, nm in enumerate(("E1", "E2", "E3")):
                    S.dma("sp", o[:, j], bb[nm][:], reads=[])
                S.barrier()

    if dbg == "R2a":
        r2_round([(0, 0), (1, 0)], 1)
        S.finish()
        return
    R2N = 34 if dbg != "R2" else 6
    if dbg == "R2":
        r2_round([(0, 0), (1, 0)], R2N)
        o = dout("dbg", [2, 4, 64, 512])
        S.dma("sp", o[0], YS[0, 0:4, :, 0:512], reads=[])
        S.dma("sp", o[1], YS[1, 0:4, :, SEQ - 512:SEQ], reads=[])
        S.finish()
        return
    if dbg == "R3":
        r2_round([(0, 0), (1, 0)])
    elif not SKIP_RW:
        for rnd in range(2):
            r2_round([(0, 2 * rnd), (1, 2 * rnd), (0, 2 * rnd + 1), (1, 2 * rnd + 1)])

    YD_s = nc.dram_tensor("ss_yd", [2, SEQ, 2048], F32).ap()

    def s2_scan(nsteps=34):
        with contextlib.ExitStack() as st:
            ones128 = sb("ones128", [128, 128], st=st)
            S.op("dve", lambda e: e.memset(ones128[:], 1.0), writes=["ones128"])
            negm = sb("negm", [128, 2, 128], st=st)
            S.op("dve", lambda e: e.tensor_scalar(negm[:, 0, :], msk[:, 2, :], -1e30, None, ALU.mult), reads=["msk"], writes=["negm"])
            S.op("dve", lambda e: e.tensor_scalar(negm[:, 1, :], msk[:, 0, :], -1e30, None, ALU.mult), reads=["msk"], writes=["negm"])
            B = []
            for d in range(2):
                def T_(nm, shape, dt=BF16):
                    return sb("s2%s_%d" % (nm, d), list(shape), dt, st=st)
                bb = dict(XT=T_("XT", [128, 32, 64]), BT=T_("BT", [128, 512]), BF=T_("BF", [128, 4, 128]), CF=T_("CF", [128, 4, 128]),
                          DT=T_("DT", [128, 32], F32), DA=T_("DA", [128, 32], F32), cum=T_("cum", [128, 32], F32), ncum=T_("ncum", [128, 32], F32),
                          Ecum=T_("Ecum", [128, 32], F32), Gd=T_("Gd", [128, 32], F32), Etot=T_("Etot", [128, 32], F32),
                          xdt=T_("xdt", [128, 32, 64]), xs=T_("xs", [128, 32, 64]), rhsA=T_("rhsA", [128, 32, 128], F32),
                          CBT=T_("CBT", [128, 4, 128], F32), seg0=T_("seg0", [128, 8, 128], F32), seg1=T_("seg1", [128, 8, 128], F32),
                          MT0=T_("MT0", [128, 8, 128]), MT1=T_("MT1", [128, 8, 128]),
                          H32=T_("H32", [128, 4, 512], F32), Hb=T_("Hb", [128, 4, 512]), yt0=T_("yt0", [128, 512], F32), yt1=T_("yt1", [128, 512], F32),
                          ti0=T_("ti0", [128, 512], F32), ti1=T_("ti1", [128, 512], F32))
                S.op("pool", lambda e, bb=bb: e.memset(bb["H32"][:], 0.0), writes=["H32_%d" % d])
                S.op("pool", lambda e, bb=bb: e.memset(bb["Hb"][:], 0.0), writes=["Hb_%d" % d])
                B.append(bb)

            def k_(nm, d):
                return "s2%s_%d" % (nm, d)

            for step in range(nsteps):
                info = []
                for d in range(2):
                    c = step if d == 0 else ((1 - step) if step < 2 else (35 - step))
                    info.append((d, c, c * 128, B[d]))
                for d, c, t0, bb in info:
                    S.dma("sp", bb["XT"][:], XT_s[t0:t0 + 128, :].rearrange("t (h p) -> t h p", h=32), writes=[k_("XT", d)])
                    S.dma("sp", bb["BT"][:], BT_s[t0:t0 + 128, :], writes=[k_("BT", d)])
                    S.dma("sp", bb["BF"][:], BF_s[:, :, t0:t0 + 128].rearrange("g n t -> n g t"), writes=[k_("BF", d)])
                    if c >= 2:
                        S.dma("sp", bb["CF"][:], CF_s[:, :, t0:t0 + 128].rearrange("g n t -> n g t"), writes=[k_("CF", d)])
                    S.dma("sp", bb["DT"][:], DT_s[t0:t0 + 128, d * 32:(d + 1) * 32], writes=[k_("DT", d)])
                    S.dma("sp", bb["DA"][:], DA_s[t0:t0 + 128, d * 32:(d + 1) * 32], writes=[k_("DA", d)])
                for d, c, t0, bb in info:
                    mi = 1 if d == 0 else 3
                    pc, pck = nextps_()
                    S.op("pe", lambda e, bb=bb, pc=pc, mi=mi: e.matmul(pc[:, 0:32], msk[:, mi, :], bb["DA"][:], start=True, stop=True), reads=["msk", k_("DA", d)], writes=[pck])
                    S.op("pe", lambda e, bb=bb, pc=pc: e.matmul(pc[:, 32:64], ones128[:], bb["DA"][:], start=True, stop=True), reads=["ones128", k_("DA", d)], writes=[pck])
                    S.op("act", lambda e, bb=bb, pc=pc: e.copy(bb["cum"][:], pc[:, 0:32]), reads=[pck], writes=[k_("cum", d)])
                    S.op("dve", lambda e, bb=bb: e.tensor_scalar(bb["ncum"][:], bb["cum"][:], -1.0, None, ALU.mult), reads=[k_("cum", d)], writes=[k_("ncum", d)])
                    S.op("act", lambda e, bb=bb: e.activation(bb["Ecum"][:], bb["cum"][:], AF.Exp), reads=[k_("cum", d)], writes=[k_("Ecum", d)])
                    S.op("dve", lambda e, bb=bb, pc=pc: e.tensor_tensor(bb["Gd"][:], pc[:, 32:64], bb["cum"][:], ALU.subtract), reads=[pck, k_("cum", d)], writes=[k_("Gd", d)])
                    S.op("act", lambda e, bb=bb: e.activation(bb["Gd"][:], bb["Gd"][:], AF.Exp), reads=[k_("Gd", d)], writes=[k_("Gd", d)])
                    S.op("act", lambda e, bb=bb, pc=pc: e.activation(bb["Etot"][:], pc[:, 32:64], AF.Exp), reads=[pck], writes=[k_("Etot", d)])
                    S.op("dve", lambda e, bb=bb: e.tensor_tensor(bb["xdt"][:], bb["XT"][:], bb["DT"][:].unsqueeze(2).to_broadcast([128, 32, 64]), ALU.mult), reads=[k_("XT", d), k_("DT", d)], writes=[k_("xdt", d)])
                    S.op("dve", lambda e, bb=bb: e.tensor_tensor(bb["xs"][:], bb["xdt"][:], bb["Gd"][:].unsqueeze(2).to_broadcast([128, 32, 64]), ALU.mult), reads=[k_("xdt", d), k_("Gd", d)], writes=[k_("xs", d)])
                    if c >= 2:
                        S.op("dve", lambda e, bb=bb, mi=mi: e.tensor_tensor(bb["rhsA"][:], msk[:, mi, :].unsqueeze(1).to_broadcast([128, 32, 128]), bb["DA"][:].unsqueeze(2).to_broadcast([128, 32, 128]), ALU.mult), reads=["msk", k_("DA", d)], writes=[k_("rhsA", d)])
                        pcb, pcbk = nextps_()
                        for g in range(4):
                            S.op("pe", lambda e, bb=bb, g=g, pcb=pcb: e.matmul(pcb[:, g * 128:(g + 1) * 128], bb["BF"][:, g, :], bb["CF"][:, g, :], start=True, stop=True), reads=[k_("BF", d), k_("CF", d)], writes=[pcbk])
                        S.op("act", lambda e, bb=bb, pcb=pcb: e.copy(bb["CBT"][:], pcb[:].rearrange("p (g t) -> p g t", g=4)), reads=[pcbk], writes=[k_("CBT", d)])
                for g in range(4):
                    for d, c, t0, bb in info:
                        if c < 2:
                            continue
                        sgn, mtn, ytn, tin = "seg%d" % (g % 2), "MT%d" % (g % 2), "yt%d" % (g % 2), "ti%d" % (g % 2)
                        for half in range(2):
                            p4, p4k = nextps_()
                            for e4 in range(4):
                                h = g * 8 + half * 4 + e4
                                o_ = p4[:, e4 * 128:(e4 + 1) * 128]
                                S.op("pe", lambda e, bb=bb, h=h, o_=o_: e.matmul(o_, ones128[:], bb["rhsA"][:, h, :], start=True, stop=False), reads=["ones128", k_("rhsA", d)], writes=[p4k])
                                S.op("pe", lambda e, d=d, o_=o_: e.matmul(o_, ident32[:], negm[:, d, :], start=False, stop=True), reads=["ident32", "negm"], writes=[p4k])
                            for e4 in range(4):
                                h = g * 8 + half * 4 + e4
                                S.op("act", lambda e, bb=bb, h=h, e4=e4, half=half, p4=p4, sgn=sgn: e.activation(bb[sgn][:, half * 4 + e4, :], p4[:, e4 * 128:(e4 + 1) * 128], AF.Exp, bias=bb["ncum"][:, h:h + 1]),
                                     reads=[p4k, k_("ncum", d)], writes=[k_(sgn, d)])
                        S.op("dve", lambda e, bb=bb, g=g, sgn=sgn, mtn=mtn: e.tensor_tensor(bb[mtn][:], bb[sgn][:], bb["CBT"][:, g, :].unsqueeze(1).to_broadcast([128, 8, 128]), ALU.mult),
                             reads=[k_(sgn, d), k_("CBT", d)], writes=[k_(mtn, d)])
                        py, pyk = nextps_(); pi_, pik = nextps_()
                        for e8 in range(8):
                            S.op("pe", lambda e, bb=bb, g=g, e8=e8, py=py, mtn=mtn: e.matmul(py[:, e8 * 64:(e8 + 1) * 64], bb[mtn][:, e8, :], bb["xdt"][:, g * 8 + e8, :], start=True, stop=True),
                                 reads=[k_(mtn, d), k_("xdt", d)], writes=[pyk])
                        S.op("pe", lambda e, bb=bb, g=g, pi_=pi_: e.matmul(pi_[:], bb["CF"][:, g, :], bb["Hb"][:, g, :], start=True, stop=True), reads=[k_("CF", d), k_("Hb", d)], writes=[pik])
                        S.op("dve", lambda e, bb=bb, g=g, pi_=pi_, tin=tin: e.tensor_tensor(bb[tin][:].rearrange("p (h q) -> p h q", h=8), pi_[:].rearrange("p (h q) -> p h q", h=8), bb["Ecum"][:, g * 8:(g + 1) * 8].unsqueeze(2).to_broadcast([128, 8, 64]), ALU.mult),
                             reads=[pik, k_("Ecum", d)], writes=[k_(tin, d)])
                        S.op("dve", lambda e, bb=bb, py=py, tin=tin, ytn=ytn: e.tensor_tensor(bb[ytn][:], py[:], bb[tin][:], ALU.add), reads=[pyk, k_(tin, d)], writes=[k_(ytn, d)])
                        l0 = t0 - CTX
                        S.dma("pool", YD_s[d, l0:l0 + 128, g * 512:(g + 1) * 512], bb[ytn][:], reads=[k_(ytn, d)])
                for g in range(4):
                    for d, c, t0, bb in info:
                        ph, phk = nextps_()
                        S.op("pe", lambda e, bb=bb, g=g, ph=ph: e.matmul(ph[:], bb["BT"][:, g * 128:(g + 1) * 128], bb["xs"][:, g * 8:(g + 1) * 8, :].rearrange("p h q -> p (h q)"), start=True, stop=True),
                             reads=[k_("BT", d), k_("xs", d)], writes=[phk])
                        S.op("dve", lambda e, bb=bb, g=g: e.tensor_tensor(bb["H32"][:, g, :].rearrange("p (h q) -> p h q", h=8), bb["H32"][:, g, :].rearrange("p (h q) -> p h q", h=8), bb["Etot"][:, g * 8:(g + 1) * 8].unsqueeze(2).to_broadcast([128, 8, 64]), ALU.mult),
                             reads=[k_("H32", d), k_("Etot", d)], writes=[k_("H32", d)])
                        S.op("dve", lambda e, bb=bb, g=g, ph=ph: e.tensor_tensor(bb["H32"][:, g, :], bb["H32"][:, g, :], ph[:], ALU.add), reads=[phk, k_("H32", d)], writes=[k_("H32", d)])
                        S.op("act", lambda e, bb=bb, g=g: e.copy(bb["Hb"][:, g, :], bb["H32"][:, g, :]), reads=[k_("H32", d)], writes=[k_("Hb", d)])
            S.barrier()

    if dbg == "S2":
        s2_scan(6)
        o = dout("dbg", [2, 512, 2048])
        S.dma("sp", o[0], YD_s[0, 0:512, :], reads=[])
        S.dma("sp", o[1], YD_s[1, SEQ - 512:SEQ, :], reads=[])
        S.finish()
        return
    s2_scan()

    YA_s = nc.dram_tensor("rw_ya", [1024, SEQ], BF16).ap()
    with contextlib.ExitStack() as st:
        lnw = sb("lnw", [64, 16], st=st); lnb = sb("lnb", [64, 16], st=st)
        S.dma("sp", lnw[:], rwlnw_d.rearrange("(h v) -> v h", v=64), writes=["lnw"], allow_slow_non_contiguous=True)
        S.dma("sp", lnb[:], rwlnb_d.rearrange("(h v) -> v h", v=64), writes=["lnb"], allow_slow_non_contiguous=True)
        gneps = sb("gneps", [64, 1], st=st)
        S.op("dve", lambda e: e.memset(gneps[:], 64e-5), writes=["gneps"])
        ones64 = sb("ones64", [64, 64], st=st)
        S.op("dve", lambda e: e.memset(ones64[:], 1.0), writes=["ones64"])
        def T2(nm, dt=F32):
            return [sb("r3%s_%d" % (nm, i), [64, 4, 128], dt, st=st) for i in range(2)]
        yf_, yb_, ysq_, mm_, vv_, yo_ = T2("yf"), T2("yb"), T2("ysq"), T2("mm"), T2("vv"), T2("yo", BF16)
        bvt_, gt_ = T2("bvt", BF16), T2("gt", BF16)
        it = 0
        for hg in range(1 if dbg == "R3" else 4):
            for blk in range(SEQ // 128):
                b = it % 2; it += 1
                sfx = "_%d" % b
                l0 = blk * 128
                yf, yb, ysq, mm, vv, yo, bvt, gt = (x[b] for x in (yf_, yb_, ysq_, mm_, vv_, yo_, bvt_, gt_))
                S.dma("sp", yf[:], YS[0, hg * 4:hg * 4 + 4, :, l0:l0 + 128].rearrange("h v t -> v h t"), writes=["yf" + sfx])
                S.dma("sp", yb[:], YS[1, hg * 4:hg * 4 + 4, :, l0:l0 + 128].rearrange("h v t -> v h t"), writes=["yb" + sfx])
                S.dma("sp", bvt[:], BV_s[hg * 256:(hg + 1) * 256, l0:l0 + 128].rearrange("(h v) t -> v h t", v=64), writes=["bvt" + sfx])
                S.dma("sp", gt[:], G_s[hg * 256:(hg + 1) * 256, l0:l0 + 128].rearrange("(h v) t -> v h t", v=64), writes=["gt" + sfx])
                S.op("dve", lambda e: e.tensor_tensor(yf[:], yf[:], yb[:], ALU.add), reads=["yf" + sfx, "yb" + sfx], writes=["yf" + sfx])
                S.op("act", lambda e: e.activation(ysq[:], yf[:], AF.Square), reads=["yf" + sfx], writes=["ysq" + sfx])
                p1, p1k = nextps(); p2, p2k = nextps()
                S.op("pe", lambda e: e.matmul(p1[0:64, :], ones64[:], yf[:].rearrange("p h t -> p (h t)"), start=True, stop=True), reads=["ones64", "yf" + sfx], writes=[p1k])
                S.op("pe", lambda e: e.matmul(p2[0:64, :], ones64[:], ysq[:].rearrange("p h t -> p (h t)"), start=True, stop=True), reads=["ones64", "ysq" + sfx], writes=[p2k])
                v3 = lambda t: t[0:64, :].rearrange("p (h t) -> p h t", h=4)
                S.op("act", lambda e: e.activation(mm[:], v3(p1), AF.Identity, scale=1.0 / 64), reads=[p1k], writes=["mm" + sfx])
                S.op("dve", lambda e: e.tensor_tensor(vv[:], mm[:], mm[:], ALU.mult), reads=["mm" + sfx], writes=["vv" + sfx])
                S.op("dve", lambda e: e.scalar_tensor_tensor(vv[:], v3(p2), 1.0 / 64, vv[:], ALU.mult, ALU.subtract), reads=[p2k, "vv" + sfx], writes=["vv" + sfx])
                S.op("act", lambda e: e.activation(vv[:], vv[:], AF.Sqrt, bias=gneps[:]), reads=["vv" + sfx, "gneps"], writes=["vv" + sfx])
                S.op("dve", lambda e: e.reciprocal(vv[:], vv[:]), reads=["vv" + sfx], writes=["vv" + sfx])
                S.op("dve", lambda e: e.tensor_tensor(yf[:], yf[:], mm[:], ALU.subtract), reads=["yf" + sfx, "mm" + sfx], writes=["yf" + sfx])
                S.op("dve", lambda e: e.tensor_tensor(yf[:], yf[:], vv[:], ALU.mult), reads=["yf" + sfx, "vv" + sfx], writes=["yf" + sfx])
                S.op("dve", lambda e: e.tensor_tensor(yf[:], yf[:], lnw[:, hg * 4:hg * 4 + 4].unsqueeze(2).to_broadcast([64, 4, 128]), ALU.mult), reads=["yf" + sfx, "lnw"], writes=["yf" + sfx])
                S.op("dve", lambda e: e.tensor_tensor(yf[:], yf[:], lnb[:, hg * 4:hg * 4 + 4].unsqueeze(2).to_broadcast([64, 4, 128]), ALU.add), reads=["yf" + sfx, "lnb"], writes=["yf" + sfx])
                S.op("dve", lambda e: e.tensor_tensor(yf[:], yf[:], bvt[:], ALU.add), reads=["yf" + sfx, "bvt" + sfx], writes=["yf" + sfx])
                S.op("dve", lambda e: e.tensor_tensor(yo[:], yf[:], gt[:], ALU.mult), reads=["yf" + sfx, "gt" + sfx], writes=["yo" + sfx])
                S.dma("pool", YA_s[hg * 256:(hg + 1) * 256, l0:l0 + 128].rearrange("(h v) t -> v h t", v=64), yo[:], reads=["yo" + sfx])
        S.barrier()
    if dbg == "R3":
        o = dout("dbg", [256, SEQ], BF16)
        S.dma("sp", o, YA_s[0:256, :], reads=[])
        S.finish()
        return

    ssmd_d = din("ssm_d", [32]); ssmn_d = din("ssm_norm", [2048]); projb_d = din("proj_b", [2048, 1024])
    proja_d = din("proj_a", [1024, 1024]); wout_d = din("w_out", [1024, 1024])
    n1post_d = din("norm1_post", [D]); n2pre_d = din("norm2_pre", [D]); n2post_d = din("norm2_post", [D])
    router_d = din("router", [D, 16])
    PBG_s = nc.dram_tensor("mg_pbg", [SEQ, 1024], BF16).ap()
    X1_s = nc.dram_tensor("x1_s", [SEQ, D], F32).ap()
    H2T_s = nc.dram_tensor("h2t_s", [D, SEQ], BF16).ap()
    AFT_s = nc.dram_tensor("aft_s", [16, SEQ], F32).ap()

    def rstd_of(ssq, n, key):
        S.op("act", lambda e: e.activation(ssq, ssq, AF.Sqrt, bias=epsc[:], scale=1.0 / n), reads=[key, "epsc"], writes=[key])
        S.op("dve", lambda e: e.reciprocal(ssq, ssq), reads=[key], writes=[key])

    with contextlib.ExitStack() as st:
        pbw = sb("pbw", [128, 16, 1024], BF16, st=st)
        stg = [sb("pbstg%d" % i, [128, 2, 1024], st=st) for i in range(2)]
        for q_ in range(8):
            S.dma("sp", stg[q_ % 2][:], projb_d[q_ * 256:(q_ + 1) * 256, :].rearrange("(cc p) d -> p cc d", p=128), writes=["pbstg%d" % (q_ % 2)])
            S.op("pool", lambda e, q_=q_: e.tensor_copy(pbw[:, 2 * q_:2 * q_ + 2, :], stg[q_ % 2][:]), reads=["pbstg%d" % (q_ % 2)], writes=["pbw"])
        Dbc = sb("Dbc", [128, 32], st=st); snb = sb("snb", [128, 2048], st=st)
        S.dma("sp", Dbc[:], ssmd_d.partition_broadcast(128), writes=["Dbc"])
        S.dma("sp", snb[:], ssmn_d.partition_broadcast(128), writes=["snb"])
        yf_ = [sb("s3yf%d" % i, [128, 2048], st=st) for i in range(2)]
        yb_ = [sb("s3yb%d" % i, [128, 2048], st=st) for i in range(2)]
        xt_ = [sb("s3xt%d" % i, [128, 2048], BF16, st=st) for i in range(2)]
        zs_ = [sb("s3zs%d" % i, [128, 2048], BF16, st=st) for i in range(2)]
        gb_ = [sb("s3gb%d" % i, [128, 1024], BF16, st=st) for i in range(2)]
        pbg_ = [sb("s3pbg%d" % i, [128, 1024], BF16, st=st) for i in range(2)]
        s3junk = sb("s3junk", [128, 2048], st=st)
        ynb = sb("s3ynb", [128, 2048], BF16, st=st); ynT = sb("s3ynT", [128, 16, 128], BF16, st=st)
        ssq_ = [sb("s3ss%d" % i, [128, 1], st=st) for i in range(2)]
        PBG_v = PBG_s.rearrange("(r c) d -> c r d", c=64)
        for tt in range(32 if dbg != "S3" else 2):
            b = tt % 2
            sfx = "%d" % b
            yf, yb, xt, zs, gbt, pbg, ssq = yf_[b], yb_[b], xt_[b], zs_[b], gb_[b], pbg_[b], ssq_[b]
            S.dma("sp", yf[:], YD_s[0, tt * 128:(tt + 1) * 128, :], writes=["s3yf" + sfx])
            S.dma("sp", yb[:], YD_s[1, tt * 128:(tt + 1) * 128, :], writes=["s3yb" + sfx])
            S.dma("sp", xt[:], XT_s[CTX + tt * 128:CTX + (tt + 1) * 128, :], writes=["s3xt" + sfx])
            S.dma("sp", zs[:], ZS_s[tt * 128:(tt + 1) * 128, :], writes=["s3zs" + sfx])
            S.dma("sp", gbt[:], GB_s[tt * 128:(tt + 1) * 128, :], writes=["s3gb" + sfx])
            S.op("dve", lambda e: e.tensor_tensor(yf[:], yf[:], yb[:], ALU.add), reads=["s3yf" + sfx, "s3yb" + sfx], writes=["s3yf" + sfx])
            S.op("dve", lambda e: e.tensor_tensor(yb[:].rearrange("p (h q) -> p h q", h=32), xt[:].rearrange("p (h q) -> p h q", h=32), Dbc[:].unsqueeze(2).to_broadcast([128, 32, 64]), ALU.mult),
                 reads=["s3xt" + sfx, "Dbc", "s3yb" + sfx], writes=["s3yb" + sfx])
            S.op("dve", lambda e: e.tensor_tensor(yf[:], yf[:], yb[:], ALU.add), reads=["s3yf" + sfx, "s3yb" + sfx], writes=["s3yf" + sfx])
            S.op("dve", lambda e: e.tensor_tensor(yf[:], yf[:], zs[:], ALU.mult), reads=["s3yf" + sfx, "s3zs" + sfx], writes=["s3yf" + sfx])
            S.op("act", lambda e: e.activation(s3junk[:], yf[:], AF.Square, accum_out=ssq[:]), reads=["s3yf" + sfx], writes=["s3junk", "s3ss" + sfx])
            rstd_of(ssq[:], 2048, "s3ss" + sfx)
            S.op("dve", lambda e: e.scalar_tensor_tensor(ynb[:], yf[:], ssq[:], snb[:], ALU.mult, ALU.mult), reads=["s3yf" + sfx, "s3ss" + sfx, "snb"], writes=["s3ynb"])
            for hh in range(2):
                for j in range(8):
                    cc = hh * 8 + j
                    S.op("pe", lambda e, cc=cc, j=j, hh=hh: e.transpose(PSB[hh][:, j * 128:(j + 1) * 128], ynb[:, cc * 128:(cc + 1) * 128], identb[:]), reads=["s3ynb", "identb"], writes=["PSB%d" % hh])
                S.op("act", lambda e, hh=hh: e.copy(ynT[:, hh * 8:(hh + 1) * 8, :], PSB[hh][:].rearrange("p (j t) -> p j t", j=8)), reads=["PSB%d" % hh], writes=["s3ynT"])
            for half in range(2):
                pt, pk = nextps_()
                for cc in range(16):
                    S.op("pe", lambda e, cc=cc, half=half, pt=pt: e.matmul(pt[:], ynT[:, cc, :], pbw[:, cc, half * 512:(half + 1) * 512], start=(cc == 0), stop=(cc == 15)), reads=["s3ynT", "pbw"], writes=[pk])
                S.op("dve", lambda e, half=half, pt=pt: e.tensor_tensor(pbg[:, half * 512:(half + 1) * 512], pt[:], gbt[:, half * 512:(half + 1) * 512], ALU.mult), reads=[pk, "s3gb" + sfx], writes=["s3pbg" + sfx])
            for j in range(2):
                S.dma("pool", PBG_v[2 * tt + j], pbg[j * 64:(j + 1) * 64, :], reads=["s3pbg" + sfx])
        S.barrier()
    if dbg == "S3":
        o = dout("dbg", [4, 64, 1024], BF16)
        S.dma("sp", o, PBG_v[0:4], reads=[])
        S.finish()
        return

    with contextlib.ExitStack() as st:
        def load_sq(name, src):
            wbf = sb(name, [128, 8, 1024], BF16, st=st)
            for q_ in range(4):
                S.dma("sp", stg2[q_ % 2][:], src[q_ * 256:(q_ + 1) * 256, :].rearrange("(cc p) d -> p cc d", p=128), writes=["mstg%d" % (q_ % 2)])
                S.op("pool", lambda e, q_=q_: e.tensor_copy(wbf[:, 2 * q_:2 * q_ + 2, :], stg2[q_ % 2][:]), reads=["mstg%d" % (q_ % 2)], writes=[name])
            return wbf
        stg2 = [sb("mstg%d" % i, [128, 2, 1024], st=st) for i in range(2)]
        paw = load_sq("paw", proja_d); wow = load_sq("wow", wout_d)
        rtr = sb("rtr", [128, 8, 16], st=st)
        S.dma("sp", rtr[:], router_d.rearrange("(k p) e -> p k e", p=128), writes=["rtr"])
        nb = sb("nbc", [128, 3, D], st=st)
        for i_, src in enumerate((n1post_d, n2pre_d, n2post_d)):
            S.dma("sp", nb[:, i_, :], src.partition_broadcast(128), writes=["nbc"])
        S.op("dve", lambda e: e.tensor_tensor(modL[:, 2 * D:3 * D], modL[:, 2 * D:3 * D], nb[:, 0, :], ALU.mult), reads=["nbc", "mod"], writes=["mod"])
        S.op("dve", lambda e: e.scalar_tensor_tensor(modL[:, 4 * D:5 * D], modL[:, 4 * D:5 * D], 1.0, nb[:, 1, :], ALU.add, ALU.mult), reads=["nbc", "mod"], writes=["mod"])
        S.op("dve", lambda e: e.tensor_tensor(modL[:, 5 * D:6 * D], modL[:, 5 * D:6 * D], nb[:, 2, :], ALU.mult), reads=["nbc", "mod"], writes=["mod"])
        S.op("dve", lambda e: e.tensor_copy(g2t[:], modL[:, 5 * D:6 * D]), reads=["mod"], writes=["g2t"])
        yat_ = [sb("myat%d" % i, [128, 8, 128], BF16, st=st) for i in range(2)]
        gat_ = [sb("mgat%d" % i, [128, 1024], BF16, st=st) for i in range(2)]
        pbt_ = [sb("mpbt%d" % i, [128, 1024], BF16, st=st) for i in range(2)]
        xin_ = [sb("mxin%d" % i, [128, 1024], st=st) for i in range(2)]
        mrg = sb("mmrg", [128, 1024], BF16, st=st); mrgT = sb("mmrgT", [128, 8, 128], BF16, st=st)
        tmpm = sb("mtmp", [128, 1024], st=st); ml = sb("mml", [128, 1024], st=st); mjunk = sb("mjunk", [128, 1024], st=st)
        x1_ = [sb("mx1%d" % i, [128, 1024], st=st) for i in range(2)]
        h2 = sb("mh2", [128, 1024], st=st); h2T32 = sb("mh2T32", [128, 8, 128], st=st); h2Tb_ = [sb("mh2Tb%d" % i, [128, 8, 128], BF16, st=st) for i in range(2)]
        lg = sb("mlg", [128, 16], st=st); lgs = sb("mlgs", [128, 1], st=st); affT_ = [sb("maffT%d" % i, [16, 128], st=st) for i in range(2)]
        ssa = sb("mssa", [128, 1], st=st); ssb = sb("mssb", [128, 1], st=st)
        YA_v = YA_s.rearrange("(cc p) t -> p cc t", p=128)
        H2T_v = H2T_s.rearrange("(k p) t -> p k t", p=128)
        for tt in range(32):
            b = tt % 2
            sfx = "%d" % b
            t0 = tt * 128
            yat, gat, pbt, xin, x1, h2Tb, affT = yat_[b], gat_[b], pbt_[b], xin_[b], x1_[b], h2Tb_[b], affT_[b]
            S.dma("sp", yat[:], YA_v[:, :, t0:t0 + 128], writes=["myat" + sfx])
            S.dma("sp", gat[:], GA_s[t0:t0 + 128, :], writes=["mgat" + sfx])
            S.dma("sp", pbt[:], PBG_s[t0:t0 + 128, :], writes=["mpbt" + sfx])
            S.dma("sp", xin[:], x_d[t0:t0 + 128, :], writes=["mxin" + sfx])
            for half in range(2):
                pt, pk = nextps_()
                for cc in range(8):
                    S.op("pe", lambda e, cc=cc, half=half, pt=pt: e.matmul(pt[:], yat[:, cc, :], paw[:, cc, half * 512:(half + 1) * 512], start=(cc == 0), stop=(cc == 7)), reads=["myat" + sfx, "paw"], writes=[pk])
                hs = slice(half * 512, (half + 1) * 512)
                S.op("dve", lambda e, pt=pt, hs=hs: e.tensor_tensor(tmpm[:, hs], pt[:], gat[:, hs], ALU.mult), reads=[pk, "mgat" + sfx], writes=["mtmp"])
                S.op("dve", lambda e, hs=hs: e.tensor_tensor(mrg[:, hs], tmpm[:, hs], pbt[:, hs], ALU.add), reads=["mtmp", "mpbt" + sfx], writes=["mmrg"])
            for j in range(8):
                S.op("pe", lambda e, j=j: e.transpose(PSB[0][:, j * 128:(j + 1) * 128], mrg[:, j * 128:(j + 1) * 128], identb[:]), reads=["mmrg", "identb"], writes=["PSB0"])
            S.op("act", lambda e: e.copy(mrgT[:], PSB[0][:].rearrange("p (j t) -> p j t", j=8)), reads=["PSB0"], writes=["mmrgT"])
            for half in range(2):
                pt, pk = nextps_()
                for k in range(8):
                    S.op("pe", lambda e, k=k, half=half, pt=pt: e.matmul(pt[:], mrgT[:, k, :], wow[:, k, half * 512:(half + 1) * 512], start=(k == 0), stop=(k == 7)), reads=["mmrgT", "wow"], writes=[pk])
                S.op("act", lambda e, half=half, pt=pt: e.copy(ml[:, half * 512:(half + 1) * 512], pt[:]), reads=[pk], writes=["mml"])
            S.op("act", lambda e: e.activation(mjunk[:], ml[:], AF.Square, accum_out=ssa[:]), reads=["mml"], writes=["mjunk", "mssa"])
            rstd_of(ssa[:], D, "mssa")
            S.op("dve", lambda e: e.scalar_tensor_tensor(tmpm[:], ml[:], ssa[:], modL[:, 2 * D:3 * D], ALU.mult, ALU.mult), reads=["mml", "mssa", "mod"], writes=["mtmp"])
            S.op("dve", lambda e: e.tensor_tensor(x1[:], tmpm[:], xin[:], ALU.add), reads=["mtmp", "mxin" + sfx], writes=["mx1" + sfx])
            S.dma("pool", X1_s[t0:t0 + 128, :], x1[:], reads=["mx1" + sfx])
            S.op("act", lambda e: e.activation(mjunk[:], x1[:], AF.Square, accum_out=ssb[:]), reads=["mx1" + sfx], writes=["mjunk", "mssb"])
            rstd_of(ssb[:], D, "mssb")
            S.op("dve", lambda e: e.scalar_tensor_tensor(h2[:], x1[:], ssb[:], modL[:, 4 * D:5 * D], ALU.mult, ALU.mult), reads=["mx1" + sfx, "mssb", "mod"], writes=["mh2"])
            S.op("dve", lambda e: e.tensor_tensor(h2[:], h2[:], modL[:, 3 * D:4 * D], ALU.add), reads=["mh2", "mod"], writes=["mh2"])
            for hh in range(2):
                pt, pk = nextps_()
                for j in range(4):
                    kk_ = hh * 4 + j
                    S.op("pe", lambda e, j=j, kk_=kk_, pt=pt: e.transpose(pt[:, j * 128:(j + 1) * 128], h2[:, kk_ * 128:(kk_ + 1) * 128], ident32[:]), reads=["mh2", "ident32"], writes=[pk])
                S.op("act", lambda e, hh=hh, pt=pt: e.copy(h2T32[:, hh * 4:(hh + 1) * 4, :], pt[:].rearrange("p (j t) -> p j t", j=4)), reads=[pk], writes=["mh2T32"])
            S.op("dve", lambda e: e.tensor_copy(h2Tb[:], h2T32[:]), reads=["mh2T32"], writes=["mh2Tb" + sfx])
            S.dma("pool", H2T_v[:, :, t0:t0 + 128], h2Tb[:], reads=["mh2Tb" + sfx])
            pt, pk = nextps_()
            for k in range(8):
                S.op("pe", lambda e, k=k, pt=pt: e.matmul(pt[:, 0:16], h2T32[:, k, :], rtr[:, k, :], start=(k == 0), stop=(k == 7)), reads=["mh2T32", "rtr"], writes=[pk])
            S.op("act", lambda e, pt=pt: e.activation(lg[:], pt[:, 0:16], AF.Exp, accum_out=lgs[:]), reads=[pk], writes=["mlg", "mlgs"])
            S.op("dve", lambda e: e.reciprocal(lgs[:], lgs[:]), reads=["mlgs"], writes=["mlgs"])
            S.op("dve", lambda e: e.tensor_scalar(lg[:], lg[:], lgs[:], None, ALU.mult), reads=["mlg", "mlgs"], writes=["mlg"])
            pt2, pk2 = nextps_()
            S.op("pe", lambda e, pt2=pt2: e.transpose(pt2[0:16, 0:128], lg[:], ident32[:]), reads=["mlg", "ident32"], writes=[pk2])
            S.op("act", lambda e, pt2=pt2: e.copy(affT[:], pt2[0:16, 0:128]), reads=[pk2], writes=["maffT" + sfx])
            S.dma("pool", AFT_s[:, t0:t0 + 128], affT[:], reads=["maffT" + sfx])
        S.barrier()
    if dbg == "M":
        o = dout("dbgX1", [512, D]); S.dma("sp", o, X1_s[1024:1536], reads=[])
        o = dout("dbgH2", [D, 512], BF16); S.dma("sp", o, H2T_s[:, 1024:1536], reads=[])
        o = dout("dbgAF", [16, SEQ]); S.dma("sp", o, AFT_s, reads=[])
        S.finish()
        return

    stM.close()

    w1_d = din("exp_w1", [16, D, 1024]); w3_d = din("exp_w3", [16, D, 1024]); w2_d = din("exp_w2", [16, 1024, D])
    selm_d = din("selm", [16, 16, 128])
    GM_s = nc.dram_tensor("gm_s", [16, SEQ], F32).ap()
    CAP = 2 * SEQ // 16
    with contextlib.ExitStack() as st:
        aff = sb("eaff", [16, SEQ], st=st); cmp_ = sb("ecmp", [16, SEQ], st=st)
        S.dma("sp", aff[:], AFT_s, writes=["eaff"])
        lo = sb("elo", [16, 1], st=st); hi = sb("ehi", [16, 1], st=st); mid = sb("emid", [16, 1], st=st)
        cnt = sb("ecnt", [16, 1], st=st); sel = sb("esel", [16, 1], st=st); dl = sb("edl", [16, 1], st=st); halfc = sb("ehalf", [16, 1], st=st)
        S.op("dve", lambda e: e.memset(lo[:], 0.0), writes=["elo"])
        S.op("dve", lambda e: e.memset(hi[:], 1.0), writes=["ehi"])
        S.op("dve", lambda e: e.memset(halfc[:], 0.5), writes=["ehalf"])
        for it in range(36):
            S.op("dve", lambda e: e.scalar_tensor_tensor(mid[:], lo[:], hi[:, 0:1], halfc[:], ALU.add, ALU.mult), reads=["elo", "ehi", "ehalf"], writes=["emid"])
            S.op("dve", lambda e: e.tensor_scalar(cmp_[:], aff[:], mid[:, 0:1], None, ALU.is_ge), reads=["eaff", "emid"], writes=["ecmp"])
            S.op("dve", lambda e: e.reduce_sum(cnt[:], cmp_[:], AX.X), reads=["ecmp"], writes=["ecnt"])
            S.op("dve", lambda e: e.tensor_scalar(sel[:], cnt[:], CAP - 0.5, None, ALU.is_ge), reads=["ecnt"], writes=["esel"])
            S.op("dve", lambda e: e.tensor_tensor(dl[:], mid[:], lo[:], ALU.subtract), reads=["emid", "elo"], writes=["edl"])
            S.op("dve", lambda e: e.scalar_tensor_tensor(lo[:], dl[:], sel[:, 0:1], lo[:], ALU.mult, ALU.add), reads=["edl", "esel", "elo"], writes=["elo"])
            S.op("dve", lambda e: e.tensor_tensor(dl[:], hi[:], mid[:], ALU.subtract), reads=["emid", "ehi"], writes=["edl"])
            S.op("dve", lambda e: e.scalar_tensor_tensor(hi[:], dl[:], sel[:, 0:1], mid[:], ALU.mult, ALU.add), reads=["edl", "esel", "emid"], writes=["ehi"])
        S.op("dve", lambda e: e.tensor_scalar(cmp_[:], aff[:], lo[:, 0:1], None, ALU.is_ge), reads=["eaff", "elo"], writes=["ecmp"])
        S.op("dve", lambda e: e.tensor_tensor(cmp_[:], cmp_[:], aff[:], ALU.mult), reads=["ecmp", "eaff"], writes=["ecmp"])
        S.dma("sp", GM_s, cmp_[:], reads=["ecmp"])
        S.barrier()
    if dbg == "E0":
        o = dout("dbg", [16, SEQ]); S.dma("sp", o, GM_s, reads=[])
        S.finish()
        return

    with contextlib.ExitStack() as st:
        selm = sb("selm_sb", [16, 16, 128], st=st)
        S.dma("sp", selm[:], selm_d, writes=["selm"])
        QT = 1024
        h2q = sb("eh2q", [128, 8, QT], BF16, st=st)
        yacc = sb("eyacc", [128, 8, QT], st=st)
        gmq = sb("egmq", [16, QT], st=st)
        wbs = [[sb("ew%d_%d" % (i, sset), [128, 8, 1024], BF16, st=st) for i in range(3)] for sset in range(2)]
        hg = sb("ehg", [128, 8, 512], BF16, st=st)
        sa_ = [sb("esa%d" % i, [128, 512], st=st) for i in range(2)]
        hb_ = [sb("ehb%d" % i, [128, 512], st=st) for i in range(2)]
        gmbs = sb("egmbs", [128, 512], st=st)
        ytok = sb("eytok", [128, D], st=st); x1t = sb("ex1t", [128, D], st=st); ejunk = sb("ejunk", [128, D], st=st)
        ess = sb("eess", [128, 1], st=st)
        NQ = SEQ // QT
        nq_run = NQ if dbg != "E1" else 1

        def load_expert(n):
            ex_ = n % 16
            for wi, src in enumerate((w1_d[ex_], w3_d[ex_], w2_d[ex_])):
                S.dma("pool", wbs[n % 2][wi][:], src.rearrange("(cc p) d -> p cc d", p=128), writes=["ew%d_%d" % (wi, n % 2)])

        load_expert(0)
        for qi in range(nq_run):
            T0 = qi * QT
            S.dma("sp", h2q[:], H2T_v[:, :, T0:T0 + QT], writes=["eh2q"])
            S.dma("sp", gmq[:], GM_s[:, T0:T0 + QT], writes=["egmq"])
            S.op("pool", lambda e: e.memset(yacc[:], 0.0), writes=["eyacc"])
            for ex in range(16):
                n_ = qi * 16 + ex
                if n_ + 1 < nq_run * 16:
                    load_expert(n_ + 1)
                wb = wbs[n_ % 2]
                wk = ["ew%d_%d" % (wi, n_ % 2) for wi in range(3)]
                for tg in range(QT // 512):
                    tl = tg * 512
                    pg, pgk = nextps_()
                    S.op("pe", lambda e, pg=pg: e.matmul(pg[:], selm[:, ex, :], gmq[:, tl:tl + 512], start=True, stop=True), reads=["selm", "egmq"], writes=[pgk])
                    S.op("act", lambda e, pg=pg: e.copy(gmbs[:], pg[:]), reads=[pgk], writes=["egmbs"])
                    for ft in range(8):
                        b = ft % 2
                        pa, pak = nextps_(); pb, pbk = nextps_()
                        for k in range(8):
                            S.op("pe", lambda e, k=k, pa=pa: e.matmul(pa[:], wb[0][:, k, ft * 128:(ft + 1) * 128], h2q[:, k, tl:tl + 512], start=(k == 0), stop=(k == 7)), reads=[wk[0], "eh2q"], writes=[pak])
                        for k in range(8):
                            S.op("pe", lambda e, k=k, pb=pb: e.matmul(pb[:], wb[1][:, k, ft * 128:(ft + 1) * 128], h2q[:, k, tl:tl + 512], start=(k == 0), stop=(k == 7)), reads=[wk[1], "eh2q"], writes=[pbk])
                        S.op("act", lambda e, pa=pa: e.activation(sa_[b][:], pa[:], AF.Silu), reads=[pak], writes=["esa%d" % b])
                        S.op("dve", lambda e, pb=pb: e.tensor_tensor(hb_[b][:], sa_[b][:], pb[:], ALU.mult), reads=["esa%d" % b, pbk], writes=["ehb%d" % b])
                        S.op("pool", lambda e: e.tensor_tensor(hg[:, ft, :], hb_[b][:], gmbs[:], ALU.mult), reads=["ehb%d" % b, "egmbs"], writes=["ehg"])
                    for dt_ in range(8):
                        py, pyk = nextps_()
                        for ft in range(8):
                            S.op("pe", lambda e, ft=ft, py=py: e.matmul(py[:], wb[2][:, ft, dt_ * 128:(dt_ + 1) * 128], hg[:, ft, :], start=(ft == 0), stop=(ft == 7)), reads=[wk[2], "ehg"], writes=[pyk])
                        S.op("dve", lambda e, py=py: e.tensor_tensor(yacc[:, dt_, tl:tl + 512], yacc[:, dt_, tl:tl + 512], py[:], ALU.add), reads=[pyk, "eyacc"], writes=["eyacc"])
            for tt in range(QT // 128):
                t0 = T0 + tt * 128
                S.dma("sp", x1t[:], X1_s[t0:t0 + 128, :], writes=["ex1t"])
                for hh in range(2):
                    pt, pk = nextps_()
                    for j in range(4):
                        dd = hh * 4 + j
                        S.op("pe", lambda e, j=j, dd=dd, pt=pt: e.transpose(pt[:, j * 128:(j + 1) * 128], yacc[:, dd, tt * 128:(tt + 1) * 128], ident32[:]), reads=["eyacc", "ident32"], writes=[pk])
                    S.op("act", lambda e, hh=hh, pt=pt: e.copy(ytok[:, hh * 512:(hh + 1) * 512], pt[:]), reads=[pk], writes=["eytok"])
                S.op("act", lambda e: e.activation(ejunk[:], ytok[:], AF.Square, accum_out=ess[:]), reads=["eytok"], writes=["ejunk", "eess"])
                rstd_of(ess[:], D, "eess")
                S.op("dve", lambda e: e.scalar_tensor_tensor(ytok[:], ytok[:], ess[:], g2t[:], ALU.mult, ALU.mult), reads=["eytok", "eess", "g2t"], writes=["eytok"])
                S.op("dve", lambda e: e.tensor_tensor(ytok[:], ytok[:], x1t[:], ALU.add), reads=["eytok", "ex1t"], writes=["eytok"])
                if out_d is not None:
                    S.dma("pool", out_d[t0:t0 + 128, :], ytok[:], reads=["eytok"])
                elif dbg == "E1":
                    if tt == 0:
                        dbg_o = dout("dbg", [QT, D])
                    S.dma("pool", dbg_o[tt * 128:(tt + 1) * 128, :], ytok[:], reads=["eytok"])
        S.barrier()

    S.finish()


def _prep_inputs(inputs):
    sq = {}
    for k, v in inputs.items():
        v = np.asarray(v)
        if k in ("x", "c", "ctx", "c_ctx"):
            sq[k] = v
        elif k == "rw_rk":
            sq[k] = v[0].reshape(1024)
        elif k in ("ssm_dt_bias", "ssm_a_log"):
            sq[k] = v[0].reshape(64)
        else:
            sq[k] = v[0]
    return sq


def kernel(**inputs):
    sq = _prep_inputs(inputs)
    nc = build(None)
    cst = consts()
    in_maps = []
    for b in range(8):
        full = dict(sq)
        full["x"] = sq["x"][b]
        full["ctx"] = sq["ctx"][b]
        full["c"] = sq["c"][b]
        full.update(cst)
        in_maps.append({k: np.ascontiguousarray(full[k], dtype=np.float32) for k in INPUT_NAMES})
    res = run_bass_kernel_spmd(nc, in_maps, core_ids=list(range(8)))
    return np.stack([np.asarray(r["out"], dtype=np.float32) for r in res.results], axis=0)
```

```python
import contextlib
import math
import numpy as np
import ml_dtypes
import concourse.bass as bass
import concourse.mybir as mybir
from concourse.bass_utils import run_bass_kernel_spmd

F32 = mybir.dt.float32
BF16 = mybir.dt.bfloat16
AF = mybir.ActivationFunctionType
ALU = mybir.AluOpType
AX = mybir.AxisListType

D = 1024
SEQ = 4096
CTX = 256
NTOK = SEQ + CTX
PADW = NTOK + 4
C0 = 1
L0 = CTX + 3
N_IN = 10688
EPS = 1e-6

SEM_CAP = 24000
NDMA_SEM = 12


class Sched:
    def __init__(self, nc, stack):
        self.nc = nc
        self.stack = stack
        self.engs = {"pe": nc.tensor, "dve": nc.vector, "act": nc.scalar, "pool": nc.gpsimd, "sp": nc.sync}
        self.sem = {}
        self.cnt = {}
        self.nsem = 0
        for e in self.engs:
            self._new_sem(e)
        self.dq = {"sp": "sp", "pool": "pool", "act": "act"}
        self.dsem = {q: [self._mk_sem("d%s%d" % (q, i)) for i in range(NDMA_SEM)] for q in self.dq}
        self.dcnt = {q: 0 for q in self.dq}
        self.seen = {e: {} for e in self.engs}
        self.lastw = {}
        self.reads = {}
        self.nins = 0

    def _mk_sem(self, name):
        self.nsem += 1
        return self.stack.enter_context(self.nc.semaphore("s%d_%s" % (self.nsem, name)))

    def _new_sem(self, e):
        self.sem[e] = self._mk_sem(e)
        self.cnt[e] = 0

    def _wait(self, e, ev):
        sem, val, src = ev
        sid = id(sem)
        if self.seen[e].get(sid, 0) >= val:
            return
        self.seen[e][sid] = val
        self.engs[e].wait_ge(sem, val)
        self.nins += 1

    def _deps(self, e, reads, writes, me):
        evs = []
        for k in reads:
            w = self.lastw.get(k)
            if w is not None and not (w[2] == me and me == "pe"):
                evs.append(w)
        for k in writes:
            w = self.lastw.get(k)
            if w is not None and w[2] != me:
                evs.append(w)
            for r in self.reads.get(k, ()):
                if r[2] != me:
                    evs.append(r)
        for ev in evs:
            self._wait(e, ev)

    def _record(self, ev, reads, writes):
        for k in reads:
            lst = self.reads.setdefault(k, [])
            lst.append(ev)
            if len(lst) > 12:
                d = {}
                for r in lst:
                    d[(r[2], id(r[0]))] = r
                self.reads[k] = list(d.values())
        for k in writes:
            self.lastw[k] = ev
            self.reads[k] = []

    def op(self, e, fn, reads=(), writes=()):
        self._deps(e, reads, writes, e)
        if self.cnt[e] >= SEM_CAP:
            self._new_sem(e)
        ins = fn(self.engs[e])
        self.cnt[e] += 1
        ins.then_inc(self.sem[e], 1)
        self.nins += 1
        ev = (self.sem[e], self.cnt[e], e)
        self._record(ev, reads, writes)
        return ev

    def dma(self, q, out, in_, reads=(), writes=(), **kw):
        e = self.dq[q]
        self._deps(e, reads, writes, None)
        i = self.dcnt[q]
        slot = i % NDMA_SEM
        rnd = i // NDMA_SEM
        sem = self.dsem[q][slot]
        if rnd > 0:
            self._wait(e, (sem, 16 * rnd, "dma" + q))
        self.engs[e].dma_start(out=out, in_=in_, **kw).then_inc(sem, 16)
        self.dcnt[q] += 1
        self.nins += 1
        ev = (sem, 16 * (rnd + 1), "dma" + q)
        self._record(ev, reads, writes)
        return ev

    def _all_events(self):
        evs = []
        for e in self.engs:
            if self.cnt[e] > 0:
                evs.append((self.sem[e], self.cnt[e], e))
        for q in self.dq:
            n = self.dcnt[q]
            for slot in range(NDMA_SEM):
                k = (n - slot + NDMA_SEM - 1) // NDMA_SEM if n > slot else 0
                if k > 0:
                    evs.append((self.dsem[q][slot], 16 * k, "dma" + q))
        return evs

    def barrier(self):
        evs = self._all_events()
        for e in self.engs:
            for ev in evs:
                if ev[2] != e:
                    self._wait(e, ev)
        self.lastw.clear()
        self.reads.clear()

    def finish(self, e="sp"):
        for ev in self._all_events():
            if ev[2] != e:
                self._wait(e, ev)


INPUT_NAMES = []


def consts():
    i = np.arange(128)
    row, col = i[:, None], i[None, :]
    masks = np.stack([col > row, col >= row, col < row, col <= row]).astype(np.float32)
    blk = (row // 64 == col // 64).astype(np.float32)
    selm = np.zeros((16, 16, 128), np.float32)
    for e_ in range(16):
        selm[e_, e_, :] = 1.0
    return {"ident": np.eye(128, dtype=np.float32), "masks": masks, "blk64": blk, "selm": selm}


def build(dbg=None):
    nc = bass.Bass("TRN2", target_bir_lowering=False)
    stack = contextlib.ExitStack()
    with stack:
        _emit(nc, stack, dbg)
    return nc


def _emit(nc, stack, dbg):
    S = Sched(nc, stack)

    del INPUT_NAMES[:]

    def din(name, shape, dt=F32):
        INPUT_NAMES.append(name)
        return nc.dram_tensor(name, list(shape), dt, kind="ExternalInput").ap()

    def dout(name, shape, dt=F32):
        return nc.dram_tensor(name, list(shape), dt, kind="ExternalOutput").ap()

    def sb(name, shape, dt=F32, st=None):
        return (st or stack).enter_context(nc.sbuf_tensor(name, list(shape), dt))

    def ps(name, shape, dt=F32, st=None):
        return (st or stack).enter_context(nc.psum_tensor(name, list(shape), dt))

    x_d = din("x", [SEQ, D])
    ctx_d = din("ctx", [CTX, D])
    c_d = din("c", [D])
    cctx_d = din("c_ctx", [D])
    adaw_d = din("ada_w", [D, 6 * D])
    adab_d = din("ada_b", [6 * D])
    n1pre_d = din("norm1_pre", [D])
    win_d = din("w_in", [D, N_IN])
    ident_d = din("ident", [128, 128])
    out_d = dout("out", [SEQ, D]) if dbg is None else None

    ident32 = sb("ident32", [128, 128])
    identb = sb("identb", [128, 128], BF16)
    S.dma("sp", ident32[:], ident_d, writes=["ident32"])
    S.op("dve", lambda e: e.tensor_copy(identb[:], ident32[:]), reads=["ident32"], writes=["identb"])

    blk_d = din("blk64", [128, 128]); masks_d = din("masks", [4, 128, 128])
    blk32 = sb("blk32", [128, 128])
    S.dma("sp", blk32[:], blk_d, writes=["blk32"])
    msk = sb("msk", [128, 4, 128])
    S.dma("sp", msk[:], masks_d.rearrange("m p c -> p m c"), writes=["msk"])
    onec = sb("onec", [128, 1])
    S.op("dve", lambda e: e.memset(onec[:], 1.0), writes=["onec"])
    epsc = sb("epsc", [128, 1])
    S.op("dve", lambda e: e.memset(epsc[:], EPS), writes=["epsc"])
    S.barrier()

    PS = [ps("ps%d" % i, [128, 512]) for i in range(6)]
    PSB = [ps("psb%d" % i, [128, 1024], BF16) for i in range(2)]

    def run_lanes(items, body, L):
        items = list(items)
        pos = [0]
        active = {}
        for lane in range(L):
            if pos[0] < len(items):
                active[lane] = body(items[pos[0]], lane); pos[0] += 1
        while active:
            for lane in list(active.keys()):
                try:
                    next(active[lane])
                except StopIteration:
                    if pos[0] < len(items):
                        active[lane] = body(items[pos[0]], lane); pos[0] += 1
                    else:
                        del active[lane]

    psn = [0]

    def nextps_():
        i = psn[0] % 6
        psn[0] += 1
        return PS[i], "PS%d" % i

    g2t = sb("g2t", [128, D])
    stM = contextlib.ExitStack()
    stack.callback(stM.close)
    modL = sb("modL", [128, 6 * D], st=stM)
    modC = sb("modC", [128, 2 * D], st=stM)
    with contextlib.ExitStack() as st:
        cs = sb("cs", [128, 2, 8], st=st)
        S.dma("sp", cs[:, 0, :], c_d.rearrange("(k p) -> p k", p=128), writes=["cs"], allow_slow_non_contiguous=True)
        S.dma("sp", cs[:, 1, :], cctx_d.rearrange("(k p) -> p k", p=128), writes=["cs"], allow_slow_non_contiguous=True)
        css = sb("css", [128, 2, 8], st=st)
        S.op("act", lambda e: e.activation(css[:], cs[:], AF.Silu), reads=["cs"], writes=["css"])
        cbc = sb("cbc", [128, 2, 8, 128], st=st)
        for w in range(2):
            S.op("dve", lambda e, w=w: e.tensor_copy(cbc[:, w], css[:, w, :].unsqueeze(2).to_broadcast([128, 8, 128])),
                 reads=["css"], writes=["cbc"])
        adab = sb("adab", [128, 6 * D], st=st)
        S.dma("sp", adab[:], adab_d.partition_broadcast(128), writes=["adab"])
        wbuf = [sb("adawbuf%d" % i, [128, 8, 512], st=st) for i in range(2)]
        jobs = [(0, j) for j in range(12)] + [(1, j) for j in range(4)]
        for n, (w, j) in enumerate(jobs):
            wb = wbuf[n % 2]
            S.dma("sp", wb[:], adaw_d[:, j * 512:(j + 1) * 512].rearrange("(k p) c -> p k c", p=128),
                  writes=["adaw%d" % (n % 2)])
            pt = PS[n % 2]
            for k in range(8):
                S.op("pe", lambda e, k=k, wb=wb, pt=pt, w=w: e.matmul(pt[:], cbc[:, w, k, :], wb[:, k, :], start=(k == 0), stop=(k == 7)),
                     reads=["cbc", "adaw%d" % (n % 2)], writes=["PS%d" % (n % 2)])
            dst = (modL if w == 0 else modC)[:, j * 512:(j + 1) * 512]
            S.op("dve", lambda e, dst=dst, pt=pt, j=j: e.tensor_tensor(dst, pt[:], adab[:, j * 512:(j + 1) * 512], ALU.add),
                 reads=["PS%d" % (n % 2), "adab"], writes=["mod"])
        n1pre = sb("n1pre", [128, D], st=st)
        S.dma("sp", n1pre[:], n1pre_d.partition_broadcast(128), writes=["n1pre"])
        for m in (modL, modC):
            S.op("dve", lambda e, m=m: e.scalar_tensor_tensor(m[:, D:2 * D], m[:, D:2 * D], 1.0, n1pre[:], ALU.add, ALU.mult),
                 reads=["mod", "n1pre"], writes=["mod"])
        S.barrier()

    stH = contextlib.ExitStack()
    stack.callback(stH.close)
    hT = sb("hT", [128, 8, PADW], BF16, st=stH)
    S.op("pool", lambda e: e.memset(hT[:, :, 0:1], 0.0), writes=["hT"])
    S.op("pool", lambda e: e.memset(hT[:, :, CTX + 1:CTX + 3], 0.0), writes=["hT"])
    S.op("pool", lambda e: e.memset(hT[:, :, PADW - 1:PADW], 0.0), writes=["hT"])
    with contextlib.ExitStack() as st:
        xt = [sb("xt%d" % i, [128, D], st=st) for i in range(2)]
        junk = sb("junk", [128, D], st=st)
        hb = [sb("hb%d" % i, [128, D], BF16, st=st) for i in range(2)]
        t32 = sb("t32", [128, D], st=st)
        ss = [sb("ss%d" % i, [128, 1], st=st) for i in range(2)]
        for tt in range(NTOK // 128):
            b = tt % 2
            src = ctx_d[tt * 128:(tt + 1) * 128, :] if tt < 2 else x_d[(tt - 2) * 128:(tt - 1) * 128, :]
            m = modC if tt < 2 else modL
            S.dma("sp", xt[b][:], src, writes=["xt%d" % b])
            S.op("act", lambda e, b=b: e.activation(junk[:], xt[b][:], AF.Square, accum_out=ss[b][:]),
                 reads=["xt%d" % b], writes=["junk", "ss%d" % b])
            S.op("act", lambda e, b=b: e.activation(ss[b][:], ss[b][:], AF.Sqrt, bias=epsc[:], scale=1.0 / D),
                 reads=["ss%d" % b], writes=["ss%d" % b])
            S.op("dve", lambda e, b=b: e.reciprocal(ss[b][:], ss[b][:]),
                 reads=["ss%d" % b], writes=["ss%d" % b])
            S.op("dve", lambda e, b=b, m=m: e.scalar_tensor_tensor(t32[:], xt[b][:], ss[b][:], m[:, D:2 * D], ALU.mult, ALU.mult),
                 reads=["xt%d" % b, "ss%d" % b, "mod"], writes=["t32"])
            S.op("pool", lambda e, b=b, m=m: e.tensor_tensor(hb[b][:], t32[:], m[:, 0:D], ALU.add),
                 reads=["t32", "mod"], writes=["hb%d" % b])
            pt = PSB[b]
            for k in range(8):
                S.op("pe", lambda e, k=k, b=b, pt=pt: e.transpose(pt[:, k * 128:(k + 1) * 128], hb[b][:, k * 128:(k + 1) * 128], identb[:]),
                     reads=["hb%d" % b, "identb"], writes=["PSB%d" % b])
            pp = (C0 + tt * 128) if tt < 2 else (L0 + (tt - 2) * 128)
            S.op("act", lambda e, pp=pp, pt=pt: e.copy(hT[:, :, pp:pp + 128], pt[:].rearrange("p (k t) -> p k t", k=8)),
                 reads=["PSB%d" % b], writes=["hT"])
        S.barrier()

    if dbg == "B":
        o = dout("dbg", [128, 8, PADW], BF16)
        S.dma("sp", o, hT[:], reads=["hT"])
        o2 = dout("dbg2", [128, 6 * D])
        S.dma("sp", o2, modL[:], reads=["mod"])
        S.finish()
        return

    rwmu_d = din("rw_mu", [2, 3456]); rww0_d = din("rw_w0", [2, 1024]); rww2_d = din("rw_w2", [2, 64, 1024])
    rwa0_d = din("rw_a0", [2, 1024]); rwa2_d = din("rw_a2", [2, 64, 1024]); rwg2_d = din("rw_g2", [128, 1024])
    rwkk_d = din("rw_kk", [1024]); rwka_d = din("rw_ka", [1024]); rwrk_d = din("rw_rk", [1024])
    rwlnw_d = din("rw_ln_w", [1024]); rwlnb_d = din("rw_ln_b", [1024])
    A2_s = nc.dram_tensor("rw_a2s", [1024, 2, NTOK], BF16).ap()
    D2_s = nc.dram_tensor("rw_d2s", [2, 1024, 2, NTOK], BF16).ap()
    LWT_s = nc.dram_tensor("rw_lwt", [2, NTOK, 1024], F32).ap()
    VT_s = nc.dram_tensor("rw_vt", [NTOK, 1024], BF16).ap()
    G_s = nc.dram_tensor("rw_gs", [1024, SEQ], BF16).ap()
    BV_s = nc.dram_tensor("rw_bvs", [1024, SEQ], BF16).ap()
    SKIP_RW = dbg in ("S1", "S2", "S3")
    TG = [(C0, 256, 0)] + [(L0 + 256 * g, 256, CTX + 256 * g) for g in range(16)]
    NEG_E = -math.exp(-0.5)

    with contextlib.ExitStack() as st:
        def cols(name, src):
            t = sb(name, [128, 8], st=st)
            S.dma("sp", t[:], src.rearrange("(h p) -> p h", p=128), writes=[name], allow_slow_non_contiguous=True)
            return t
        kkw = cols("kkw", rwkk_d); kaw = cols("kaw", rwka_d); rkw = cols("rkw", rwrk_d)
        w0c = [cols("w0c%d" % d, rww0_d[d]) for d in range(2)]
        a0c = [cols("a0c%d" % d, rwa0_d[d]) for d in range(2)]
        omka = sb("omka", [128, 8], st=st)
        S.op("dve", lambda e: e.tensor_scalar(omka[:], kaw[:], -1.0, 1.0, ALU.mult, ALU.add), reads=["kaw"], writes=["omka"])
        mu_all = sb("mu_all", [128, 27, 2], st=st)
        for m_ in range(2):
            S.dma("sp", mu_all[:, :, m_], rwmu_d[m_].rearrange("(t p) -> p t", p=128), writes=["mu_all"], allow_slow_non_contiguous=True)
        c0_all = sb("c0_all", [128, 27], st=st)
        S.op("dve", lambda e: e.tensor_tensor(c0_all[:], mu_all[:, :, 0], mu_all[:, :, 1], ALU.add), reads=["mu_all"], writes=["c0_all"])
        S.op("dve", lambda e: e.tensor_scalar(c0_all[:], c0_all[:], -1.0, 1.0, ALU.mult, ALU.add), reads=["c0_all"], writes=["c0_all"])
        lw32 = sb("lw32", [128, 1024], st=st)
        w2b = sb("w2b", [128, 1024], BF16, st=st); a2b = sb("a2b", [128, 1024], BF16, st=st); g2b = sb("g2b", [128, 1024], BF16, st=st)
        for dst, src in ((w2b, rww2_d.rearrange("d e c -> (d e) c")), (a2b, rwa2_d.rearrange("d e c -> (d e) c")), (g2b, rwg2_d)):
            S.dma("sp", lw32[:], src, writes=["lw32"])
            S.op("dve", lambda e, dst=dst: e.tensor_copy(dst[:], lw32[:]), reads=["lw32"], writes=["lorab"])
        th = sb("th", [128, NTOK], BF16, st=st); xab = sb("xab", [128, NTOK], BF16, st=st); sg = sb("sg", [128, NTOK], BF16, st=st)
        wl_ = [[sb("r1w%d_%d" % (i, ln), [128, 8, 128], BF16, st=st) for i in range(3)] for ln in range(2)]
        nw = [0]

        def load_w(ct):
            i = nw[0] % 3; nw[0] += 1
            b = wl_[0][i]
            S.dma("pool", b[:], win_d[:, ct * 128:(ct + 1) * 128].rearrange("(k p) c -> p k c", p=128), writes=["r1w%d_0" % i])
            return b, "r1w%d_0" % i

        def proj(wb, wkey, pt, pkey, p0, n):
            for k in range(8):
                S.op("pe", lambda e, k=k: e.matmul(pt[:, 0:n + 2], wb[:, k, :], hT[:, k, p0 - 1:p0 + n + 1], start=(k == 0), stop=(k == 7)),
                     reads=[wkey, "hT"], writes=[pkey])

        def shift(pt, pkey, dst, dkey, ct, n):
            S.op("act", lambda e: e.activation(dst, pt[:, 1:n + 1], AF.Identity, scale=c0_all[:, ct:ct + 1]),
                 reads=[pkey, "c0_all"], writes=[dkey])
            S.op("dve", lambda e: e.scalar_tensor_tensor(dst, pt[:, 0:n], mu_all[:, ct, 0:1], dst, ALU.mult, ALU.add),
                 reads=[pkey, "mu_all", dkey], writes=[dkey])
            S.op("dve", lambda e: e.scalar_tensor_tensor(dst, pt[:, 2:n + 2], mu_all[:, ct, 1:2], dst, ALU.mult, ALU.add),
                 reads=[pkey, "mu_all", dkey], writes=[dkey])

        u32 = [sb("u32_%d" % i, [128, 256], st=st) for i in range(2)]
        for ct, dst, fn in (() if SKIP_RW else ((24, th, AF.Tanh), (25, xab, AF.Identity), (26, sg, AF.Sigmoid))):
            wb, wkey = load_w(ct)
            for gi, (p0, n, t0) in enumerate(TG):
                pt, pkey = PS[gi % 2], "PS%d" % (gi % 2)
                proj(wb, wkey, pt, pkey, p0, n)
                u = u32[gi % 2]; ukey = "u32_%d" % (gi % 2)
                shift(pt, pkey, u[:], ukey, ct, n)
                S.op("act", lambda e, dst=dst, fn=fn, u=u, t0=t0, n=n: e.activation(dst[:, t0:t0 + n], u[:], fn),
                     reads=[ukey], writes=["lorares"])

        def t32(name, shape=(128, 256), dt=F32):
            return [sb("%s_%d" % (name, i), list(shape), dt, st=st) for i in range(2)]
        ru_, ku_, vu_, kq_, sq_, rs_, kk_ = [t32(nm) for nm in ("ru", "ku", "vu", "kq", "sq", "rs", "kk")]
        lw_, a_, tm_, ks_ = [t32(nm) for nm in ("lw", "aa", "tm", "ks")]
        A2t_ = t32("A2t", (128, 2, 256), BF16); D2t_ = [t32("D2t%d" % d, (128, 2, 256), BF16) for d in range(2)]
        lwT_ = [t32("lwT%d" % d, (128, 2, 128)) for d in range(2)]
        vb_ = t32("vb", (128, 256), BF16); vT_ = t32("vT", (128, 2, 128), BF16)
        gb_ = t32("gb", (128, 256), BF16); bv_ = t32("bv", (128, 256), BF16)
        def r1_body(hp, lane):
            ws = []
            for i, ct_ in enumerate((hp, 8 + hp, 16 + hp)):
                S.dma("pool", wl_[lane][i][:], win_d[:, ct_ * 128:(ct_ + 1) * 128].rearrange("(k p) c -> p k c", p=128), writes=["r1w%d_%d" % (i, lane)])
                ws.append((wl_[lane][i], "r1w%d_%d" % (i, lane)))
            (wr, wrk), (wk, wkk), (wv, wvk) = ws
            yield
            PSl = [PS[3 * lane + j] for j in range(3)]
            PSk = ["PS%d" % (3 * lane + j) for j in range(3)]
            PSBl, PSBk = PSB[lane], "PSB%d" % lane
            for gi, (p0, n, t0) in enumerate(TG):
                b = lane
                sfx = "_%d" % b
                ru, ku, vu, kq, sq, rs, kk = (x[b] for x in (ru_, ku_, vu_, kq_, sq_, rs_, kk_))
                lw, aa, tm, ks = (x[b] for x in (lw_, a_, tm_, ks_))
                A2t = A2t_[b]; vb = vb_[b]; vT = vT_[b]; gb = gb_[b]; bv = bv_[b]
                for (wb, wkey, pi, dst, dk, ct) in ((wr, wrk, 0, ru, "ru", hp), (wk, wkk, 1, ku, "ku", 8 + hp), (wv, wvk, 2, vu, "vu", 16 + hp)):
                    proj(wb, wkey, PSl[pi], PSk[pi], p0, n)
                    shift(PSl[pi], PSk[pi], dst[:], dk + sfx, ct, n)
                    yield
                S.op("dve", lambda e: e.tensor_scalar(kq[:], ku[:], kkw[:, hp:hp + 1], None, ALU.mult), reads=["ku" + sfx, "kkw"], writes=["kq" + sfx])
                S.op("act", lambda e: e.activation(sq[:], kq[:], AF.Square), reads=["kq" + sfx], writes=["sq" + sfx])
                S.op("pe", lambda e: e.matmul(PSl[0][:, 0:n], blk32[:], sq[:], start=True, stop=True), reads=["blk32", "sq" + sfx], writes=[PSk[0]])
                S.op("dve", lambda e: e.tensor_scalar(rs[:], PSl[0][:, 0:n], 1e-12, None, ALU.max), reads=[PSk[0]], writes=["rs" + sfx])
                S.op("act", lambda e: e.activation(rs[:], rs[:], AF.Sqrt), reads=["rs" + sfx], writes=["rs" + sfx])
                S.op("dve", lambda e: e.reciprocal(rs[:], rs[:]), reads=["rs" + sfx], writes=["rs" + sfx])
                S.op("dve", lambda e: e.tensor_tensor(kk[:], kq[:], rs[:], ALU.mult), reads=["kq" + sfx, "rs" + sfx], writes=["kk" + sfx])
                S.op("dve", lambda e: e.tensor_scalar(A2t[:, 0, :], kk[:], -1.0, None, ALU.mult), reads=["kk" + sfx], writes=["A2t" + sfx])
                S.op("act", lambda e: e.copy(A2t[:, 1, :], ru[:]), reads=["ru" + sfx], writes=["A2t" + sfx])
                S.dma("pool", A2_s[hp * 128:(hp + 1) * 128, :, t0:t0 + n], A2t[:], reads=["A2t" + sfx])
                yield
                for d in range(2):
                    D2t = D2t_[d][b]; lwT = lwT_[d][b]
                    dk = "d%d%s" % (d, sfx)
                    S.op("pe", lambda e, d=d: e.matmul(PSl[1][:, 0:n], w2b[d * 64:(d + 1) * 64, hp * 128:(hp + 1) * 128], th[d * 64:(d + 1) * 64, t0:t0 + n], start=True, stop=True),
                         reads=["lorab", "lorares"], writes=[PSk[1]])
                    S.op("act", lambda e, d=d: e.activation(lw[:], PSl[1][:, 0:n], AF.Sigmoid, bias=w0c[d][:, hp:hp + 1]), reads=[PSk[1], "w0c%d" % d], writes=["lw" + sfx])
                    S.op("dve", lambda e: e.tensor_scalar(lw[:], lw[:], NEG_E, None, ALU.mult), reads=["lw" + sfx], writes=["lw" + sfx])
                    for j in range(2):
                        S.op("pe", lambda e, j=j: e.transpose(PSl[2][:, j * 128:(j + 1) * 128], lw[:, j * 128:(j + 1) * 128], ident32[:]),
                             reads=["lw" + sfx, "ident32"], writes=[PSk[2]])
                    S.op("act", lambda e, lwT=lwT: e.copy(lwT[:], PSl[2][:, 0:256].rearrange("p (j c) -> p j c", j=2)), reads=[PSk[2]], writes=["lwT" + dk])
                    S.dma("pool", LWT_s[d, t0:t0 + n, hp * 128:(hp + 1) * 128].rearrange("(j p) c -> p j c", p=128), lwT[:], reads=["lwT" + dk])
                    S.op("pe", lambda e, d=d: e.matmul(PSl[1][:, 0:n], a2b[d * 64:(d + 1) * 64, hp * 128:(hp + 1) * 128], xab[d * 64:(d + 1) * 64, t0:t0 + n], start=True, stop=True),
                         reads=["lorab", "lorares"], writes=[PSk[1]])
                    S.op("act", lambda e, d=d: e.activation(aa[:], PSl[1][:, 0:n], AF.Sigmoid, bias=a0c[d][:, hp:hp + 1]), reads=[PSk[1], "a0c%d" % d], writes=["aa" + sfx])
                    S.op("dve", lambda e, D2t=D2t: e.tensor_tensor(D2t[:, 0, :], kk[:], aa[:], ALU.mult), reads=["kk" + sfx, "aa" + sfx], writes=["D2t" + dk])
                    S.op("dve", lambda e: e.tensor_scalar(tm[:], aa[:], kaw[:, hp:hp + 1], omka[:, hp:hp + 1], ALU.mult, ALU.add), reads=["aa" + sfx, "kaw", "omka"], writes=["tm" + sfx])
                    S.op("dve", lambda e: e.tensor_tensor(tm[:], tm[:], ku[:], ALU.mult), reads=["tm" + sfx, "ku" + sfx], writes=["tm" + sfx])
                    S.op("act", lambda e, D2t=D2t: e.copy(D2t[:, 1, :], tm[:]), reads=["tm" + sfx], writes=["D2t" + dk])
                    if d == 0:
                        S.op("act", lambda e: e.copy(ks[:], tm[:]), reads=["tm" + sfx], writes=["ks" + sfx])
                    else:
                        S.op("dve", lambda e: e.tensor_tensor(ks[:], ks[:], tm[:], ALU.add), reads=["tm" + sfx, "ks" + sfx], writes=["ks" + sfx])
                    S.dma("pool", D2_s[d, hp * 128:(hp + 1) * 128, :, t0:t0 + n], D2t[:], reads=["D2t" + dk])
                    yield
                S.op("act", lambda e: e.copy(vb[:], vu[:]), reads=["vu" + sfx], writes=["vb" + sfx])
                for j in range(2):
                    S.op("pe", lambda e, j=j: e.transpose(PSBl[:, j * 128:(j + 1) * 128], vb[:, j * 128:(j + 1) * 128], identb[:]),
                         reads=["vb" + sfx, "identb"], writes=[PSBk])
                S.op("act", lambda e: e.copy(vT[:], PSBl[:, 0:256].rearrange("p (j c) -> p j c", j=2)), reads=[PSBk], writes=["vT" + sfx])
                S.dma("pool", VT_s[t0:t0 + n, hp * 128:(hp + 1) * 128].rearrange("(j p) c -> p j c", p=128), vT[:], reads=["vT" + sfx])
                yield
                if t0 >= CTX:
                    l0 = t0 - CTX
                    S.op("pe", lambda e: e.matmul(PSl[1][:, 0:n], g2b[:, hp * 128:(hp + 1) * 128], sg[:, t0:t0 + n], start=True, stop=True),
                         reads=["lorab", "lorares"], writes=[PSk[1]])
                    S.op("act", lambda e: e.copy(gb[:], PSl[1][:, 0:n]), reads=[PSk[1]], writes=["gb" + sfx])
                    S.dma("pool", G_s[hp * 128:(hp + 1) * 128, l0:l0 + n], gb[:], reads=["gb" + sfx])
                    S.op("dve", lambda e: e.scalar_tensor_tensor(ks[:], ks[:], rkw[:, hp:hp + 1], ru[:], ALU.mult, ALU.mult), reads=["ks" + sfx, "rkw", "ru" + sfx], writes=["ks" + sfx])
                    S.op("pe", lambda e: e.matmul(PSl[0][:, 0:n], blk32[:], ks[:], start=True, stop=True), reads=["blk32", "ks" + sfx], writes=[PSk[0]])
                    S.op("dve", lambda e: e.tensor_tensor(bv[:], PSl[0][:, 0:n], vu[:], ALU.mult), reads=[PSk[0], "vu" + sfx], writes=["bv" + sfx])
                    S.dma("pool", BV_s[hp * 128:(hp + 1) * 128, l0:l0 + n], bv[:], reads=["bv" + sfx])

        if not SKIP_RW:
            run_lanes(range(8), r1_body, 2)
        S.barrier()

    if dbg == "R1":
        o = dout("dbg", [4, 128, 4, NTOK], BF16)
        S.dma("sp", o[0, :, 0:2, :], A2_s[0:128], reads=[])
        S.dma("sp", o[1, :, 0:2, :], D2_s[0, 0:128], reads=[])
        S.dma("sp", o[2, :, 0:2, :], D2_s[1, 0:128], reads=[])
        S.dma("sp", o[3, :, 0, 0:SEQ], G_s[0:128], reads=[])
        S.dma("sp", o[3, :, 1, 0:SEQ], BV_s[0:128], reads=[])
        o2 = dout("dbg2", [2, NTOK, 128])
        S.dma("sp", o2[0], LWT_s[0, :, 0:128], reads=[])
        S.dma("sp", o2[1], LWT_s[1, :, 0:128], reads=[])
        o3 = dout("dbg3", [NTOK, 128], BF16)
        S.dma("sp", o3, VT_s[:, 0:128], reads=[])
        S.finish()
        return

    convw_d = din("ssm_conv_w", [3072, 5]); convb_d = din("ssm_conv_b", [3072])
    dtb_d = din("ssm_dt_bias", [64]); alog_d = din("ssm_a_log", [64])
    XT_s = nc.dram_tensor("ss_xt", [NTOK, 2048], BF16).ap()
    BT_s = nc.dram_tensor("ss_bt", [NTOK, 512], BF16).ap()
    BF_s = nc.dram_tensor("ss_bf", [4, 128, NTOK], BF16).ap()
    CF_s = nc.dram_tensor("ss_cf", [4, 128, NTOK], BF16).ap()
    ZS_s = nc.dram_tensor("ss_zs", [SEQ, 2048], BF16).ap()
    DT_s = nc.dram_tensor("ss_dt", [NTOK, 64], F32).ap()
    DA_s = nc.dram_tensor("ss_da", [NTOK, 64], F32).ap()
    GA_s = nc.dram_tensor("mg_ga", [SEQ, 1024], BF16).ap()
    GB_s = nc.dram_tensor("mg_gb", [SEQ, 1024], BF16).ap()
    hT_cm = hT[:, :, L0:L0 + SEQ].rearrange("p k (r c) -> p k c r", c=64)
    XBC0 = 3456 + 2048
    DT0 = XBC0 + 3072
    GATE0 = 3456 + 5184
    with contextlib.ExitStack() as st:
        cw = sb("cw", [128, 24, 5], st=st); cb = sb("cb", [128, 24], st=st)
        S.dma("sp", cw[:], convw_d.rearrange("(t p) j -> p t j", p=128), writes=["cw"], allow_slow_non_contiguous=True)
        S.dma("sp", cb[:], convb_d.rearrange("(t p) -> p t", p=128), writes=["cb"], allow_slow_non_contiguous=True)
        NL = 4
        w32 = [sb("s1w32_%d" % i, [128, 8, 128], st=st) for i in range(NL)]
        wbt = [sb("s1wbt_%d" % i, [128, 8, 128], BF16, st=st) for i in range(NL)]
        raw_ = [sb("s1raw_%d" % i, [128, 260], st=st) for i in range(2 * NL)]
        acc_ = [sb("s1acc_%d" % i, [128, 256], st=st) for i in range(2 * NL)]
        ob_ = [sb("s1ob_%d" % i, [128, 256], BF16, st=st) for i in range(2 * NL)]
        oT_ = [sb("s1oT_%d" % i, [128, 2, 128], BF16, st=st) for i in range(2 * NL)]

        def conv_body(ct, lane):
            a, wb = w32[lane], wbt[lane]
            wkey = "s1wbt_%d" % lane
            S.dma("sp", a[:], win_d[:, XBC0 + ct * 128:XBC0 + (ct + 1) * 128].rearrange("(k p) c -> p k c", p=128), writes=["s1w32_%d" % lane])
            yield
            S.op("pool", lambda e: e.tensor_copy(wb[:], a[:]), reads=["s1w32_%d" % lane], writes=[wkey])
            yield
            for gi in range(17):
                b = lane * 2 + gi % 2
                sfx = "_%d" % b
                raw, acc, ob, oT = raw_[b], acc_[b], ob_[b], oT_[b]
                pt, pk = PS[lane], "PS%d" % lane
                pbt, pbk = PSB[lane % 2], "PSB%d" % (lane % 2)
                s0 = 0 if gi == 0 else CTX + 256 * (gi - 1)
                if gi == 0:
                    for k in range(8):
                        S.op("pe", lambda e, k=k: e.matmul(pt[:, 2:258], wb[:, k, :], hT[:, k, C0:C0 + 256], start=(k == 0), stop=(k == 7)), reads=[wkey, "hT"], writes=[pk])
                else:
                    g = gi - 1
                    for k in range(8):
                        S.op("pe", lambda e, k=k, g=g: e.matmul(pt[:, 2:258].rearrange("p (c r) -> p c r", c=4), wb[:, k, :], hT_cm[:, k, 4 * g:4 * g + 4, :], start=(k == 0), stop=(k == 7)), reads=[wkey, "hT"], writes=[pk])
                    if g > 0:
                        for k in range(8):
                            S.op("pe", lambda e, k=k, g=g: e.matmul(pt[:, 0:2], wb[:, k, :], hT_cm[:, k, 4 * g - 1, 62:64], start=(k == 0), stop=(k == 7)), reads=[wkey, "hT"], writes=[pk])
                    if g < 15:
                        for k in range(8):
                            S.op("pe", lambda e, k=k, g=g: e.matmul(pt[:, 258:260], wb[:, k, :], hT_cm[:, k, 4 * g + 4, 0:2], start=(k == 0), stop=(k == 7)), reads=[wkey, "hT"], writes=[pk])
                yield
                S.op("act", lambda e: e.copy(raw[:], pt[:, 0:260]), reads=[pk], writes=["s1raw" + sfx])
                if gi <= 1:
                    S.op("pool", lambda e: e.memset(raw[:, 0:2], 0.0), writes=["s1raw" + sfx])
                if gi == 0 or gi == 16:
                    S.op("pool", lambda e: e.memset(raw[:, 258:260], 0.0), writes=["s1raw" + sfx])
                yield
                S.op("dve", lambda e: e.tensor_scalar(acc[:], raw[:, 0:256], cw[:, ct, 0:1], cb[:, ct:ct + 1], ALU.mult, ALU.add), reads=["s1raw" + sfx, "cw", "cb"], writes=["s1acc" + sfx])
                for j in range(1, 5):
                    S.op("dve", lambda e, j=j: e.scalar_tensor_tensor(acc[:], raw[:, j:j + 256], cw[:, ct, j:j + 1], acc[:], ALU.mult, ALU.add), reads=["s1raw" + sfx, "cw", "s1acc" + sfx], writes=["s1acc" + sfx])
                yield
                S.op("act", lambda e: e.activation(ob[:], acc[:], AF.Silu), reads=["s1acc" + sfx], writes=["s1ob" + sfx])
                yield
                if ct >= 16:
                    dst = (BF_s if ct < 20 else CF_s)[(ct - 16) % 4, :, s0:s0 + 256]
                    S.dma("pool", dst, ob[:], reads=["s1ob" + sfx])
                if ct < 20:
                    for j in range(2):
                        S.op("pe", lambda e, j=j: e.transpose(pbt[:, j * 128:(j + 1) * 128], ob[:, j * 128:(j + 1) * 128], identb[:]), reads=["s1ob" + sfx, "identb"], writes=[pbk])
                    S.op("act", lambda e: e.copy(oT[:], pbt[:, 0:256].rearrange("p (j c) -> p j c", j=2)), reads=[pbk], writes=["s1oT" + sfx])
                    yield
                    if ct < 16:
                        dst = XT_s[s0:s0 + 256, ct * 128:(ct + 1) * 128]
                    else:
                        dst = BT_s[s0:s0 + 256, (ct - 16) * 128:(ct - 15) * 128]
                    S.dma("pool", dst.rearrange("(j p) c -> p j c", p=128), oT[:], reads=["s1oT" + sfx])
                yield

        run_lanes(range(24), conv_body, NL)
        S.barrier()

    def tiles_for(order, with_ctx):
        out = []
        if with_ctx:
            for j in range(2):
                out.append(((lambda j: (lambda k: hT[:, k, C0 + j * 128:C0 + (j + 1) * 128]))(j), 128, j * 128))
        base = CTX if with_ctx else 0
        if order == "ssm":
            for c in range(64):
                out.append(((lambda c: (lambda k: hT_cm[:, k, c, :]))(c), 64, base + c * 64))
        else:
            for tt in range(32):
                out.append(((lambda tt: (lambda k: hT[:, k, L0 + tt * 128:L0 + (tt + 1) * 128]))(tt), 128, base + tt * 128))
        return out

    for name, col0, ncol, fn, dst_s, order in (("z", 3456, 2048, AF.Silu, ZS_s, "ssm"), ("gb", GATE0 + 1024, 1024, AF.Sigmoid, GB_s, "ssm"), ("ga", GATE0, 1024, AF.Sigmoid, GA_s, "nat")):
        with contextlib.ExitStack() as st:
            wz = sb("wz" + name, [128, 8, ncol], BF16, st=st)
            stg = [sb("wzs%s%d" % (name, i), [128, 8, 512], st=st) for i in range(2)]
            for cbk in range(ncol // 512):
                S.dma("sp", stg[cbk % 2][:], win_d[:, col0 + cbk * 512:col0 + (cbk + 1) * 512].rearrange("(k p) c -> p k c", p=128), writes=["wzs%d" % (cbk % 2)])
                S.op("pool", lambda e, cbk=cbk: e.tensor_copy(wz[:, :, cbk * 512:(cbk + 1) * 512], stg[cbk % 2][:]), reads=["wzs%d" % (cbk % 2)], writes=["wz"])
            zt_ = [sb("zt%s%d" % (name, i), [128, ncol], BF16, st=st) for i in range(2)]
            for ti, (lf, nr, r0) in enumerate(tiles_for(order, False)):
                zt = zt_[ti % 2]
                for cbk in range(ncol // 512):
                    pt, pk = nextps_()
                    for k in range(8):
                        S.op("pe", lambda e, k=k, cbk=cbk, pt=pt: e.matmul(pt[0:nr, :], lf(k), wz[:, k, cbk * 512:(cbk + 1) * 512], start=(k == 0), stop=(k == 7)), reads=["wz", "hT"], writes=[pk])
                    S.op("act", lambda e, cbk=cbk, pt=pt: e.activation(zt[0:nr, cbk * 512:(cbk + 1) * 512], pt[0:nr, :], fn), reads=[pk], writes=["zt%d" % (ti % 2)])
                S.dma("pool", dst_s[r0:r0 + nr, :], zt[0:nr, :], reads=["zt%d" % (ti % 2)])
            S.barrier()
    with contextlib.ExitStack() as st:
        wd32 = sb("wd32", [128, 8, 64], st=st); wdb = sb("wdb", [128, 8, 64], BF16, st=st)
        S.dma("sp", wd32[:], win_d[:, DT0:DT0 + 64].rearrange("(k p) c -> p k c", p=128), writes=["wd32"])
        S.op("dve", lambda e: e.tensor_copy(wdb[:], wd32[:]), reads=["wd32"], writes=["wdb"])
        dtb = sb("dtb", [128, 64], st=st); aneg = sb("aneg", [128, 64], st=st)
        S.dma("sp", dtb[:], dtb_d.partition_broadcast(128), writes=["dtb"])
        S.dma("sp", aneg[:], alog_d.partition_broadcast(128), writes=["aneg"])
        S.op("act", lambda e: e.activation(aneg[:], aneg[:], AF.Exp), reads=["aneg"], writes=["aneg"])
        S.op("dve", lambda e: e.tensor_scalar(aneg[:], aneg[:], -1.0, None, ALU.mult), reads=["aneg"], writes=["aneg"])
        dx_ = [sb("dx%d" % i, [128, 64], st=st) for i in range(2)]
        da_ = [sb("dax%d" % i, [128, 64], st=st) for i in range(2)]
        for ti, (lf, nr, r0) in enumerate(tiles_for("ssm", True)):
            b = ti % 2
            dx, da = dx_[b], da_[b]
            pt, pk = nextps_()
            for k in range(8):
                S.op("pe", lambda e, k=k: e.matmul(pt[0:nr, 0:64], lf(k), wdb[:, k, :], start=(k == 0), stop=(k == 7)), reads=["wdb", "hT"], writes=[pk])
            S.op("dve", lambda e: e.scalar_tensor_tensor(dx[0:nr, :], pt[0:nr, 0:64], 30.0, dtb[0:nr, :], ALU.min, ALU.add), reads=[pk, "dtb"], writes=["dx%d" % b])
            S.op("act", lambda e: e.activation(dx[0:nr, :], dx[0:nr, :], AF.Exp), reads=["dx%d" % b], writes=["dx%d" % b])
            S.op("act", lambda e: e.activation(dx[0:nr, :], dx[0:nr, :], AF.Ln, bias=onec[0:nr, :]), reads=["dx%d" % b, "onec"], writes=["dx%d" % b])
            S.op("dve", lambda e: e.tensor_tensor(da[0:nr, :], dx[0:nr, :], aneg[0:nr, :], ALU.mult), reads=["dx%d" % b, "aneg"], writes=["dax%d" % b])
            S.dma("pool", DT_s[r0:r0 + nr, :], dx[0:nr, :], reads=["dx%d" % b])
            S.dma("pool", DA_s[r0:r0 + nr, :], da[0:nr, :], reads=["dax%d" % b])
        S.barrier()
    if dbg == "S1":
        o = dout("dbgX", [512, 2048], BF16); S.dma("sp", o[0:256], XT_s[0:256], reads=[]); S.dma("sp", o[256:512], XT_s[NTOK - 256:NTOK], reads=[])
        o = dout("dbgB", [NTOK, 512], BF16); S.dma("sp", o, BT_s, reads=[])
        o = dout("dbgBF", [128, NTOK], BF16); S.dma("sp", o, BF_s[1], reads=[])
        o = dout("dbgC", [128, NTOK], BF16); S.dma("sp", o, CF_s[2], reads=[])
        o = dout("dbgZ", [256, 2048], BF16); S.dma("sp", o, ZS_s[1024:1280], reads=[])
        o = dout("dbgGA", [256, 1024], BF16); S.dma("sp", o, GA_s[1024:1280], reads=[])
        o = dout("dbgGB", [256, 1024], BF16); S.dma("sp", o, GB_s[1024:1280], reads=[])
        o = dout("dbgDT", [NTOK, 64]); S.dma("sp", o, DT_s, reads=[])
        o = dout("dbgDA", [NTOK, 64]); S.dma("sp", o, DA_s, reads=[])
        S.finish()
        return

    S.barrier()
    stH.close()

    YS = nc.dram_tensor("rw_ys", [2, 16, 64, SEQ], F32).ap()
    nextps = nextps_

    r2cnt = [0]

    def r2_round(streams, nchunks=34):
        r2cnt[0] += 1
        rid = r2cnt[0]
        with contextlib.ExitStack() as st:
            B = []
            for si, (d, hg) in enumerate(streams):
                def T_(nm, shape, dt=BF16):
                    return sb("r2%s_%d_%d" % (nm, si, rid), list(shape), dt, st=st)
                bb = dict(
                    lwT=T_("lwT", [128, 256], F32), A2c=T_("A2c", [64, 4, 2, 128]), D2c=T_("D2c", [64, 4, 2, 128]), Vc=T_("Vc", [128, 4, 64]),
                    E1=T_("E1", [64, 4, 128], F32), E2=T_("E2", [64, 4, 128], F32), E3=T_("E3", [64, 4, 128], F32),
                    alt=T_("alt", [64, 4, 128]), rt=T_("rt", [64, 4, 128]), bet=T_("bet", [64, 4, 128]), kat=T_("kat", [64, 4, 128]),
                    W0=T_("W0", [128, 4, 128], F32), W1=T_("W1", [128, 4, 128], F32), N0=T_("N0", [128, 4, 128], F32), N1=T_("N1", [128, 4, 128], F32),
                    XA=T_("XA", [128, 4, 64]),
                    Wak=T_("Wak", [128, 4, 128]), Mrb=T_("Mrb", [128, 4, 128]), Mrk=T_("Mrk", [128, 4, 128]),
                    TT=T_("TT", [128, 2, 4, 64]), X=T_("X", [128, 4, 128], F32), AtF=T_("AtF", [64, 4, 128]), U=T_("U", [128, 4, 64]),
                    Z32=T_("Z32", [64, 4, 64], F32), Zb=T_("Zb", [64, 4, 64]), tz=T_("tz", [64, 4, 64], F32), Yt=T_("Yt", [64, 4, 128], F32),
                )
                S.op("pool", lambda e, bb=bb: e.memset(bb["Z32"][:], 0.0), writes=["Z32_%d" % si])
                S.op("pool", lambda e, bb=bb: e.memset(bb["Zb"][:], 0.0), writes=["Zb_%d" % si])
                B.append(bb)

            def k_(nm, si):
                return "%s_%d" % (nm, si)

            for step in range(nchunks):
                info = []
                for si, (d, hg) in enumerate(streams):
                    if d == 0:
                        c = step
                    else:
                        c = (1 - step) if step < 2 else (35 - step)
                    info.append((si, d, hg, c, c * 128, B[si]))
                for si, d, hg, c, t0, bb in info:
                    ch0 = hg * 256
                    S.dma("sp", bb["lwT"][:], LWT_s[d, t0:t0 + 128, ch0:ch0 + 256], writes=[k_("lwT", si)])
                    for a_ in range(2):
                        S.dma("sp", bb["A2c"][:, :, a_, :], A2_s[ch0:ch0 + 256, a_, t0:t0 + 128].rearrange("(h k) t -> k h t", k=64), writes=[k_("A2c", si)])
                        S.dma("sp", bb["D2c"][:, :, a_, :], D2_s[d, ch0:ch0 + 256, a_, t0:t0 + 128].rearrange("(h k) t -> k h t", k=64), writes=[k_("D2c", si)])
                    S.dma("sp", bb["Vc"][:], VT_s[t0:t0 + 128, ch0:ch0 + 256].rearrange("t (h v) -> t h v", h=4), writes=[k_("Vc", si)])
                for si, d, hg, c, t0, bb in info:
                    mi, me = (1, 0) if d == 0 else (3, 2)
                    pi_, pik = nextps(); pe_, pek = nextps()
                    for h in range(4):
                        S.op("pe", lambda e, h=h, bb=bb, pi_=pi_, mi=mi: e.matmul(pi_[0:64, h * 128:(h + 1) * 128], bb["lwT"][:, h * 64:(h + 1) * 64], msk[:, mi, :], start=True, stop=True),
                             reads=[k_("lwT", si), "msk"], writes=[pik])
                    for h in range(4):
                        S.op("pe", lambda e, h=h, bb=bb, pe_=pe_, me=me: e.matmul(pe_[0:64, h * 128:(h + 1) * 128], bb["lwT"][:, h * 64:(h + 1) * 64], msk[:, me, :], start=True, stop=True),
                             reads=[k_("lwT", si), "msk"], writes=[pek])
                    v3 = lambda t: t[0:64, :].rearrange("p (h t) -> p h t", h=4)
                    S.op("act", lambda e, bb=bb, pe_=pe_: e.activation(bb["E1"][:], v3(pe_), AF.Exp), reads=[pek], writes=[k_("E1", si)])
                    S.op("act", lambda e, bb=bb, pi_=pi_: e.activation(bb["E2"][:], v3(pi_), AF.Exp), reads=[pik], writes=[k_("E2", si)])
                    S.op("act", lambda e, bb=bb, pi_=pi_: e.activation(bb["E3"][:], v3(pi_), AF.Exp, scale=-1.0), reads=[pik], writes=[k_("E3", si)])
                for si, d, hg, c, t0, bb in info:
                    for dst, src, a_, E in (("alt", "A2c", 0, "E1"), ("rt", "A2c", 1, "E2"), ("bet", "D2c", 0, "E3"), ("kat", "D2c", 1, "E3")):
                        S.op("dve", lambda e, bb=bb, dst=dst, src=src, a_=a_, E=E: e.tensor_tensor(bb[dst][:], bb[src][:, :, a_, :], bb[E][:], ALU.mult),
                             reads=[k_(src, si), k_(E, si)], writes=[k_(dst, si)])
                for si, d, hg, c, t0, bb in info:
                    Wm, Nm, Mm = (0, 2, 1) if d == 0 else (2, 0, 3)
                    for dst, L, R_, mm in (("W0", "bet", "alt", Wm), ("N0", "alt", "bet", Nm), ("Wak", "kat", "alt", Wm), ("Mrb", "bet", "rt", Mm), ("Mrk", "kat", "rt", Mm)):
                        pa, pk = nextps()
                        for h in range(4):
                            S.op("pe", lambda e, h=h, bb=bb, pa=pa, L=L, R_=R_: e.matmul(pa[:, h * 128:(h + 1) * 128], bb[L][:, h, :], bb[R_][:, h, :], start=True, stop=True),
                                 reads=[k_(L, si), k_(R_, si)], writes=[pk])
                        S.op("dve", lambda e, bb=bb, pa=pa, dst=dst, mm=mm: e.tensor_tensor(bb[dst][:], pa[:].rearrange("p (h t) -> p h t", h=4), msk[:, mm, :].unsqueeze(1).to_broadcast([128, 4, 128]), ALU.mult),
                             reads=[pk, "msk"], writes=[k_(dst, si)])
                    for j, src in enumerate(("alt", "bet", "kat")):
                        for h in range(4):
                            S.op("pe", lambda e, h=h, j=j, bb=bb, src=src: e.transpose(PSB[0][:, (j * 4 + h) * 64:(j * 4 + h + 1) * 64], bb[src][:, h, :], identb[0:64, 0:64]),
                                 reads=[k_(src, si), "identb"], writes=["PSB0"])
                    S.op("act", lambda e, bb=bb: e.copy(bb["X"][:, :, 0:64], PSB[0][:, 0:256].rearrange("p (h k) -> p h k", h=4)), reads=["PSB0"], writes=[k_("X", si)])
                    S.op("act", lambda e, bb=bb: e.copy(bb["TT"][:], PSB[0][:, 256:768].rearrange("p (j h k) -> p j h k", j=2, h=4)), reads=["PSB0"], writes=[k_("TT", si)])
                    pa, pk = nextps()
                    for h in range(4):
                        S.op("pe", lambda e, h=h, bb=bb, pa=pa: e.matmul(pa[:, h * 64:(h + 1) * 64], bb["Wak"][:, h, :], bb["Vc"][:, h, :], start=True, stop=True),
                             reads=[k_("Wak", si), k_("Vc", si)], writes=[pk])
                    S.op("act", lambda e, bb=bb, pa=pa: e.copy(bb["X"][:, :, 64:128], pa[:, 0:256].rearrange("p (h v) -> p h v", h=4)), reads=[pk], writes=[k_("X", si)])
                for lvl in range(7):
                    for si, d, hg, c, t0, bb in info:
                        Wc, Nc = ("W0", "N0") if lvl % 2 == 0 else ("W1", "N1")
                        Wn, Nn = ("W1", "N1") if lvl % 2 == 0 else ("W0", "N0")
                        px, pxk = nextps()
                        for h in range(4):
                            S.op("pe", lambda e, h=h, bb=bb, px=px, Wc=Wc: e.matmul(px[:, h * 128:(h + 1) * 128], bb[Wc][:, h, :], bb["X"][:, h, :], start=True, stop=True),
                                 reads=[k_(Wc, si), k_("X", si)], writes=[pxk])
                        if lvl < 6:
                            pw, pwk = nextps(); pn, pnk = nextps()
                            for h in range(4):
                                S.op("pe", lambda e, h=h, bb=bb, pw=pw, Wc=Wc, Nc=Nc: e.matmul(pw[:, h * 128:(h + 1) * 128], bb[Nc][:, h, :], bb[Wc][:, h, :], start=True, stop=True),
                                     reads=[k_(Wc, si), k_(Nc, si)], writes=[pwk])
                            for h in range(4):
                                S.op("pe", lambda e, h=h, bb=bb, pn=pn, Wc=Wc, Nc=Nc: e.matmul(pn[:, h * 128:(h + 1) * 128], bb[Wc][:, h, :], bb[Nc][:, h, :], start=True, stop=True),
                                     reads=[k_(Wc, si), k_(Nc, si)], writes=[pnk])
                        S.op("dve", lambda e, bb=bb, px=px: e.tensor_tensor(bb["X"][:], px[:].rearrange("p (h t) -> p h t", h=4), bb["X"][:], ALU.add),
                             reads=[pxk, k_("X", si)], writes=[k_("X", si)])
                        if lvl < 6:
                            S.op("act", lambda e, bb=bb, pw=pw, Wn=Wn: e.copy(bb[Wn][:], pw[:].rearrange("p (h t) -> p h t", h=4)), reads=[pwk], writes=[k_(Wn, si)])
                            S.op("dve", lambda e, bb=bb, pn=pn, Nn=Nn: e.tensor_copy(bb[Nn][:], pn[:].rearrange("p (h t) -> p h t", h=4)), reads=[pnk], writes=[k_(Nn, si)])
                for si, d, hg, c, t0, bb in info:
                    S.op("act", lambda e, bb=bb: e.copy(bb["XA"][:], bb["X"][:, :, 0:64]), reads=[k_("X", si)], writes=[k_("XA", si)])
                    for h in range(4):
                        S.op("pe", lambda e, h=h, bb=bb: e.transpose(PSB[1][0:64, h * 128:(h + 1) * 128], bb["XA"][:, h, :], identb[:]),
                             reads=[k_("XA", si), "identb"], writes=["PSB1"])
                    S.op("act", lambda e, bb=bb: e.copy(bb["AtF"][:], PSB[1][0:64, 0:512].rearrange("p (h t) -> p h t", h=4)), reads=["PSB1"], writes=[k_("AtF", si)])
                for si, d, hg, c, t0, bb in info:
                    pa, pk = nextps()
                    for h in range(4):
                        S.op("pe", lambda e, h=h, bb=bb, pa=pa: e.matmul(pa[:, h * 64:(h + 1) * 64], bb["AtF"][:, h, :], bb["Zb"][:, h, :], start=True, stop=True),
                             reads=[k_("AtF", si), k_("Zb", si)], writes=[pk])
                    S.op("dve", lambda e, bb=bb, pa=pa: e.tensor_tensor(bb["U"][:], pa[:, 0:256].rearrange("p (h v) -> p h v", h=4), bb["X"][:, :, 64:128], ALU.add),
                         reads=[pk, k_("X", si)], writes=[k_("U", si)])
                for si, d, hg, c, t0, bb in info:
                    if c >= 2:
                        py, pyk = nextps()
                        for h in range(4):
                            o_ = py[0:64, h * 128:(h + 1) * 128]
                            S.op("pe", lambda e, h=h, bb=bb, o_=o_: e.matmul(o_, bb["Zb"][:, h, :], bb["rt"][:, h, :], start=True, stop=False), reads=[k_("Zb", si), k_("rt", si)], writes=[pyk])
                            S.op("pe", lambda e, h=h, bb=bb, o_=o_: e.matmul(o_, bb["U"][:, h, :], bb["Mrb"][:, h, :], start=False, stop=False), reads=[k_("U", si), k_("Mrb", si)], writes=[pyk])
                            S.op("pe", lambda e, h=h, bb=bb, o_=o_: e.matmul(o_, bb["Vc"][:, h, :], bb["Mrk"][:, h, :], start=False, stop=True), reads=[k_("Vc", si), k_("Mrk", si)], writes=[pyk])
                        S.op("act", lambda e, bb=bb, py=py: e.copy(bb["Yt"][:], py[0:64, :].rearrange("p (h t) -> p h t", h=4)), reads=[pyk], writes=[k_("Yt", si)])
                        l0 = t0 - CTX
                        S.dma("pool", YS[d, hg * 4:hg * 4 + 4, :, l0:l0 + 128].rearrange("h v t -> v h t"), bb["Yt"][:], reads=[k_("Yt", si)])
                    pz, pzk = nextps()
                    for h in range(4):
                        o_ = pz[0:64, h * 64:(h + 1) * 64]
                        S.op("pe", lambda e, h=h, bb=bb, o_=o_: e.matmul(o_, bb["TT"][:, 0, h, :], bb["U"][:, h, :], start=True, stop=False), reads=[k_("TT", si), k_("U", si)], writes=[pzk])
                        S.op("pe", lambda e, h=h, bb=bb, o_=o_: e.matmul(o_, bb["TT"][:, 1, h, :], bb["Vc"][:, h, :], start=False, stop=True), reads=[k_("TT", si), k_("Vc", si)], writes=[pzk])
                    last = 127 if d == 0 else 0
                    S.op("dve", lambda e, bb=bb, pz=pz: e.tensor_tensor(bb["tz"][:], pz[0:64, 0:256].rearrange("p (h v) -> p h v", h=4), bb["Z32"][:], ALU.add),
                         reads=[pzk, k_("Z32", si)], writes=[k_("tz", si)])
                    S.op("dve", lambda e, bb=bb, last=last: e.tensor_tensor(bb["Z32"][:], bb["tz"][:], bb["E2"][:, :, last:last + 1].to_broadcast([64, 4, 64]), ALU.mult),
                         reads=[k_("tz", si), k_("E2", si)], writes=[k_("Z32", si)])
                    S.op("act", lambda e, bb=bb: e.copy(bb["Zb"][:], bb["Z32"][:]), reads=[k_("Z32", si)], writes=[k_("Zb", si)])
            S.barrier()
            if dbg == "R2a":
                bb = B[0]
                o = dout("dbgA", [64, 4, 4, 128], BF16)
                for j, nm in enumerate(("alt", "rt", "bet", "kat")):
                    S.dma("sp", o[:, j], bb[nm][:], reads=[])
                o = dout("dbgB", [128, 4, 128])
                S.dma("sp", o, bb["X"][:], reads=[])
                o = dout("dbgC", [128, 4, 64], BF16)
                S.dma("sp", o, bb["U"][:], reads=[])
                o = dout("dbgD", [64, 4, 64])
                S.dma("sp", o, bb["Z32"][:], reads=[])
                o = dout("dbgE", [64, 3, 4, 128])
                for j, nm in enumerate(("E1", "E2", "E3")):
                    S.dma("sp", o[:, j], bb[nm][:], reads=[])
                S.barrier()

    if dbg == "R2a":
        r2_round([(0, 0), (1, 0)], 1)
        S.finish()
        return
    R2N = 34 if dbg != "R2" else 6
    if dbg == "R2":
        r2_round([(0, 0), (1, 0)], R2N)
        o = dout("dbg", [2, 4, 64, 512])
        S.dma("sp", o[0], YS[0, 0:4, :, 0:512], reads=[])
        S.dma("sp", o[1], YS[1, 0:4, :, SEQ - 512:SEQ], reads=[])
        S.finish()
        return
    if dbg == "R3":
        r2_round([(0, 0), (1, 0)])
    elif not SKIP_RW:
        for rnd in range(2):
            r2_round([(0, 2 * rnd), (1, 2 * rnd), (0, 2 * rnd + 1), (1, 2 * rnd + 1)])

    YD_s = nc.dram_tensor("ss_yd", [2, SEQ, 2048], F32).ap()

    def s2_scan(nsteps=34):
        with contextlib.ExitStack() as st:
            ones128 = sb("ones128", [128, 128], st=st)
            S.op("dve", lambda e: e.memset(ones128[:], 1.0), writes=["ones128"])
            negm = sb("negm", [128, 2, 128], st=st)
            S.op("dve", lambda e: e.tensor_scalar(negm[:, 0, :], msk[:, 2, :], -1e30, None, ALU.mult), reads=["msk"], writes=["negm"])
            S.op("dve", lambda e: e.tensor_scalar(negm[:, 1, :], msk[:, 0, :], -1e30, None, ALU.mult), reads=["msk"], writes=["negm"])
            B = []
            for d in range(2):
                def T_(nm, shape, dt=BF16):
                    return sb("s2%s_%d" % (nm, d), list(shape), dt, st=st)
                bb = dict(XT=T_("XT", [128, 32, 64]), BT=T_("BT", [128, 512]), BF=T_("BF", [128, 4, 128]), CF=T_("CF", [128, 4, 128]),
                          DT=T_("DT", [128, 32], F32), DA=T_("DA", [128, 32], F32), cum=T_("cum", [128, 32], F32), ncum=T_("ncum", [128, 32], F32),
                          Ecum=T_("Ecum", [128, 32], F32), Gd=T_("Gd", [128, 32], F32), Etot=T_("Etot", [128, 32], F32),
                          xdt=T_("xdt", [128, 32, 64]), xs=T_("xs", [128, 32, 64]), rhsA=T_("rhsA", [128, 32, 128], F32),
                          CBT=T_("CBT", [128, 4, 128], F32), seg0=T_("seg0", [128, 8, 128], F32), seg1=T_("seg1", [128, 8, 128], F32),
                          MT0=T_("MT0", [128, 8, 128]), MT1=T_("MT1", [128, 8, 128]),
                          H32=T_("H32", [128, 4, 512], F32), Hb=T_("Hb", [128, 4, 512]), yt0=T_("yt0", [128, 512], F32), yt1=T_("yt1", [128, 512], F32),
                          ti0=T_("ti0", [128, 512], F32), ti1=T_("ti1", [128, 512], F32))
                S.op("pool", lambda e, bb=bb: e.memset(bb["H32"][:], 0.0), writes=["H32_%d" % d])
                S.op("pool", lambda e, bb=bb: e.memset(bb["Hb"][:], 0.0), writes=["Hb_%d" % d])
                B.append(bb)

            def k_(nm, d):
                return "s2%s_%d" % (nm, d)

            for step in range(nsteps):
                info = []
                for d in range(2):
                    c = step if d == 0 else ((1 - step) if step < 2 else (35 - step))
                    info.append((d, c, c * 128, B[d]))
                for d, c, t0, bb in info:
                    S.dma("sp", bb["XT"][:], XT_s[t0:t0 + 128, :].rearrange("t (h p) -> t h p", h=32), writes=[k_("XT", d)])
                    S.dma("sp", bb["BT"][:], BT_s[t0:t0 + 128, :], writes=[k_("BT", d)])
                    S.dma("sp", bb["BF"][:], BF_s[:, :, t0:t0 + 128].rearrange("g n t -> n g t"), writes=[k_("BF", d)])
                    if c >= 2:
                        S.dma("sp", bb["CF"][:], CF_s[:, :, t0:t0 + 128].rearrange("g n t -> n g t"), writes=[k_("CF", d)])
                    S.dma("sp", bb["DT"][:], DT_s[t0:t0 + 128, d * 32:(d + 1) * 32], writes=[k_("DT", d)])
                    S.dma("sp", bb["DA"][:], DA_s[t0:t0 + 128, d * 32:(d + 1) * 32], writes=[k_("DA", d)])
                for d, c, t0, bb in info:
                    mi = 1 if d == 0 else 3
                    pc, pck = nextps_()
                    S.op("pe", lambda e, bb=bb, pc=pc, mi=mi: e.matmul(pc[:, 0:32], msk[:, mi, :], bb["DA"][:], start=True, stop=True), reads=["msk", k_("DA", d)], writes=[pck])
                    S.op("pe", lambda e, bb=bb, pc=pc: e.matmul(pc[:, 32:64], ones128[:], bb["DA"][:], start=True, stop=True), reads=["ones128", k_("DA", d)], writes=[pck])
                    S.op("act", lambda e, bb=bb, pc=pc: e.copy(bb["cum"][:], pc[:, 0:32]), reads=[pck], writes=[k_("cum", d)])
                    S.op("dve", lambda e, bb=bb: e.tensor_scalar(bb["ncum"][:], bb["cum"][:], -1.0, None, ALU.mult), reads=[k_("cum", d)], writes=[k_("ncum", d)])
                    S.op("act", lambda e, bb=bb: e.activation(bb["Ecum"][:], bb["cum"][:], AF.Exp), reads=[k_("cum", d)], writes=[k_("Ecum", d)])
                    S.op("dve", lambda e, bb=bb, pc=pc: e.tensor_tensor(bb["Gd"][:], pc[:, 32:64], bb["cum"][:], ALU.subtract), reads=[pck, k_("cum", d)], writes=[k_("Gd", d)])
                    S.op("act", lambda e, bb=bb: e.activation(bb["Gd"][:], bb["Gd"][:], AF.Exp), reads=[k_("Gd", d)], writes=[k_("Gd", d)])
                    S.op("act", lambda e, bb=bb, pc=pc: e.activation(bb["Etot"][:], pc[:, 32:64], AF.Exp), reads=[pck], writes=[k_("Etot", d)])
                    S.op("dve", lambda e, bb=bb: e.tensor_tensor(bb["xdt"][:], bb["XT"][:], bb["DT"][:].unsqueeze(2).to_broadcast([128, 32, 64]), ALU.mult), reads=[k_("XT", d), k_("DT", d)], writes=[k_("xdt", d)])
                    S.op("dve", lambda e, bb=bb: e.tensor_tensor(bb["xs"][:], bb["xdt"][:], bb["Gd"][:].unsqueeze(2).to_broadcast([128, 32, 64]), ALU.mult), reads=[k_("xdt", d), k_("Gd", d)], writes=[k_("xs", d)])
                    if c >= 2:
                        S.op("dve", lambda e, bb=bb, mi=mi: e.tensor_tensor(bb["rhsA"][:], msk[:, mi, :].unsqueeze(1).to_broadcast([128, 32, 128]), bb["DA"][:].unsqueeze(2).to_broadcast([128, 32, 128]), ALU.mult), reads=["msk", k_("DA", d)], writes=[k_("rhsA", d)])
                        pcb, pcbk = nextps_()
                        for g in range(4):
                            S.op("pe", lambda e, bb=bb, g=g, pcb=pcb: e.matmul(pcb[:, g * 128:(g + 1) * 128], bb["BF"][:, g, :], bb["CF"][:, g, :], start=True, stop=True), reads=[k_("BF", d), k_("CF", d)], writes=[pcbk])
                        S.op("act", lambda e, bb=bb, pcb=pcb: e.copy(bb["CBT"][:], pcb[:].rearrange("p (g t) -> p g t", g=4)), reads=[pcbk], writes=[k_("CBT", d)])
                def seg_part(g):
                    for d, c, t0, bb in info:
                        if c < 2:
                            continue
                        sgn, mtn, ytn, tin = "seg%d" % (g % 2), "MT%d" % (g % 2), "yt%d" % (g % 2), "ti%d" % (g % 2)
                        for half in range(2):
                            p4, p4k = nextps_()
                            for e4 in range(4):
                                h = g * 8 + half * 4 + e4
                                o_ = p4[:, e4 * 128:(e4 + 1) * 128]
                                S.op("pe", lambda e, bb=bb, h=h, o_=o_: e.matmul(o_, ones128[:], bb["rhsA"][:, h, :], start=True, stop=False), reads=["ones128", k_("rhsA", d)], writes=[p4k])
                                S.op("pe", lambda e, d=d, o_=o_: e.matmul(o_, ident32[:], negm[:, d, :], start=False, stop=True), reads=["ident32", "negm"], writes=[p4k])
                            for e4 in range(4):
                                h = g * 8 + half * 4 + e4
                                S.op("act", lambda e, bb=bb, h=h, e4=e4, half=half, p4=p4, sgn=sgn: e.activation(bb[sgn][:, half * 4 + e4, :], p4[:, e4 * 128:(e4 + 1) * 128], AF.Exp, bias=bb["ncum"][:, h:h + 1]),
                                     reads=[p4k, k_("ncum", d)], writes=[k_(sgn, d)])
                        S.op("dve", lambda e, bb=bb, g=g, sgn=sgn, mtn=mtn: e.tensor_tensor(bb[mtn][:], bb[sgn][:], bb["CBT"][:, g, :].unsqueeze(1).to_broadcast([128, 8, 128]), ALU.mult),
                             reads=[k_(sgn, d), k_("CBT", d)], writes=[k_(mtn, d)])

                def y_part(g):
                    for d, c, t0, bb in info:
                        if c < 2:
                            continue
                        sgn, mtn, ytn, tin = "seg%d" % (g % 2), "MT%d" % (g % 2), "yt%d" % (g % 2), "ti%d" % (g % 2)
                        py, pyk = nextps_(); pi_, pik = nextps_()
                        for e8 in range(8):
                            S.op("pe", lambda e, bb=bb, g=g, e8=e8, py=py, mtn=mtn: e.matmul(py[:, e8 * 64:(e8 + 1) * 64], bb[mtn][:, e8, :], bb["xdt"][:, g * 8 + e8, :], start=True, stop=True),
                                 reads=[k_(mtn, d), k_("xdt", d)], writes=[pyk])
                        S.op("pe", lambda e, bb=bb, g=g, pi_=pi_: e.matmul(pi_[:], bb["CF"][:, g, :], bb["Hb"][:, g, :], start=True, stop=True), reads=[k_("CF", d), k_("Hb", d)], writes=[pik])
                        S.op("dve", lambda e, bb=bb, g=g, pi_=pi_, tin=tin: e.tensor_tensor(bb[tin][:].rearrange("p (h q) -> p h q", h=8), pi_[:].rearrange("p (h q) -> p h q", h=8), bb["Ecum"][:, g * 8:(g + 1) * 8].unsqueeze(2).to_broadcast([128, 8, 64]), ALU.mult),
                             reads=[pik, k_("Ecum", d)], writes=[k_(tin, d)])
                        S.op("dve", lambda e, bb=bb, py=py, tin=tin, ytn=ytn: e.tensor_tensor(bb[ytn][:], py[:], bb[tin][:], ALU.add), reads=[pyk, k_(tin, d)], writes=[k_(ytn, d)])
                        l0 = t0 - CTX
                        S.dma("pool", YD_s[d, l0:l0 + 128, g * 512:(g + 1) * 512], bb[ytn][:], reads=[k_(ytn, d)])

                for g in range(5):
                    if g < 4:
                        seg_part(g)
                    if g >= 1:
                        y_part(g - 1)
                for g in range(4):
                    for d, c, t0, bb in info:
                        ph, phk = nextps_()
                        S.op("pe", lambda e, bb=bb, g=g, ph=ph: e.matmul(ph[:], bb["BT"][:, g * 128:(g + 1) * 128], bb["xs"][:, g * 8:(g + 1) * 8, :].rearrange("p h q -> p (h q)"), start=True, stop=True),
                             reads=[k_("BT", d), k_("xs", d)], writes=[phk])
                        S.op("dve", lambda e, bb=bb, g=g: e.tensor_tensor(bb["H32"][:, g, :].rearrange("p (h q) -> p h q", h=8), bb["H32"][:, g, :].rearrange("p (h q) -> p h q", h=8), bb["Etot"][:, g * 8:(g + 1) * 8].unsqueeze(2).to_broadcast([128, 8, 64]), ALU.mult),
                             reads=[k_("H32", d), k_("Etot", d)], writes=[k_("H32", d)])
                        S.op("dve", lambda e, bb=bb, g=g, ph=ph: e.tensor_tensor(bb["H32"][:, g, :], bb["H32"][:, g, :], ph[:], ALU.add), reads=[phk, k_("H32", d)], writes=[k_("H32", d)])
                        S.op("act", lambda e, bb=bb, g=g: e.copy(bb["Hb"][:, g, :], bb["H32"][:, g, :]), reads=[k_("H32", d)], writes=[k_("Hb", d)])
            S.barrier()

    if dbg == "S2":
        s2_scan(6)
        o = dout("dbg", [2, 512, 2048])
        S.dma("sp", o[0], YD_s[0, 0:512, :], reads=[])
        S.dma("sp", o[1], YD_s[1, SEQ - 512:SEQ, :], reads=[])
        S.finish()
        return
    s2_scan()

    YA_s = nc.dram_tensor("rw_ya", [1024, SEQ], BF16).ap()
    with contextlib.ExitStack() as st:
        lnw = sb("lnw", [64, 16], st=st); lnb = sb("lnb", [64, 16], st=st)
        S.dma("sp", lnw[:], rwlnw_d.rearrange("(h v) -> v h", v=64), writes=["lnw"], allow_slow_non_contiguous=True)
        S.dma("sp", lnb[:], rwlnb_d.rearrange("(h v) -> v h", v=64), writes=["lnb"], allow_slow_non_contiguous=True)
        gneps = sb("gneps", [64, 1], st=st)
        S.op("dve", lambda e: e.memset(gneps[:], 64e-5), writes=["gneps"])
        ones64 = sb("ones64", [64, 64], st=st)
        S.op("dve", lambda e: e.memset(ones64[:], 1.0), writes=["ones64"])
        def T2(nm, dt=F32):
            return [sb("r3%s_%d" % (nm, i), [64, 4, 128], dt, st=st) for i in range(2)]
        yf_, yb_, ysq_, mm_, vv_, yo_ = T2("yf"), T2("yb"), T2("ysq"), T2("mm"), T2("vv"), T2("yo", BF16)
        bvt_, gt_ = T2("bvt", BF16), T2("gt", BF16)
        it = 0
        for hg in range(1 if dbg == "R3" else 4):
            for blk in range(SEQ // 128):
                b = it % 2; it += 1
                sfx = "_%d" % b
                l0 = blk * 128
                yf, yb, ysq, mm, vv, yo, bvt, gt = (x[b] for x in (yf_, yb_, ysq_, mm_, vv_, yo_, bvt_, gt_))
                S.dma("sp", yf[:], YS[0, hg * 4:hg * 4 + 4, :, l0:l0 + 128].rearrange("h v t -> v h t"), writes=["yf" + sfx])
                S.dma("sp", yb[:], YS[1, hg * 4:hg * 4 + 4, :, l0:l0 + 128].rearrange("h v t -> v h t"), writes=["yb" + sfx])
                S.dma("sp", bvt[:], BV_s[hg * 256:(hg + 1) * 256, l0:l0 + 128].rearrange("(h v) t -> v h t", v=64), writes=["bvt" + sfx])
                S.dma("sp", gt[:], G_s[hg * 256:(hg + 1) * 256, l0:l0 + 128].rearrange("(h v) t -> v h t", v=64), writes=["gt" + sfx])
                S.op("dve", lambda e: e.tensor_tensor(yf[:], yf[:], yb[:], ALU.add), reads=["yf" + sfx, "yb" + sfx], writes=["yf" + sfx])
                S.op("act", lambda e: e.activation(ysq[:], yf[:], AF.Square), reads=["yf" + sfx], writes=["ysq" + sfx])
                p1, p1k = nextps(); p2, p2k = nextps()
                S.op("pe", lambda e: e.matmul(p1[0:64, :], ones64[:], yf[:].rearrange("p h t -> p (h t)"), start=True, stop=True), reads=["ones64", "yf" + sfx], writes=[p1k])
                S.op("pe", lambda e: e.matmul(p2[0:64, :], ones64[:], ysq[:].rearrange("p h t -> p (h t)"), start=True, stop=True), reads=["ones64", "ysq" + sfx], writes=[p2k])
                v3 = lambda t: t[0:64, :].rearrange("p (h t) -> p h t", h=4)
                S.op("act", lambda e: e.activation(mm[:], v3(p1), AF.Identity, scale=1.0 / 64), reads=[p1k], writes=["mm" + sfx])
                S.op("dve", lambda e: e.tensor_tensor(vv[:], mm[:], mm[:], ALU.mult), reads=["mm" + sfx], writes=["vv" + sfx])
                S.op("dve", lambda e: e.scalar_tensor_tensor(vv[:], v3(p2), 1.0 / 64, vv[:], ALU.mult, ALU.subtract), reads=[p2k, "vv" + sfx], writes=["vv" + sfx])
                S.op("act", lambda e: e.activation(vv[:], vv[:], AF.Sqrt, bias=gneps[:]), reads=["vv" + sfx, "gneps"], writes=["vv" + sfx])
                S.op("dve", lambda e: e.reciprocal(vv[:], vv[:]), reads=["vv" + sfx], writes=["vv" + sfx])
                S.op("dve", lambda e: e.tensor_tensor(yf[:], yf[:], mm[:], ALU.subtract), reads=["yf" + sfx, "mm" + sfx], writes=["yf" + sfx])
                S.op("dve", lambda e: e.tensor_tensor(yf[:], yf[:], vv[:], ALU.mult), reads=["yf" + sfx, "vv" + sfx], writes=["yf" + sfx])
                S.op("dve", lambda e: e.tensor_tensor(yf[:], yf[:], lnw[:, hg * 4:hg * 4 + 4].unsqueeze(2).to_broadcast([64, 4, 128]), ALU.mult), reads=["yf" + sfx, "lnw"], writes=["yf" + sfx])
                S.op("dve", lambda e: e.tensor_tensor(yf[:], yf[:], lnb[:, hg * 4:hg * 4 + 4].unsqueeze(2).to_broadcast([64, 4, 128]), ALU.add), reads=["yf" + sfx, "lnb"], writes=["yf" + sfx])
                S.op("dve", lambda e: e.tensor_tensor(yf[:], yf[:], bvt[:], ALU.add), reads=["yf" + sfx, "bvt" + sfx], writes=["yf" + sfx])
                S.op("dve", lambda e: e.tensor_tensor(yo[:], yf[:], gt[:], ALU.mult), reads=["yf" + sfx, "gt" + sfx], writes=["yo" + sfx])
                S.dma("pool", YA_s[hg * 256:(hg + 1) * 256, l0:l0 + 128].rearrange("(h v) t -> v h t", v=64), yo[:], reads=["yo" + sfx])
        S.barrier()
    if dbg == "R3":
        o = dout("dbg", [256, SEQ], BF16)
        S.dma("sp", o, YA_s[0:256, :], reads=[])
        S.finish()
        return

    ssmd_d = din("ssm_d", [32]); ssmn_d = din("ssm_norm", [2048]); projb_d = din("proj_b", [2048, 1024])
    proja_d = din("proj_a", [1024, 1024]); wout_d = din("w_out", [1024, 1024])
    n1post_d = din("norm1_post", [D]); n2pre_d = din("norm2_pre", [D]); n2post_d = din("norm2_post", [D])
    router_d = din("router", [D, 16])
    PBG_s = nc.dram_tensor("mg_pbg", [SEQ, 1024], BF16).ap()
    X1_s = nc.dram_tensor("x1_s", [SEQ, D], F32).ap()
    H2T_s = nc.dram_tensor("h2t_s", [D, SEQ], BF16).ap()
    AFT_s = nc.dram_tensor("aft_s", [16, SEQ], F32).ap()

    def rstd_of(ssq, n, key):
        S.op("act", lambda e: e.activation(ssq, ssq, AF.Sqrt, bias=epsc[:], scale=1.0 / n), reads=[key, "epsc"], writes=[key])
        S.op("dve", lambda e: e.reciprocal(ssq, ssq), reads=[key], writes=[key])

    with contextlib.ExitStack() as st:
        pbw = sb("pbw", [128, 16, 1024], BF16, st=st)
        stg = [sb("pbstg%d" % i, [128, 2, 1024], st=st) for i in range(2)]
        for q_ in range(8):
            S.dma("sp", stg[q_ % 2][:], projb_d[q_ * 256:(q_ + 1) * 256, :].rearrange("(cc p) d -> p cc d", p=128), writes=["pbstg%d" % (q_ % 2)])
            S.op("pool", lambda e, q_=q_: e.tensor_copy(pbw[:, 2 * q_:2 * q_ + 2, :], stg[q_ % 2][:]), reads=["pbstg%d" % (q_ % 2)], writes=["pbw"])
        Dbc = sb("Dbc", [128, 32], st=st); snb = sb("snb", [128, 2048], st=st)
        S.dma("sp", Dbc[:], ssmd_d.partition_broadcast(128), writes=["Dbc"])
        S.dma("sp", snb[:], ssmn_d.partition_broadcast(128), writes=["snb"])
        yf_ = [sb("s3yf%d" % i, [128, 2048], st=st) for i in range(2)]
        yb_ = [sb("s3yb%d" % i, [128, 2048], st=st) for i in range(2)]
        xt_ = [sb("s3xt%d" % i, [128, 2048], BF16, st=st) for i in range(2)]
        zs_ = [sb("s3zs%d" % i, [128, 2048], BF16, st=st) for i in range(2)]
        gb_ = [sb("s3gb%d" % i, [128, 1024], BF16, st=st) for i in range(2)]
        pbg_ = [sb("s3pbg%d" % i, [128, 1024], BF16, st=st) for i in range(2)]
        s3junk = sb("s3junk", [128, 2048], st=st)
        ynb = sb("s3ynb", [128, 2048], BF16, st=st); ynT = sb("s3ynT", [128, 16, 128], BF16, st=st)
        ssq_ = [sb("s3ss%d" % i, [128, 1], st=st) for i in range(2)]
        PBG_v = PBG_s.rearrange("(r c) d -> c r d", c=64)
        for tt in range(32 if dbg != "S3" else 2):
            b = tt % 2
            sfx = "%d" % b
            yf, yb, xt, zs, gbt, pbg, ssq = yf_[b], yb_[b], xt_[b], zs_[b], gb_[b], pbg_[b], ssq_[b]
            S.dma("sp", yf[:], YD_s[0, tt * 128:(tt + 1) * 128, :], writes=["s3yf" + sfx])
            S.dma("sp", yb[:], YD_s[1, tt * 128:(tt + 1) * 128, :], writes=["s3yb" + sfx])
            S.dma("sp", xt[:], XT_s[CTX + tt * 128:CTX + (tt + 1) * 128, :], writes=["s3xt" + sfx])
            S.dma("sp", zs[:], ZS_s[tt * 128:(tt + 1) * 128, :], writes=["s3zs" + sfx])
            S.dma("sp", gbt[:], GB_s[tt * 128:(tt + 1) * 128, :], writes=["s3gb" + sfx])
            S.op("dve", lambda e: e.tensor_tensor(yf[:], yf[:], yb[:], ALU.add), reads=["s3yf" + sfx, "s3yb" + sfx], writes=["s3yf" + sfx])
            S.op("dve", lambda e: e.tensor_tensor(yb[:].rearrange("p (h q) -> p h q", h=32), xt[:].rearrange("p (h q) -> p h q", h=32), Dbc[:].unsqueeze(2).to_broadcast([128, 32, 64]), ALU.mult),
                 reads=["s3xt" + sfx, "Dbc", "s3yb" + sfx], writes=["s3yb" + sfx])
            S.op("dve", lambda e: e.tensor_tensor(yf[:], yf[:], yb[:], ALU.add), reads=["s3yf" + sfx, "s3yb" + sfx], writes=["s3yf" + sfx])
            S.op("dve", lambda e: e.tensor_tensor(yf[:], yf[:], zs[:], ALU.mult), reads=["s3yf" + sfx, "s3zs" + sfx], writes=["s3yf" + sfx])
            S.op("act", lambda e: e.activation(s3junk[:], yf[:], AF.Square, accum_out=ssq[:]), reads=["s3yf" + sfx], writes=["s3junk", "s3ss" + sfx])
            rstd_of(ssq[:], 2048, "s3ss" + sfx)
            S.op("dve", lambda e: e.scalar_tensor_tensor(ynb[:], yf[:], ssq[:], snb[:], ALU.mult, ALU.mult), reads=["s3yf" + sfx, "s3ss" + sfx, "snb"], writes=["s3ynb"])
            for hh in range(2):
                for j in range(8):
                    cc = hh * 8 + j
                    S.op("pe", lambda e, cc=cc, j=j, hh=hh: e.transpose(PSB[hh][:, j * 128:(j + 1) * 128], ynb[:, cc * 128:(cc + 1) * 128], identb[:]), reads=["s3ynb", "identb"], writes=["PSB%d" % hh])
                S.op("act", lambda e, hh=hh: e.copy(ynT[:, hh * 8:(hh + 1) * 8, :], PSB[hh][:].rearrange("p (j t) -> p j t", j=8)), reads=["PSB%d" % hh], writes=["s3ynT"])
            for half in range(2):
                pt, pk = nextps_()
                for cc in range(16):
                    S.op("pe", lambda e, cc=cc, half=half, pt=pt: e.matmul(pt[:], ynT[:, cc, :], pbw[:, cc, half * 512:(half + 1) * 512], start=(cc == 0), stop=(cc == 15)), reads=["s3ynT", "pbw"], writes=[pk])
                S.op("dve", lambda e, half=half, pt=pt: e.tensor_tensor(pbg[:, half * 512:(half + 1) * 512], pt[:], gbt[:, half * 512:(half + 1) * 512], ALU.mult), reads=[pk, "s3gb" + sfx], writes=["s3pbg" + sfx])
            for j in range(2):
                S.dma("pool", PBG_v[2 * tt + j], pbg[j * 64:(j + 1) * 64, :], reads=["s3pbg" + sfx])
        S.barrier()
    if dbg == "S3":
        o = dout("dbg", [4, 64, 1024], BF16)
        S.dma("sp", o, PBG_v[0:4], reads=[])
        S.finish()
        return

    with contextlib.ExitStack() as st:
        def load_sq(name, src):
            wbf = sb(name, [128, 8, 1024], BF16, st=st)
            for q_ in range(4):
                S.dma("sp", stg2[q_ % 2][:], src[q_ * 256:(q_ + 1) * 256, :].rearrange("(cc p) d -> p cc d", p=128), writes=["mstg%d" % (q_ % 2)])
                S.op("pool", lambda e, q_=q_: e.tensor_copy(wbf[:, 2 * q_:2 * q_ + 2, :], stg2[q_ % 2][:]), reads=["mstg%d" % (q_ % 2)], writes=[name])
            return wbf
        stg2 = [sb("mstg%d" % i, [128, 2, 1024], st=st) for i in range(2)]
        paw = load_sq("paw", proja_d); wow = load_sq("wow", wout_d)
        rtr = sb("rtr", [128, 8, 16], st=st)
        S.dma("sp", rtr[:], router_d.rearrange("(k p) e -> p k e", p=128), writes=["rtr"])
        nb = sb("nbc", [128, 3, D], st=st)
        for i_, src in enumerate((n1post_d, n2pre_d, n2post_d)):
            S.dma("sp", nb[:, i_, :], src.partition_broadcast(128), writes=["nbc"])
        S.op("dve", lambda e: e.tensor_tensor(modL[:, 2 * D:3 * D], modL[:, 2 * D:3 * D], nb[:, 0, :], ALU.mult), reads=["nbc", "mod"], writes=["mod"])
        S.op("dve", lambda e: e.scalar_tensor_tensor(modL[:, 4 * D:5 * D], modL[:, 4 * D:5 * D], 1.0, nb[:, 1, :], ALU.add, ALU.mult), reads=["nbc", "mod"], writes=["mod"])
        S.op("dve", lambda e: e.tensor_tensor(modL[:, 5 * D:6 * D], modL[:, 5 * D:6 * D], nb[:, 2, :], ALU.mult), reads=["nbc", "mod"], writes=["mod"])
        S.op("dve", lambda e: e.tensor_copy(g2t[:], modL[:, 5 * D:6 * D]), reads=["mod"], writes=["g2t"])
        yat_ = [sb("myat%d" % i, [128, 8, 128], BF16, st=st) for i in range(2)]
        gat_ = [sb("mgat%d" % i, [128, 1024], BF16, st=st) for i in range(2)]
        pbt_ = [sb("mpbt%d" % i, [128, 1024], BF16, st=st) for i in range(2)]
        xin_ = [sb("mxin%d" % i, [128, 1024], st=st) for i in range(2)]
        mrg = sb("mmrg", [128, 1024], BF16, st=st); mrgT = sb("mmrgT", [128, 8, 128], BF16, st=st)
        tmpm = sb("mtmp", [128, 1024], st=st); ml = sb("mml", [128, 1024], st=st); mjunk = sb("mjunk", [128, 1024], st=st)
        x1_ = [sb("mx1%d" % i, [128, 1024], st=st) for i in range(2)]
        h2 = sb("mh2", [128, 1024], st=st); h2T32 = sb("mh2T32", [128, 8, 128], st=st); h2Tb_ = [sb("mh2Tb%d" % i, [128, 8, 128], BF16, st=st) for i in range(2)]
        lg = sb("mlg", [128, 16], st=st); lgs = sb("mlgs", [128, 1], st=st); affT_ = [sb("maffT%d" % i, [16, 128], st=st) for i in range(2)]
        ssa = sb("mssa", [128, 1], st=st); ssb = sb("mssb", [128, 1], st=st)
        YA_v = YA_s.rearrange("(cc p) t -> p cc t", p=128)
        H2T_v = H2T_s.rearrange("(k p) t -> p k t", p=128)
        for tt in range(32):
            b = tt % 2
            sfx = "%d" % b
            t0 = tt * 128
            yat, gat, pbt, xin, x1, h2Tb, affT = yat_[b], gat_[b], pbt_[b], xin_[b], x1_[b], h2Tb_[b], affT_[b]
            S.dma("sp", yat[:], YA_v[:, :, t0:t0 + 128], writes=["myat" + sfx])
            S.dma("sp", gat[:], GA_s[t0:t0 + 128, :], writes=["mgat" + sfx])
            S.dma("sp", pbt[:], PBG_s[t0:t0 + 128, :], writes=["mpbt" + sfx])
            S.dma("sp", xin[:], x_d[t0:t0 + 128, :], writes=["mxin" + sfx])
            for half in range(2):
                pt, pk = nextps_()
                for cc in range(8):
                    S.op("pe", lambda e, cc=cc, half=half, pt=pt: e.matmul(pt[:], yat[:, cc, :], paw[:, cc, half * 512:(half + 1) * 512], start=(cc == 0), stop=(cc == 7)), reads=["myat" + sfx, "paw"], writes=[pk])
                hs = slice(half * 512, (half + 1) * 512)
                S.op("dve", lambda e, pt=pt, hs=hs: e.tensor_tensor(tmpm[:, hs], pt[:], gat[:, hs], ALU.mult), reads=[pk, "mgat" + sfx], writes=["mtmp"])
                S.op("dve", lambda e, hs=hs: e.tensor_tensor(mrg[:, hs], tmpm[:, hs], pbt[:, hs], ALU.add), reads=["mtmp", "mpbt" + sfx], writes=["mmrg"])
            for j in range(8):
                S.op("pe", lambda e, j=j: e.transpose(PSB[0][:, j * 128:(j + 1) * 128], mrg[:, j * 128:(j + 1) * 128], identb[:]), reads=["mmrg", "identb"], writes=["PSB0"])
            S.op("act", lambda e: e.copy(mrgT[:], PSB[0][:].rearrange("p (j t) -> p j t", j=8)), reads=["PSB0"], writes=["mmrgT"])
            for half in range(2):
                pt, pk = nextps_()
                for k in range(8):
                    S.op("pe", lambda e, k=k, half=half, pt=pt: e.matmul(pt[:], mrgT[:, k, :], wow[:, k, half * 512:(half + 1) * 512], start=(k == 0), stop=(k == 7)), reads=["mmrgT", "wow"], writes=[pk])
                S.op("act", lambda e, half=half, pt=pt: e.copy(ml[:, half * 512:(half + 1) * 512], pt[:]), reads=[pk], writes=["mml"])
            S.op("act", lambda e: e.activation(mjunk[:], ml[:], AF.Square, accum_out=ssa[:]), reads=["mml"], writes=["mjunk", "mssa"])
            rstd_of(ssa[:], D, "mssa")
            S.op("dve", lambda e: e.scalar_tensor_tensor(tmpm[:], ml[:], ssa[:], modL[:, 2 * D:3 * D], ALU.mult, ALU.mult), reads=["mml", "mssa", "mod"], writes=["mtmp"])
            S.op("dve", lambda e: e.tensor_tensor(x1[:], tmpm[:], xin[:], ALU.add), reads=["mtmp", "mxin" + sfx], writes=["mx1" + sfx])
            S.dma("pool", X1_s[t0:t0 + 128, :], x1[:], reads=["mx1" + sfx])
            S.op("act", lambda e: e.activation(mjunk[:], x1[:], AF.Square, accum_out=ssb[:]), reads=["mx1" + sfx], writes=["mjunk", "mssb"])
            rstd_of(ssb[:], D, "mssb")
            S.op("dve", lambda e: e.scalar_tensor_tensor(h2[:], x1[:], ssb[:], modL[:, 4 * D:5 * D], ALU.mult, ALU.mult), reads=["mx1" + sfx, "mssb", "mod"], writes=["mh2"])
            S.op("dve", lambda e: e.tensor_tensor(h2[:], h2[:], modL[:, 3 * D:4 * D], ALU.add), reads=["mh2", "mod"], writes=["mh2"])
            for hh in range(2):
                pt, pk = nextps_()
                for j in range(4):
                    kk_ = hh * 4 + j
                    S.op("pe", lambda e, j=j, kk_=kk_, pt=pt: e.transpose(pt[:, j * 128:(j + 1) * 128], h2[:, kk_ * 128:(kk_ + 1) * 128], ident32[:]), reads=["mh2", "ident32"], writes=[pk])
                S.op("act", lambda e, hh=hh, pt=pt: e.copy(h2T32[:, hh * 4:(hh + 1) * 4, :], pt[:].rearrange("p (j t) -> p j t", j=4)), reads=[pk], writes=["mh2T32"])
            S.op("dve", lambda e: e.tensor_copy(h2Tb[:], h2T32[:]), reads=["mh2T32"], writes=["mh2Tb" + sfx])
            S.dma("pool", H2T_v[:, :, t0:t0 + 128], h2Tb[:], reads=["mh2Tb" + sfx])
            pt, pk = nextps_()
            for k in range(8):
                S.op("pe", lambda e, k=k, pt=pt: e.matmul(pt[:, 0:16], h2T32[:, k, :], rtr[:, k, :], start=(k == 0), stop=(k == 7)), reads=["mh2T32", "rtr"], writes=[pk])
            S.op("act", lambda e, pt=pt: e.activation(lg[:], pt[:, 0:16], AF.Exp, accum_out=lgs[:]), reads=[pk], writes=["mlg", "mlgs"])
            S.op("dve", lambda e: e.reciprocal(lgs[:], lgs[:]), reads=["mlgs"], writes=["mlgs"])
            S.op("dve", lambda e: e.tensor_scalar(lg[:], lg[:], lgs[:], None, ALU.mult), reads=["mlg", "mlgs"], writes=["mlg"])
            pt2, pk2 = nextps_()
            S.op("pe", lambda e, pt2=pt2: e.transpose(pt2[0:16, 0:128], lg[:], ident32[:]), reads=["mlg", "ident32"], writes=[pk2])
            S.op("act", lambda e, pt2=pt2: e.copy(affT[:], pt2[0:16, 0:128]), reads=[pk2], writes=["maffT" + sfx])
            S.dma("pool", AFT_s[:, t0:t0 + 128], affT[:], reads=["maffT" + sfx])
        S.barrier()
    if dbg == "M":
        o = dout("dbgX1", [512, D]); S.dma("sp", o, X1_s[1024:1536], reads=[])
        o = dout("dbgH2", [D, 512], BF16); S.dma("sp", o, H2T_s[:, 1024:1536], reads=[])
        o = dout("dbgAF", [16, SEQ]); S.dma("sp", o, AFT_s, reads=[])
        S.finish()
        return

    stM.close()

    w1_d = din("exp_w1", [16, D, 1024]); w3_d = din("exp_w3", [16, D, 1024]); w2_d = din("exp_w2", [16, 1024, D])
    selm_d = din("selm", [16, 16, 128])
    GM_s = nc.dram_tensor("gm_s", [16, SEQ], F32).ap()
    CAP = 2 * SEQ // 16
    with contextlib.ExitStack() as st:
        aff = sb("eaff", [16, SEQ], st=st); cmp_ = sb("ecmp", [16, SEQ], st=st)
        S.dma("sp", aff[:], AFT_s, writes=["eaff"])
        lo = sb("elo", [16, 1], st=st); hi = sb("ehi", [16, 1], st=st); mid = sb("emid", [16, 1], st=st)
        cnt = sb("ecnt", [16, 1], st=st); sel = sb("esel", [16, 1], st=st); dl = sb("edl", [16, 1], st=st); halfc = sb("ehalf", [16, 1], st=st)
        S.op("dve", lambda e: e.memset(lo[:], 0.0), writes=["elo"])
        S.op("dve", lambda e: e.memset(hi[:], 1.0), writes=["ehi"])
        S.op("dve", lambda e: e.memset(halfc[:], 0.5), writes=["ehalf"])
        for it in range(36):
            S.op("dve", lambda e: e.scalar_tensor_tensor(mid[:], lo[:], hi[:, 0:1], halfc[:], ALU.add, ALU.mult), reads=["elo", "ehi", "ehalf"], writes=["emid"])
            S.op("dve", lambda e: e.tensor_scalar(cmp_[:], aff[:], mid[:, 0:1], None, ALU.is_ge), reads=["eaff", "emid"], writes=["ecmp"])
            S.op("dve", lambda e: e.reduce_sum(cnt[:], cmp_[:], AX.X), reads=["ecmp"], writes=["ecnt"])
            S.op("dve", lambda e: e.tensor_scalar(sel[:], cnt[:], CAP - 0.5, None, ALU.is_ge), reads=["ecnt"], writes=["esel"])
            S.op("dve", lambda e: e.tensor_tensor(dl[:], mid[:], lo[:], ALU.subtract), reads=["emid", "elo"], writes=["edl"])
            S.op("dve", lambda e: e.scalar_tensor_tensor(lo[:], dl[:], sel[:, 0:1], lo[:], ALU.mult, ALU.add), reads=["edl", "esel", "elo"], writes=["elo"])
            S.op("dve", lambda e: e.tensor_tensor(dl[:], hi[:], mid[:], ALU.subtract), reads=["emid", "ehi"], writes=["edl"])
            S.op("dve", lambda e: e.scalar_tensor_tensor(hi[:], dl[:], sel[:, 0:1], mid[:], ALU.mult, ALU.add), reads=["edl", "esel", "emid"], writes=["ehi"])
        S.op("dve", lambda e: e.tensor_scalar(cmp_[:], aff[:], lo[:, 0:1], None, ALU.is_ge), reads=["eaff", "elo"], writes=["ecmp"])
        S.op("dve", lambda e: e.tensor_tensor(cmp_[:], cmp_[:], aff[:], ALU.mult), reads=["ecmp", "eaff"], writes=["ecmp"])
        S.dma("sp", GM_s, cmp_[:], reads=["ecmp"])
        S.barrier()
    if dbg == "E0":
        o = dout("dbg", [16, SEQ]); S.dma("sp", o, GM_s, reads=[])
        S.finish()
        return

    with contextlib.ExitStack() as st:
        selm = sb("selm_sb", [16, 16, 128], st=st)
        S.dma("sp", selm[:], selm_d, writes=["selm"])
        QT = 1024
        h2q = sb("eh2q", [128, 8, QT], BF16, st=st)
        yacc = sb("eyacc", [128, 8, QT], st=st)
        gmq = sb("egmq", [16, QT], st=st)
        wbs = [[sb("ew%d_%d" % (i, sset), [128, 8, 1024], BF16, st=st) for i in range(3)] for sset in range(2)]
        hg = sb("ehg", [128, 8, 512], BF16, st=st)
        sa_ = [sb("esa%d" % i, [128, 512], st=st) for i in range(2)]
        hb_ = [sb("ehb%d" % i, [128, 512], st=st) for i in range(2)]
        gmbs = sb("egmbs", [128, 512], st=st)
        ytok = sb("eytok", [128, D], st=st); x1t = sb("ex1t", [128, D], st=st); ejunk = sb("ejunk", [128, D], st=st)
        ess = sb("eess", [128, 1], st=st)
        NQ = SEQ // QT
        nq_run = NQ if dbg != "E1" else 1

        def load_expert(n):
            ex_ = n % 16
            for wi, src in enumerate((w1_d[ex_], w3_d[ex_], w2_d[ex_])):
                S.dma("pool", wbs[n % 2][wi][:], src.rearrange("(cc p) d -> p cc d", p=128), writes=["ew%d_%d" % (wi, n % 2)])

        load_expert(0)
        for qi in range(nq_run):
            T0 = qi * QT
            S.dma("sp", h2q[:], H2T_v[:, :, T0:T0 + QT], writes=["eh2q"])
            S.dma("sp", gmq[:], GM_s[:, T0:T0 + QT], writes=["egmq"])
            S.op("pool", lambda e: e.memset(yacc[:], 0.0), writes=["eyacc"])
            for ex in range(16):
                n_ = qi * 16 + ex
                if n_ + 1 < nq_run * 16:
                    load_expert(n_ + 1)
                wb = wbs[n_ % 2]
                wk = ["ew%d_%d" % (wi, n_ % 2) for wi in range(3)]
                for tg in range(QT // 512):
                    tl = tg * 512
                    pg, pgk = nextps_()
                    S.op("pe", lambda e, pg=pg: e.matmul(pg[:], selm[:, ex, :], gmq[:, tl:tl + 512], start=True, stop=True), reads=["selm", "egmq"], writes=[pgk])
                    S.op("act", lambda e, pg=pg: e.copy(gmbs[:], pg[:]), reads=[pgk], writes=["egmbs"])
                    for ft in range(8):
                        b = ft % 2
                        pa, pak = nextps_(); pb, pbk = nextps_()
                        for k in range(8):
                            S.op("pe", lambda e, k=k, pa=pa: e.matmul(pa[:], wb[0][:, k, ft * 128:(ft + 1) * 128], h2q[:, k, tl:tl + 512], start=(k == 0), stop=(k == 7)), reads=[wk[0], "eh2q"], writes=[pak])
                        for k in range(8):
                            S.op("pe", lambda e, k=k, pb=pb: e.matmul(pb[:], wb[1][:, k, ft * 128:(ft + 1) * 128], h2q[:, k, tl:tl + 512], start=(k == 0), stop=(k == 7)), reads=[wk[1], "eh2q"], writes=[pbk])
                        S.op("act", lambda e, pa=pa: e.activation(sa_[b][:], pa[:], AF.Silu), reads=[pak], writes=["esa%d" % b])
                        S.op("dve", lambda e, pb=pb: e.tensor_tensor(hb_[b][:], sa_[b][:], pb[:], ALU.mult), reads=["esa%d" % b, pbk], writes=["ehb%d" % b])
                        S.op("pool", lambda e: e.tensor_tensor(hg[:, ft, :], hb_[b][:], gmbs[:], ALU.mult), reads=["ehb%d" % b, "egmbs"], writes=["ehg"])
                    for dt_ in range(8):
                        py, pyk = nextps_()
                        for ft in range(8):
                            S.op("pe", lambda e, ft=ft, py=py: e.matmul(py[:], wb[2][:, ft, dt_ * 128:(dt_ + 1) * 128], hg[:, ft, :], start=(ft == 0), stop=(ft == 7)), reads=[wk[2], "ehg"], writes=[pyk])
                        S.op("dve", lambda e, py=py: e.tensor_tensor(yacc[:, dt_, tl:tl + 512], yacc[:, dt_, tl:tl + 512], py[:], ALU.add), reads=[pyk, "eyacc"], writes=["eyacc"])
            for tt in range(QT // 128):
                t0 = T0 + tt * 128
                S.dma("sp", x1t[:], X1_s[t0:t0 + 128, :], writes=["ex1t"])
                for hh in range(2):
                    pt, pk = nextps_()
                    for j in range(4):
                        dd = hh * 4 + j
                        S.op("pe", lambda e, j=j, dd=dd, pt=pt: e.transpose(pt[:, j * 128:(j + 1) * 128], yacc[:, dd, tt * 128:(tt + 1) * 128], ident32[:]), reads=["eyacc", "ident32"], writes=[pk])
                    S.op("act", lambda e, hh=hh, pt=pt: e.copy(ytok[:, hh * 512:(hh + 1) * 512], pt[:]), reads=[pk], writes=["eytok"])
                S.op("act", lambda e: e.activation(ejunk[:], ytok[:], AF.Square, accum_out=ess[:]), reads=["eytok"], writes=["ejunk", "eess"])
                rstd_of(ess[:], D, "eess")
                S.op("dve", lambda e: e.scalar_tensor_tensor(ytok[:], ytok[:], ess[:], g2t[:], ALU.mult, ALU.mult), reads=["eytok", "eess", "g2t"], writes=["eytok"])
                S.op("dve", lambda e: e.tensor_tensor(ytok[:], ytok[:], x1t[:], ALU.add), reads=["eytok", "ex1t"], writes=["eytok"])
                if out_d is not None:
                    S.dma("pool", out_d[t0:t0 + 128, :], ytok[:], reads=["eytok"])
                elif dbg == "E1":
                    if tt == 0:
                        dbg_o = dout("dbg", [QT, D])
                    S.dma("pool", dbg_o[tt * 128:(tt + 1) * 128, :], ytok[:], reads=["eytok"])
        S.barrier()

    S.finish()


def _prep_inputs(inputs):
    sq = {}
    for k, v in inputs.items():
        v = np.asarray(v)
        if k in ("x", "c", "ctx", "c_ctx"):
            sq[k] = v
        elif k == "rw_rk":
            sq[k] = v[0].reshape(1024)
        elif k in ("ssm_dt_bias", "ssm_a_log"):
            sq[k] = v[0].reshape(64)
        else:
            sq[k] = v[0]
    return sq


def kernel(**inputs):
    sq = _prep_inputs(inputs)
    nc = build(None)
    cst = consts()
    in_maps = []
    for b in range(8):
        full = dict(sq)
        full["x"] = sq["x"][b]
        full["ctx"] = sq["ctx"][b]
        full["c"] = sq["c"][b]
        full.update(cst)
        in_maps.append({k: np.ascontiguousarray(full[k], dtype=np.float32) for k in INPUT_NAMES})
    res = run_bass_kernel_spmd(nc, in_maps, core_ids=list(range(8)))
    return np.stack([np.asarray(r["out"], dtype=np.float32) for r in res.results], axis=0)
```
